# Optimizing a Trainium2 kernel written in Bass

```python
import jax, jax.numpy as jnp
from jax import lax
import numpy as np

D_MODEL = 1024
BATCH = 4
SEQ = 8192
DEPTH = 1

CHUNK = 64
EPS = 1e-6
RET_HEADS = 4
RET_DK = 64
RET_DV = 128
RET_THETA = 10000.0
DSA_HEADS = 8
DSA_DH = 64
IDX_HEADS = 4
IDX_DH = 64
DSA_TOPK_MAX = 256
Q_BLOCK = 128
ROPE_THETA = 500000.0
ROPE_DIM = DSA_DH // 4
PEER_HEADS = 8
PEER_NKEYS = 128
PEER_N_EXPERTS = PEER_NKEYS * PEER_NKEYS
PEER_DKEY = 256
PEER_TOPK = 16
PEER_BLOCK = 128
RET_QK = RET_HEADS * RET_DK
RET_V = RET_HEADS * RET_DV
DSA_W = DSA_HEADS * DSA_DH
IDX_Q = IDX_HEADS * IDX_DH
N_BRANCH = 2
IN_SIZES = (RET_QK, RET_QK, RET_V, RET_V, DSA_W, DSA_W, DSA_W, IDX_Q, IDX_DH, IDX_HEADS, N_BRANCH * D_MODEL)
D_IN = 2 * RET_QK + 2 * RET_V + 3 * DSA_W + IDX_Q + IDX_DH + IDX_HEADS + N_BRANCH * D_MODEL

kernel_name = "hybrid_retention_dsa_peer_block"


def rms_norm(x, g):
    xf = x.astype(jnp.float32)
    y = xf * lax.rsqrt(jnp.mean(xf * xf, axis=-1, keepdims=True) + EPS)
    return (y * g.astype(jnp.float32)).astype(x.dtype)


def rope(x, pos, rot_dim, theta):
    half = rot_dim // 2
    inv = theta ** (-jnp.arange(half, dtype=jnp.float32) / half)
    ang = pos.astype(jnp.float32)[:, None] * inv[None, :]
    cos = jnp.cos(ang)[None, :, None, :]
    sin = jnp.sin(ang)[None, :, None, :]
    xr = x[..., :rot_dim].astype(jnp.float32)
    x1, x2 = xr[..., :half], xr[..., half:]
    rot = jnp.concatenate([x1 * cos - x2 * sin, x1 * sin + x2 * cos], axis=-1).astype(x.dtype)
    return jnp.concatenate([rot, x[..., rot_dim:]], axis=-1)


def split_cols(a, sizes):
    outs, off = [], 0
    for s in sizes:
        outs.append(a[..., off:off + s])
        off += s
    return outs


def retention(q, k, v, gate, gn):
    f32 = jnp.float32
    B, L, H, dk = q.shape
    dv = v.shape[-1]
    C = CHUNK
    NC = L // C
    log_g = jnp.log(1.0 - 2.0 ** (-5.0 - jnp.arange(H, dtype=f32)))
    i = jnp.arange(C, dtype=f32)
    qc = q.astype(f32).reshape(B, NC, C, H, dk) * (dk ** -0.5)
    kc = k.astype(f32).reshape(B, NC, C, H, dk)
    vc = v.astype(f32).reshape(B, NC, C, H, dv)
    intra_decay = jnp.exp(log_g[:, None, None] * jnp.abs(i[:, None] - i[None, :]))
    scores = jnp.einsum('bnihd,bnjhd->bnhij', qc, kc) * intra_decay
    intra = jnp.einsum('bnhij,bnjhe->bnihe', scores, vc)
    k_decay = jnp.exp(log_g[None, :] * (C - 1 - i)[:, None])
    kv = jnp.einsum('bnjhd,bnjhe->nbhde', kc * k_decay[:, :, None], vc)
    chunk_decay = jnp.exp(log_g * C)[None, :, None, None]

    def step(state, kv_n):
        return chunk_decay * state + kv_n, state

    _, prev = lax.scan(step, jnp.zeros_like(kv[0]), kv)
    q_decay = jnp.exp(log_g[None, :] * (i + 1.0)[:, None])
    cross = jnp.einsum('bnihd,nbhde->bnihe', qc * q_decay[:, :, None], prev)
    y = (intra + cross).reshape(B, L, H, dv)
    mu = jnp.mean(y, axis=-1, keepdims=True)
    var = jnp.mean(jnp.square(y - mu), axis=-1, keepdims=True)
    y = ((y - mu) * lax.rsqrt(var + EPS)).reshape(B, L, H * dv) * gn.astype(f32)
    return (jax.nn.swish(gate.astype(f32)) * y).astype(gate.dtype)


def dsa_attention(q, k, v, q_idx, k_idx, w_idx, topk):
    f32 = jnp.float32
    B, L, H, dh = q.shape
    NB = L // Q_BLOCK
    key_chunk = jnp.arange(L) // CHUNK

    def to_blocks(a):
        return jnp.moveaxis(a.reshape(B, NB, Q_BLOCK, *a.shape[2:]), 1, 0)

    def block(args):
        qb, qib, wb, qpos = args
        qchunk = qpos // CHUNK
        admissible = key_chunk[None, :] <= qchunk[:, None]
        rel = jax.nn.relu(jnp.einsum('bqhd,bsd->bqhs', qib, k_idx).astype(f32))
        iscore = jnp.einsum('bqhs,bqh->bqs', rel, wb.astype(f32))
        iscore = jnp.where(admissible[None], iscore, -jnp.inf)
        _, sel = lax.top_k(iscore, topk)
        valid = (sel // CHUNK) <= qchunk[None, :, None]
        k_sel = jax.vmap(lambda kb, ib: kb[ib])(k, sel)
        v_sel = jax.vmap(lambda vb, ib: vb[ib])(v, sel)
        logits = jnp.einsum('bqhd,bqkhd->bhqk', qb, k_sel).astype(f32) * (dh ** -0.5)
        logits = jnp.where(valid[:, None], logits, -jnp.inf)
        p = jax.nn.softmax(logits, axis=-1).astype(v.dtype)
        return jnp.einsum('bhqk,bqkhd->bqhd', p, v_sel)

    qpos = jnp.arange(L).reshape(NB, Q_BLOCK)
    out = lax.map(block, (to_blocks(q), to_blocks(q_idx), to_blocks(w_idx), qpos))
    return jnp.moveaxis(out, 0, 1).reshape(B, L, H * dh)


def peer_ffn(h, w_q, sub_keys, u_tab, v_tab):
    f32 = jnp.float32
    B, L, D = h.shape
    T = PEER_BLOCK
    K = PEER_TOPK
    tokens = h.reshape(-1, T, D)

    def block(xb):
        q = (xb @ w_q).reshape(T, PEER_HEADS, 2, PEER_DKEY // 2)
        s = jnp.einsum('thcd,hckd->thck', q, sub_keys).astype(f32)
        s_top, i_top = lax.top_k(s, K)
        cand = s_top[:, :, 0, :, None] + s_top[:, :, 1, None, :]
        cand_idx = i_top[:, :, 0, :, None] * PEER_NKEYS + i_top[:, :, 1, None, :]
        best, pos = lax.top_k(cand.reshape(T, PEER_HEADS, K * K), K)
        expert = jnp.take_along_axis(cand_idx.reshape(T, PEER_HEADS, K * K), pos, axis=-1)
        g = jax.nn.softmax(best, axis=-1)
        u_sel = u_tab[expert]
        a = jax.nn.gelu(jnp.einsum('thkd,td->thk', u_sel, xb).astype(f32), approximate=False)
        return jnp.einsum('thk,thkd->td', (g * a).astype(xb.dtype), v_tab[expert])

    return lax.map(block, tokens).reshape(B, L, D)


def setup_inputs(seed: int = 0) -> dict:
    key = jax.random.key(seed)
    ks = jax.random.split(key, 16)
    D = D_MODEL
    nrm = lambda k, shape, scale: jax.random.normal(k, shape, jnp.float32) * scale
    return {
        "x": nrm(ks[0], (BATCH, SEQ, D), 1.0),
        "attn_norm": 1.0 + nrm(ks[1], (DEPTH, D), 0.01),
        "w_in": nrm(ks[2], (DEPTH, D, D_IN), D ** -0.5),
        "ret_gn": 1.0 + nrm(ks[3], (DEPTH, RET_V), 0.01),
        "w_ret_o": nrm(ks[4], (DEPTH, RET_V, D), RET_V ** -0.5),
        "w_dsa_o": nrm(ks[5], (DEPTH, DSA_W, D), DSA_W ** -0.5),
        "w_out": nrm(ks[6], (DEPTH, D, D), D ** -0.5),
        "ffn_norm": 1.0 + nrm(ks[7], (DEPTH, D), 0.01),
        "peer_wq": nrm(ks[8], (DEPTH, D, PEER_HEADS * PEER_DKEY), D ** -0.5),
        "peer_subkeys": nrm(ks[9], (DEPTH, PEER_HEADS, 2, PEER_NKEYS, PEER_DKEY // 2), (PEER_DKEY // 2) ** -0.5),
        "peer_u": nrm(ks[10], (DEPTH, PEER_N_EXPERTS, D), D ** -0.5),
        "peer_v": nrm(ks[11], (DEPTH, PEER_N_EXPERTS, D), PEER_HEADS ** -0.5),
        "final_norm": 1.0 + nrm(ks[12], (D,), 0.01),
    }


def reference(x, attn_norm, w_in, ret_gn, w_ret_o, w_dsa_o, w_out, ffn_norm, peer_wq, peer_subkeys, peer_u, peer_v, final_norm):
    B, L, D = x.shape
    pos = jnp.arange(L)
    topk = min(DSA_TOPK_MAX, L // 4)
    for layer in range(DEPTH):
        h = rms_norm(x, attn_norm[layer])
        proj = h @ w_in[layer]
        (rq, rk, rv, rg, dq, dk, dv, iq, ik, iw, gates) = split_cols(proj, IN_SIZES)
        rq = rope(rq.reshape(B, L, RET_HEADS, RET_DK), pos, RET_DK, RET_THETA)
        rk = rope(rk.reshape(B, L, RET_HEADS, RET_DK), pos, RET_DK, RET_THETA)
        y_ret = retention(rq, rk, rv.reshape(B, L, RET_HEADS, RET_DV), rg, ret_gn[layer])
        dq = rope(dq.reshape(B, L, DSA_HEADS, DSA_DH), pos, ROPE_DIM, ROPE_THETA)
        dk = rope(dk.reshape(B, L, DSA_HEADS, DSA_DH), pos, ROPE_DIM, ROPE_THETA)
        iq = rope(iq.reshape(B, L, IDX_HEADS, IDX_DH), pos, IDX_DH // 4, ROPE_THETA)
        ik = rope(ik.reshape(B, L, 1, IDX_DH), pos, IDX_DH // 4, ROPE_THETA).reshape(B, L, IDX_DH)
        iw = iw * ((IDX_HEADS ** -0.5) * (IDX_DH ** -0.5))
        y_dsa = dsa_attention(dq, dk, dv.reshape(B, L, DSA_HEADS, DSA_DH), iq, ik, iw, topk)
        g_ret, g_dsa = split_cols(jax.nn.sigmoid(gates), (D, D))
        merged = g_ret * (y_ret @ w_ret_o[layer]) + g_dsa * (y_dsa @ w_dsa_o[layer])
        x = x + merged @ w_out[layer]
        h2 = rms_norm(x, ffn_norm[layer])
        x = x + peer_ffn(h2, peer_wq[layer], peer_subkeys[layer], peer_u[layer], peer_v[layer])
    return rms_norm(x, final_norm)
```

```python
from contextlib import ExitStack
import numpy as np
import ml_dtypes
import concourse.bass as bass
import concourse.mybir as mybir
from concourse.bass_utils import run_bass_kernel_spmd

F32 = mybir.dt.float32
BF = mybir.dt.bfloat16
I32 = mybir.dt.int32
U32 = mybir.dt.uint32
AF = mybir.ActivationFunctionType
OP = mybir.AluOpType
AX = mybir.AxisListType

ENGS = ("tensor", "vector", "scalar", "gpsimd", "sync")

D = 1024
L = 8192
LH = 4096
NT = 32
NCOL = 5504
EPS = 1e-6
NIT = 16
BIG = 1.0e30
NU = 4
DEBUG = False


class Res:
    __slots__ = ("name", "w", "r", "excl")

    def __init__(self, name):
        self.name = name
        self.w = None
        self.r = []
        self.excl = False


class Chan:
    __slots__ = ("key", "count")

    def __init__(self, key):
        self.key = key
        self.count = 0


class _Rec:
    def __init__(self):
        self.call = None

    def __getattr__(self, name):
        def f(*a, **kw):
            self.call = (name, a, kw)
            return self
        return f


class K:
    def __init__(self, nc, es):
        self.nc = nc
        self.es = es
        self.sems = {}
        self.cnt = {}
        self.clock = {e: {} for e in ENGS}
        self.ops = {e: [] for e in ENGS}
        self.chans = []
        self.serial = set()
        for e in ENGS:
            self._mksem("E_" + e)
        self.nres = 0

    def _mksem(self, key):
        self.sems[key] = self.es.enter_context(self.nc.semaphore(key))
        self.cnt[key] = 0

    def res(self, name=None):
        self.nres += 1
        return Res(name or f"r{self.nres}")

    def chan(self, name, serial=False):
        key = "D_" + name
        self._mksem(key)
        c = Chan(key)
        self.chans.append(c)
        if serial:
            self.serial.add(key)
        return c

    def _need(self, eng, reads, writes):
        mykey = "E_" + eng
        need = {}

        def add(tok, kind):
            if tok is None:
                return
            key, val = tok
            if key == mykey:
                if eng == "tensor":
                    return
            if need.get(key, 0) < val:
                need[key] = val

        for r in reads:
            add(r.w, "raw")
        for w in writes:
            add(w.w, "waw")
            for t in w.r:
                add(t, "war")
        clk = self.clock[eng]
        out = []
        for key, val in need.items():
            if clk.get(key, 0) >= val:
                continue
            if key.startswith("D_") and key not in self.serial:
                assert val == self.cnt[key], f"stale DMA token wait {key} {val} != {self.cnt[key]}"
            clk[key] = val
            out.append((key, val))
        return out

    def op(self, eng, fn, reads=(), writes=()):
        writes = list(writes) + [r for r in reads if r.excl and r not in writes]
        for key, val in self._need(eng, reads, writes):
            self.ops[eng].append(("w", key, val))
        key = "E_" + eng
        self.cnt[key] += 1
        tok = (key, self.cnt[key])
        rec = _Rec()
        fn(rec)
        self.ops[eng].append(("o", rec.call, key, 1))
        for r in reads:
            r.r.append(tok)
        for w in writes:
            w.w = tok
            w.r = []
        return tok

    def dma(self, eng, fn, chan, reads=(), writes=()):
        for key, val in self._need(eng, reads, writes):
            self.ops[eng].append(("w", key, val))
        chan.count += 16
        self.cnt[chan.key] = chan.count
        tok = (chan.key, chan.count)
        rec = _Rec()
        fn(rec)
        self.ops[eng].append(("o", rec.call, chan.key, 16))
        for r in reads:
            r.r.append(tok)
        for w in writes:
            w.w = tok
            w.r = []
        return tok

    def settle(self, chan, ress):
        for r in ress:
            r.w = (chan.key, chan.count)

    def wait_all(self, eng, chans=None, engines=True):
        clk = self.clock[eng]
        keys = []
        if engines:
            keys += ["E_" + e for e in ENGS if e != eng]
        keys += [c.key for c in (chans if chans is not None else self.chans)]
        for key in keys:
            val = self.cnt[key]
            if val > clk.get(key, 0):
                clk[key] = val
                self.ops[eng].append(("w", key, val))

    def barrier(self):
        for e in ENGS:
            self.wait_all(e)

    def flush(self):
        nc = self.nc
        ops = self.ops
        sems = self.sems

        def replay(e, lst):
            for it in lst:
                if it[0] == "w":
                    e.wait_ge(sems[it[1]], it[2])
                else:
                    name, a, kw = it[1]
                    getattr(e, name)(*a, **kw).then_inc(sems[it[2]], it[3])

        with nc.Block() as block:
            @block.tensor
            def _(e):
                replay(e, ops["tensor"])

            @block.vector
            def _(e):
                replay(e, ops["vector"])

            @block.scalar
            def _(e):
                replay(e, ops["scalar"])

            @block.gpsimd
            def _(e):
                replay(e, ops["gpsimd"])

            @block.sync
            def _(e):
                replay(e, ops["sync"])
        self.ops = {e: [] for e in ENGS}


class Buf:
    __slots__ = ("t", "r")

    def __init__(self, t, r):
        self.t = t
        self.r = r


def build_program(debug=False, only_a=False, nt_b=NT, tiles_a=None, tabconv=True):
    nc = bass.Bass("TRN2", target_bir_lowering=False)

    def din(name, shape, dt=F32):
        return nc.dram_tensor(name, list(shape), dt, kind="ExternalInput").ap()

    def dscr(name, shape, dt):
        return nc.dram_tensor(name, list(shape), dt, kind=("ExternalOutput" if debug else "Internal")).ap()

    xp = din("xp", [LH, D])
    xo = din("xo", [LH, D])
    ropetab = din("ropetab", [L, 80])
    w_in = din("w_in", [D, NCOL])
    w_ro = din("w_ro", [512, D])
    w_do = din("w_do", [512, D])
    w_out = din("w_out", [D, D])
    w_q = din("w_q", [D, 2048])
    subk = din("subk", [16, 128, 128])
    peer_u = din("peer_u", [16384, D])
    peer_v = din("peer_v", [16384, D])
    g_attn = din("g_attn", [128, D])
    g_gn = din("g_gn", [128, 512])
    g_ffn = din("g_ffn", [128, D])
    g_fin = din("g_fin", [128, D])
    c_ident = din("c_ident", [128, 128])
    c_kdec = din("c_kdec", [128, 512])
    c_qdec = din("c_qdec", [64, 512])
    c_dmat = din("c_dmat", [128, 512])
    c_gdec = din("c_gdec", [64, 512])
    c_diag = din("c_diag", [128, 2048])
    c_pbias = din("c_pbias", [128, 1])
    c_iota = din("c_iota", [128, 16])
    c_pow = din("c_pow", [128, NIT])
    c_nb = din("c_nb", [128, NT])
    out = nc.dram_tensor("out", [LH, D], F32, kind="ExternalOutput").ap()

    UB = nc.dram_tensor("UB", [16384, D], BF, kind="Internal").ap()
    VB = nc.dram_tensor("VB", [16384, D], BF, kind="Internal").ap()
    KTD = dscr("KTD", [64, 8, L], BF)
    VD = dscr("VD", [L, 8 * 65], BF)
    QTD = dscr("QTD", [NT, 64, 1024], BF)
    IQTD = dscr("IQTD", [NT, 64, 512], BF)
    IWD = dscr("IWD", [LH, 4], F32)
    MRET = dscr("MRET", [LH, D], BF)
    GDSA = dscr("GDSA", [LH, D], BF)
    if debug:
        DBG_X2 = nc.dram_tensor("DBG_X2", [LH, D], F32, kind="ExternalOutput").ap()
        DBG_PEER = nc.dram_tensor("DBG_PEER", [LH, D], F32, kind="ExternalOutput").ap()
        DBG_YD = nc.dram_tensor("DBG_YD", [LH, 512], F32, kind="ExternalOutput").ap()
        DBG_LO = nc.dram_tensor("DBG_LO", [LH, 4], F32, kind="ExternalOutput").ap()
        DBG_E = nc.dram_tensor("DBG_E", [LH, 128], I32, kind="ExternalOutput").ap()

    with ExitStack() as es:
        k = K(nc, es)

        def sbuf(stack, name, shape, dt):
            return Buf(stack.enter_context(nc.sbuf_tensor(name, list(shape), dt)), k.res(name))

        def psum(stack, name, shape, dt):
            b = Buf(stack.enter_context(nc.psum_tensor(name, list(shape), dt)), k.res(name))
            b.r.excl = True
            return b

        def V(fn, reads=(), writes=()):
            return k.op("vector", fn, [b.r for b in reads], [b.r for b in writes])

        def A(fn, reads=(), writes=()):
            return k.op("scalar", fn, [b.r for b in reads], [b.r for b in writes])

        def T(fn, reads=(), writes=()):
            return k.op("tensor", fn, [b.r for b in reads], [b.r for b in writes])

        def G(fn, reads=(), writes=()):
            return k.op("gpsimd", fn, [b.r for b in reads], [b.r for b in writes])

        def DMA(eng, fn, chan, reads=(), writes=()):
            return k.dma(eng, fn, chan, [b.r for b in reads], [b.r for b in writes])

        class DR:
            def __init__(self, name):
                self.r = k.res(name)

        T0 = psum(es, "T0", [128, 1024], BF)
        T1 = [psum(es, f"T1{i}", [128, 1024], BF) for i in range(2)]
        T1x = T1[1]
        FB = [psum(es, f"F{i}", [128, 512], F32) for i in range(5)]

        identb = sbuf(es, "identb", [128, 128], BF)
        zerob = sbuf(es, "zerob", [128, 512], BF)
        IKT = sbuf(es, "IKT", [64, L], BF)
        c_chan = k.chan("const")
        DMA("gpsimd", lambda e: e.dma_start(out=identb.t[:], in_=c_ident), c_chan, writes=[identb])
        cA_chan = k.chan("constA")
        G(lambda e: e.memset(zerob.t[:], 0.0), writes=[zerob])

        t1_i = [0]

        def next_t1():
            t1_i[0] += 1
            return T1[t1_i[0] % len(T1)]

        def load_w_bf(dst, src, nk, ncol, chan, colchunk=1024):
            for kc in range(nk):
                for c0 in range(0, ncol, colchunk):
                    c1 = min(ncol, c0 + colchunk)
                    DMA("gpsimd", lambda e, kc=kc, c0=c0, c1=c1: e.dma_start(
                        out=dst.t[:, kc, c0:c1], in_=src[kc * 128:(kc + 1) * 128, c0:c1]), chan, writes=[dst])

        def rmsnorm_bf(xb, gbc, hb, ss, std, rstd, junk):
            A(lambda e: e.activation(out=junk.t[:], in_=xb.t[:], func=AF.Square, accum_out=ss.t[:, 0:1]),
              reads=[xb], writes=[junk, ss])
            A(lambda e: e.activation(out=std.t[:], in_=ss.t[:], func=AF.Sqrt, scale=1.0 / D, bias=epsb.t[:, 0:1]),
              reads=[ss, epsb], writes=[std])
            V(lambda e: e.reciprocal(out=rstd.t[:], in_=std.t[:]), reads=[std], writes=[rstd])
            V(lambda e: e.scalar_tensor_tensor(out=hb.t[:], in0=xb.t[:], scalar=rstd.t[:, 0:1], in1=gbc.t[:],
                                               op0=OP.mult, op1=OP.mult), reads=[xb, rstd, gbc], writes=[hb])

        def transpose_to(src, dst, nblk, cols=128, parts=128):
            tp = next_t1()
            for j in range(nblk):
                T(lambda e, j=j: e.transpose(tp.t[0:cols, j * parts:(j + 1) * parts],
                                             src.t[0:parts, j * cols:(j + 1) * cols], identb.t[0:parts, 0:parts]),
                  reads=[src, identb], writes=[tp])
            return tp

        epsb = sbuf(es, "epsb", [128, 1], F32)
        V(lambda e: e.memset(epsb.t[:], EPS), writes=[epsb])

        with ExitStack() as pa:
            wsb = sbuf(pa, "wsb", [128, 8, NCOL], BF)
            wro = sbuf(pa, "wro", [128, 4, D], BF)
            gbc = sbuf(pa, "gbc", [128, D], F32)
            gnbc = sbuf(pa, "gnbc", [128, 512], F32)
            kdec = sbuf(pa, "kdec", [128, 512], F32)
            qdec = sbuf(pa, "qdec", [64, 512], F32)
            dmat = sbuf(pa, "dmat", [128, 512], F32)
            gdec = sbuf(pa, "gdec", [64, 512], F32)
            wchan = k.chan("wA")
            DMA("sync", lambda e: e.dma_start(out=gbc.t[:], in_=g_attn), cA_chan, writes=[gbc])
            DMA("sync", lambda e: e.dma_start(out=gnbc.t[:], in_=g_gn), cA_chan, writes=[gnbc])
            DMA("sync", lambda e: e.dma_start(out=kdec.t[:], in_=c_kdec), cA_chan, writes=[kdec])
            DMA("sync", lambda e: e.dma_start(out=qdec.t[:], in_=c_qdec), cA_chan, writes=[qdec])
            DMA("sync", lambda e: e.dma_start(out=dmat.t[:], in_=c_dmat), cA_chan, writes=[dmat])
            DMA("sync", lambda e: e.dma_start(out=gdec.t[:], in_=c_gdec), cA_chan, writes=[gdec])
            wsbA = Buf(wsb.t, k.res("wsbA"))
            wsbB = Buf(wsb.t, k.res("wsbB"))
            wchanB = k.chan("wA2")
            for (rngs, wres, wch) in (([(0, 1024), (2048, 3396)], wsbA, wchan), ([(1024, 2048), (3396, NCOL)], wsbB, wchanB)):
                for (ra, rb) in rngs:
                    for c0 in range(ra, rb, 1024):
                        c1 = min(rb, c0 + 1024)
                        for kc in range(8):
                            DMA("gpsimd", lambda e, kc=kc, c0=c0, c1=c1: e.dma_start(
                                out=wsb.t[:, kc, c0:c1], in_=w_in[kc * 128:(kc + 1) * 128, c0:c1]), wch, writes=[wres])
            load_w_bf(wro, w_ro, 4, D, wchanB)
            k.settle(cA_chan, [b.r for b in (gbc, gnbc, kdec, qdec, dmat, gdec)])
            k.settle(wchan, [wsbA.r])
            k.settle(wchanB, [wsbB.r, wro.r])

            tstage = [sbuf(pa, f"tstage{i}", [128, 2, D], BF) for i in range(2)]
            tch_in = [k.chan(f"tin{i}") for i in range(2)]
            tch_out = [k.chan(f"tout{i}") for i in range(2)]
            r_UB = DR("UB")
            r_VB = DR("VB")
            def conv_gen():
                ci = 0
                for (src, dst, rr) in ((peer_u, UB, r_UB), (peer_v, VB, r_VB)):
                    for c in range(64 if tabconv else 0):
                        st = tstage[ci % 2]
                        sv = src[c * 256:(c + 1) * 256, :].rearrange("(r p) c -> p r c", p=128)
                        dv = dst[c * 256:(c + 1) * 256, :].rearrange("(r p) c -> p r c", p=128)
                        DMA("gpsimd", lambda e, st=st, sv=sv: e.dma_start(out=st.t[:], in_=sv), tch_in[ci % 2], writes=[st])
                        DMA("sync", lambda e, st=st, dv=dv: e.dma_start(out=dv, in_=st.t[:]), tch_out[ci % 2], reads=[st])
                        ci += 1
                        yield 1

            xbuf = [sbuf(pa, f"xbuf{i}", [128, D], F32) for i in range(2)]
            xch = [k.chan(f"x{i}") for i in range(2)]
            rtb = [sbuf(pa, f"rtb{i}", [128, 80], F32) for i in range(2)]
            rch = [k.chan(f"rt{i}") for i in range(2)]
            junkA = sbuf(pa, "junkA", [128, D], BF)
            ss = sbuf(pa, "ss", [128, 1], F32)
            std = sbuf(pa, "std", [128, 1], F32)
            rstd = sbuf(pa, "rstd", [128, 1], F32)
            hb = sbuf(pa, "hb", [128, D], BF)
            hT = [sbuf(pa, f"hT{i}", [128, D], BF) for i in range(2)]
            rtmp = sbuf(pa, "rtmp", [128, 4, 256], F32)
            RQKs = [sbuf(pa, f"RQK{i}", [128, 512], BF) for i in range(2)]
            Vt = [sbuf(pa, f"Vt{i}", [128, 512], BF) for i in range(2)]
            Kd = [sbuf(pa, f"Kd{i}", [128, 256], BF) for i in range(2)]
            S32 = [sbuf(pa, f"S32_{i}", [64, 512], F32) for i in range(5)]
            Sbf = [sbuf(pa, f"Sbf_{i}", [64, 512], BF) for i in range(5)]
            Stmp = sbuf(pa, "Stmp", [64, 512], F32)
            KTs = sbuf(pa, "KTs", [64, 512], BF)
            QTs = sbuf(pa, "QTs", [64, 512], BF)
            QW = sbuf(pa, "QW", [64, 4, 192], BF)
            sTm = sbuf(pa, "sTm", [128, 512], BF)
            gst = sbuf(pa, "gst", [128, 32], F32)
            yc = sbuf(pa, "yc", [128, 512], F32)
            ysq = yc
            sw = sbuf(pa, "sw", [128, 512], F32)
            yret = sbuf(pa, "yret", [128, 512], BF)
            yT = sbuf(pa, "yT", [128, 512], BF)
            gr = sbuf(pa, "gr", [128, D], BF)
            mst = [sbuf(pa, "mst0", [128, D], BF)]
            gdst = [sbuf(pa, "gdst0", [128, D], BF)]
            DQ = sbuf(pa, "DQ", [128, 512], BF)
            DKb = sbuf(pa, "DKb", [128, 512], BF)
            qtst = [sbuf(pa, "qtst0", [64, 1024], BF)]
            ktst = [sbuf(pa, "ktst0", [64, 1024], BF)]
            vast = [sbuf(pa, f"vast{i}", [128, 8, 65], BF) for i in range(2)]
            IQI = sbuf(pa, "IQI", [128, 320], BF)
            iqst = [sbuf(pa, f"iqst{i}", [64, 512], BF) for i in range(2)]
            iwst = [sbuf(pa, f"iwst{i}", [128, 4], F32) for i in range(2)]
            sch = {n: [k.chan(f"{n}{i}") for i in range(2)] for n in ("m", "gd", "qt", "kt", "va", "iq", "iw")}
            r_scr = DR("scratchA")

            G(lambda e: e.memset(QW.t[:], 0.0), writes=[QW])
            for i in range(2):
                G(lambda e, i=i: e.memset(vast[i].t[:], 1.0), writes=[vast[i]])
            G(lambda e: e.memset(S32[0].t[:], 0.0), writes=[S32[0]])
            G(lambda e: e.memset(Sbf[0].t[:], 0.0), writes=[Sbf[0]])

            fb_i = [0]

            def next_fb():
                fb_i[0] += 1
                return FB[fb_i[0] % 3]

            def proj(hTt, c0, ncol):
                wres = wsbA if (c0 < 1024 or 2048 <= c0 < 3396) else wsbB
                bank = next_fb()
                for kc in range(8):
                    T(lambda e, kc=kc: e.matmul(bank.t[:, 0:ncol], lhsT=hTt.t[:, kc * 128:(kc + 1) * 128],
                                               rhs=wsb.t[:, kc, c0:c0 + ncol], start=(kc == 0), stop=(kc == 7)),
                      reads=[hTt, wres], writes=[bank])
                return bank

            def rope_tok(bank, c0, H, half, rt, cos0, dst, d0):
                src3 = bank.t[:, c0:c0 + H * 64].rearrange("p (h e) -> p h e", e=64)
                dst3 = dst.t[:, d0:d0 + H * 64].rearrange("p (h e) -> p h e", e=64)
                x1 = src3[:, :, 0:half]
                x2 = src3[:, :, half:2 * half]
                cb = rt.t[:, cos0:cos0 + half].unsqueeze(1).broadcast_to([128, H, half])
                sb_ = rt.t[:, cos0 + half:cos0 + 2 * half].unsqueeze(1).broadcast_to([128, H, half])
                tm = [rtmp.t[:, i, 0:H * half].rearrange("p (h e) -> p h e", e=half) for i in range(4)]
                V(lambda e: e.tensor_tensor(out=tm[0], in0=x1, in1=cb, op=OP.mult), reads=[bank, rt], writes=[rtmp])
                V(lambda e: e.tensor_tensor(out=tm[1], in0=x2, in1=sb_, op=OP.mult), reads=[bank, rt], writes=[rtmp])
                V(lambda e: e.tensor_tensor(out=tm[2], in0=x1, in1=sb_, op=OP.mult), reads=[bank, rt], writes=[rtmp])
                V(lambda e: e.tensor_tensor(out=tm[3], in0=x2, in1=cb, op=OP.mult), reads=[bank, rt], writes=[rtmp])
                V(lambda e: e.tensor_tensor(out=dst3[:, :, 0:half], in0=tm[0], in1=tm[1], op=OP.subtract),
                  reads=[rtmp], writes=[dst])
                V(lambda e: e.tensor_tensor(out=dst3[:, :, half:2 * half], in0=tm[2], in1=tm[3], op=OP.add),
                  reads=[rtmp], writes=[dst])
                if 2 * half < 64:
                    A(lambda e: e.activation(out=dst3[:, :, 2 * half:64], in_=src3[:, :, 2 * half:64], func=AF.Copy),
                      reads=[bank], writes=[dst])

            def tileA(t):
                own = t >= 32
                tl = t % 32
                par = t % 2
                xb = xbuf[par]
                rt = rtb[par]
                RQK = RQKs[par]
                src = xo if own else xp
                DMA("sync", lambda e, xb=xb, src=src, tl=tl: e.dma_start(out=xb.t[:], in_=src[tl * 128:(tl + 1) * 128, :]),
                    xch[par], writes=[xb])
                DMA("sync", lambda e, rt=rt, t=t: e.dma_start(out=rt.t[:], in_=ropetab[t * 128:(t + 1) * 128, :]),
                    rch[par], writes=[rt])
                rmsnorm_bf(xb, gbc, hb, ss, std, rstd, junkA)
                for kc in range(8):
                    T(lambda e, kc=kc: e.transpose(T0.t[:, kc * 128:(kc + 1) * 128], hb.t[:, kc * 128:(kc + 1) * 128],
                                                   identb.t[:]), reads=[hb, identb], writes=[T0])
                hTt = hT[par]
                A(lambda e, hTt=hTt: e.activation(out=hTt.t[:], in_=T0.t[:], func=AF.Copy), reads=[T0], writes=[hTt])
                yield 1

                b0 = proj(hTt, 0, 512)
                rope_tok(b0, 0, 8, 32, rt, 0, RQK, 0)
                yield 1
                b1 = proj(hTt, 512, 512)
                vt = Vt[par]
                A(lambda e, b1=b1, vt=vt: e.activation(out=vt.t[:], in_=b1.t[:], func=AF.Copy), reads=[b1], writes=[vt])
                yield 1
                ia, ib, inx = (2 * t) % 5, (2 * t + 1) % 5, (2 * t + 2) % 5
                for c in range(2):
                    V(lambda e, c=c: e.tensor_tensor(out=Kd[c].t[:], in0=RQK.t[:, 256:512], in1=kdec.t[:, c * 256:(c + 1) * 256],
                                                     op=OP.mult), reads=[RQK, kdec], writes=[Kd[c]])
                for c, (si, so) in enumerate(((ia, ib), (ib, inx))):
                    kvb = FB[3]
                    for h in range(4):
                        T(lambda e, c=c, h=h: e.matmul(kvb.t[0:64, h * 128:(h + 1) * 128], lhsT=Kd[c].t[:, h * 64:(h + 1) * 64],
                                                       rhs=vt.t[:, h * 128:(h + 1) * 128], start=True, stop=True),
                          reads=[Kd[c], vt], writes=[kvb])
                    V(lambda e, si=si: e.tensor_tensor(out=Stmp.t[:], in0=S32[si].t[:], in1=gdec.t[:], op=OP.mult),
                      reads=[S32[si], gdec], writes=[Stmp])
                    V(lambda e, so=so: e.tensor_tensor(out=S32[so].t[:], in0=Stmp.t[:], in1=kvb.t[0:64, :], op=OP.add),
                      reads=[Stmp, kvb], writes=[S32[so]])
                    A(lambda e, so=so: e.activation(out=Sbf[so].t[:], in_=S32[so].t[:], func=AF.Copy),
                      reads=[S32[so]], writes=[Sbf[so]])
                yield 'half'

                if own:
                    tp = transpose_to(RQK, None, 8, cols=64)
                    A(lambda e, tp=tp: e.activation(out=KTs.t[:], in_=tp.t[0:64, 512:1024], func=AF.Copy), reads=[tp], writes=[KTs])
                    A(lambda e, tp=tp: e.activation(out=QTs.t[:], in_=tp.t[0:64, 0:512], func=AF.Copy), reads=[tp], writes=[QTs])
                    tq = tp.t[0:64, 0:512].rearrange("p (h e) -> p h e", e=128)
                    qd3 = qdec.t[:, :].rearrange("p (h e) -> p h e", e=128)
                    V(lambda e: e.tensor_tensor(out=QW.t[:, :, 0:64], in0=tq[:, :, 0:64], in1=qd3[:, :, 0:64], op=OP.mult),
                      reads=[tp, qdec], writes=[QW])
                    V(lambda e: e.tensor_tensor(out=QW.t[:, :, 128:192], in0=tq[:, :, 64:128], in1=qd3[:, :, 64:128], op=OP.mult),
                      reads=[tp, qdec], writes=[QW])
                    sTb = FB[3]
                    for h in range(4):
                        T(lambda e, h=h: e.matmul(sTb.t[:, h * 128:(h + 1) * 128], lhsT=KTs.t[:, h * 128:(h + 1) * 128],
                                                  rhs=QTs.t[:, h * 128:(h + 1) * 128], start=True, stop=True),
                          reads=[KTs, QTs], writes=[sTb])
                    V(lambda e: e.tensor_tensor(out=sTm.t[:], in0=sTb.t[:], in1=dmat.t[:], op=OP.mult),
                      reads=[sTb, dmat], writes=[sTm])
                    yb = FB[4]
                    for h in range(4):
                        hs = slice(h * 128, (h + 1) * 128)
                        T(lambda e, hs=hs: e.matmul(yb.t[:, hs], lhsT=sTm.t[:, hs], rhs=vt.t[:, hs], start=True, stop=False),
                          reads=[sTm, vt], writes=[yb])
                        T(lambda e, hs=hs, h=h: e.matmul(yb.t[:, hs], lhsT=QW.t[:, h, 0:128], rhs=Sbf[ia].t[:, hs], start=False, stop=False),
                          reads=[QW, Sbf[ia]], writes=[yb])
                        T(lambda e, hs=hs, h=h: e.matmul(yb.t[:, hs], lhsT=QW.t[:, h, 64:192], rhs=Sbf[ib].t[:, hs], start=False, stop=True),
                          reads=[QW, Sbf[ib]], writes=[yb])
                    yield 1
                    y3 = yb.t[:, :].rearrange("p (h e) -> p h e", e=128)
                    V(lambda e: e.tensor_reduce(out=gst.t[:, 0:4], in_=y3, axis=AX.X, op=OP.add), reads=[yb], writes=[gst])
                    A(lambda e: e.activation(out=ysq.t[:], in_=yb.t[:], func=AF.Square), reads=[yb], writes=[ysq])
                    V(lambda e: e.tensor_reduce(out=gst.t[:, 4:8], in_=ysq.t[:, :].rearrange("p (h e) -> p h e", e=128),
                                                axis=AX.X, op=OP.add), reads=[ysq], writes=[gst])
                    V(lambda e: e.tensor_scalar(out=gst.t[:, 8:12], in0=gst.t[:, 0:4], scalar1=1.0 / 128, scalar2=None, op0=OP.mult),
                      reads=[gst], writes=[gst])
                    V(lambda e: e.tensor_tensor(out=gst.t[:, 12:16], in0=gst.t[:, 8:12], in1=gst.t[:, 8:12], op=OP.mult),
                      reads=[gst], writes=[gst])
                    V(lambda e: e.scalar_tensor_tensor(out=gst.t[:, 16:20], in0=gst.t[:, 4:8], scalar=1.0 / 128, in1=gst.t[:, 12:16],
                                                       op0=OP.mult, op1=OP.subtract), reads=[gst], writes=[gst])
                    A(lambda e: e.activation(out=gst.t[:, 20:24], in_=gst.t[:, 16:20], func=AF.Sqrt, bias=epsb.t[:, 0:1]),
                      reads=[gst, epsb], writes=[gst])
                    V(lambda e: e.reciprocal(out=gst.t[:, 24:28], in_=gst.t[:, 20:24]), reads=[gst], writes=[gst])
                    yc3 = yc.t[:, :].rearrange("p (h e) -> p h e", e=128)
                    V(lambda e: e.tensor_tensor(out=yc3, in0=y3, in1=gst.t[:, 8:12].unsqueeze(2).broadcast_to([128, 4, 128]),
                                                op=OP.subtract), reads=[yb, gst], writes=[yc])
                    V(lambda e: e.tensor_tensor(out=yc3, in0=yc3, in1=gst.t[:, 24:28].unsqueeze(2).broadcast_to([128, 4, 128]),
                                                op=OP.mult), reads=[yc, gst], writes=[yc])
                    V(lambda e: e.tensor_tensor(out=yc.t[:], in0=yc.t[:], in1=gnbc.t[:], op=OP.mult), reads=[yc, gnbc], writes=[yc])
                    yield 1
                    b2 = proj(hTt, 1024, 512)
                    A(lambda e, b2=b2: e.activation(out=sw.t[:], in_=b2.t[:], func=AF.Silu), reads=[b2], writes=[sw])
                    V(lambda e: e.tensor_tensor(out=yret.t[:], in0=yc.t[:], in1=sw.t[:], op=OP.mult), reads=[yc, sw], writes=[yret])
                    tp = transpose_to(yret, None, 4)
                    A(lambda e, tp=tp: e.activation(out=yT.t[:], in_=tp.t[:, 0:512], func=AF.Copy), reads=[tp], writes=[yT])
                    ms = mst[0]
                    for nb in range(2):
                        bg = proj(hTt, 3456 + nb * 512, 512)
                        A(lambda e, bg=bg, nb=nb: e.activation(out=gr.t[:, nb * 512:(nb + 1) * 512], in_=bg.t[:], func=AF.Sigmoid),
                          reads=[bg], writes=[gr])
                        bm = next_fb()
                        for kc in range(4):
                            T(lambda e, kc=kc, nb=nb, bm=bm: e.matmul(bm.t[:, :], lhsT=yT.t[:, kc * 128:(kc + 1) * 128],
                                                                      rhs=wro.t[:, kc, nb * 512:(nb + 1) * 512],
                                                                      start=(kc == 0), stop=(kc == 3)), reads=[yT, wro], writes=[bm])
                        V(lambda e, nb=nb, bm=bm, ms=ms: e.tensor_tensor(out=ms.t[:, nb * 512:(nb + 1) * 512], in0=bm.t[:],
                                                                         in1=gr.t[:, nb * 512:(nb + 1) * 512], op=OP.mult),
                          reads=[bm, gr], writes=[ms])
                    DMA("sync", lambda e, ms=ms, tl=tl: e.dma_start(out=MRET[tl * 128:(tl + 1) * 128, :], in_=ms.t[:]),
                        sch["m"][0], reads=[ms])
                    yield 1
                    gd = gdst[0]
                    for nb in range(2):
                        bg = proj(hTt, 4480 + nb * 512, 512)
                        A(lambda e, bg=bg, nb=nb, gd=gd: e.activation(out=gd.t[:, nb * 512:(nb + 1) * 512], in_=bg.t[:], func=AF.Sigmoid),
                          reads=[bg], writes=[gd])
                    DMA("sync", lambda e, gd=gd, tl=tl: e.dma_start(out=GDSA[tl * 128:(tl + 1) * 128, :], in_=gd.t[:]),
                        sch["gd"][0], reads=[gd])
                    yield 1
                    b3 = proj(hTt, 1536, 512)
                    rope_tok(b3, 0, 8, 8, rt, 64, DQ, 0)
                    tp = transpose_to(DQ, None, 8, cols=64)
                    qs = qtst[0]
                    A(lambda e, tp=tp, qs=qs: e.activation(out=qs.t[:], in_=tp.t[0:64, :], func=AF.Copy), reads=[tp], writes=[qs])
                    DMA("sync", lambda e, qs=qs, tl=tl: e.dma_start(out=QTD[tl], in_=qs.t[:]), sch["qt"][0], reads=[qs])
                    yield 1

                b4 = proj(hTt, 2048, 512)
                rope_tok(b4, 0, 8, 8, rt, 64, DKb, 0)
                tp = transpose_to(DKb, None, 8, cols=64)
                ks = ktst[0]
                A(lambda e, tp=tp, ks=ks: e.activation(out=ks.t[:], in_=tp.t[0:64, :], func=AF.Copy), reads=[tp], writes=[ks])
                DMA("sync", lambda e, ks=ks, t=t: e.dma_start(out=KTD[:, :, t * 128:(t + 1) * 128],
                                                             in_=ks.t[:, :].rearrange("p (h e) -> p h e", e=128)),
                    sch["kt"][0], reads=[ks])
                yield 1
                b5 = proj(hTt, 2560, 512)
                va = vast[par]
                A(lambda e, b5=b5, va=va: e.activation(out=va.t[:, :, 0:64], in_=b5.t[:, :].rearrange("p (h e) -> p h e", e=64),
                                                       func=AF.Copy), reads=[b5], writes=[va])
                DMA("sync", lambda e, va=va, t=t: e.dma_start(out=VD[t * 128:(t + 1) * 128, :],
                                                             in_=va.t[:, :, :].rearrange("p h e -> p (h e)")),
                    sch["va"][par], reads=[va])
                yield 1
                b6 = proj(hTt, 3072, 324)
                rope_tok(b6, 0, 5, 8, rt, 64, IQI, 0)
                tp = transpose_to(IQI, None, 5, cols=64)
                A(lambda e, tp=tp, t=t: e.activation(out=IKT.t[:, t * 128:(t + 1) * 128], in_=tp.t[0:64, 512:640], func=AF.Copy),
                  reads=[tp], writes=[IKT])
                if own:
                    iqs = iqst[par]
                    A(lambda e, tp=tp, iqs=iqs: e.activation(out=iqs.t[:], in_=tp.t[0:64, 0:512], func=AF.Copy), reads=[tp], writes=[iqs])
                    DMA("sync", lambda e, iqs=iqs, tl=tl: e.dma_start(out=IQTD[tl], in_=iqs.t[:]), sch["iq"][par], reads=[iqs])
                    iws = iwst[par]
                    V(lambda e, b6=b6, iws=iws: e.tensor_scalar(out=iws.t[:], in0=b6.t[:, 320:324], scalar1=1.0 / 16, scalar2=None, op0=OP.mult),
                      reads=[b6], writes=[iws])
                    DMA("sync", lambda e, iws=iws, tl=tl: e.dma_start(out=IWD[tl * 128:(tl + 1) * 128, :], in_=iws.t[:]),
                        sch["iw"][par], reads=[iws])
            prevA = None
            cgen = conv_gen()
            for ti_, t in enumerate(tiles_a if tiles_a is not None else range(64)):
                if ti_ >= 12:
                    for _ in range(3):
                        next(cgen, None)
                cur = tileA(t)
                while True:
                    r = next(cur)
                    if prevA is not None and next(prevA, "end") == "end":
                        prevA = None
                    if r == "half":
                        break
                if prevA is not None:
                    for _ in prevA:
                        pass
                prevA = cur
            for _ in prevA:
                pass
            for _ in cgen:
                pass
            k.barrier()
            k.flush()

        with ExitStack() as pb:
            if only_a:
                return nc
            wdo = sbuf(pb, "wdo", [128, 4, D], BF)
            wout = sbuf(pb, "wout", [128, 8, D], BF)
            wq = sbuf(pb, "wq", [128, 8, 2048], BF)
            skT = sbuf(pb, "skT", [128, 16, 128], BF)
            g2bc = sbuf(pb, "g2bc", [128, D], F32)
            gfbc = sbuf(pb, "gfbc", [128, D], F32)
            diag = sbuf(pb, "diag", [128, 2048], BF)
            pbias = sbuf(pb, "pbias", [128, 1], F32)
            iota = sbuf(pb, "iota", [128, 16], F32)
            cpow = sbuf(pb, "cpow", [128, NIT], F32)
            cnb = sbuf(pb, "cnb", [128, NT], F32)
            c05 = sbuf(pb, "c05", [128, 1], F32)
            nbig = sbuf(pb, "nbig", [128, 1], F32)
            dummy = sbuf(pb, "fdummy", [128, 8], F32)
            POOLW = 10240
            pool_t = pb.enter_context(nc.sbuf_tensor("pool", [128, POOLW], F32))

            def view(name, w0, w1, dt=None, shape=None):
                ap = pool_t[:, w0:w1]
                if dt is not None:
                    ap = ap.bitcast(dt)
                return Buf(ap, k.res(name))

            isc = view("isc", 0, 8192)
            junkB = view("junkB", 8192, 10240, mybir.dt.int8)
            skr = view("skr", 0, 1024, BF)
            Sc = view("Sc", 0, 2048)
            cand = view("cand", 2048, 4096)
            qT = view("qT", 4096, 5120, BF)
            oh = view("oh", 5120, 6144, BF)
            tmp2 = view("tmp2", 6144, 6656)
            Sc2 = view("Sc2", 8192, 10240)
            pf = view("pf", 6656, 7424)
            tif = view("tif", 7424, 7680)
            isel = view("isel", 7680, 7936)
            ef = view("ef", 7936, 8064)
            e0 = view("e0", 8064, 8192)
            ubuf = [sbuf(pb, f"gbuf{i}", [128, D], BF) for i in range(2 * NU)]
            vbuf = ubuf
            dsa_views = [isc, junkB]
            peer_views = [Sc, cand, qT, oh, tmp2, pf, tif, isel, ef, e0, Sc2]

            def fence(frm, to):
                V(lambda e: e.memset(dummy.t[:, 0:1], 0.0), writes=list(frm) + list(to) + [dummy])

            wchB = k.chan("wB")
            cchB = k.chan("cB")
            cl = ((g2bc, g_ffn), (gfbc, g_fin), (pbias, c_pbias), (iota, c_iota), (cpow, c_pow), (cnb, c_nb))
            for dst, srcw in cl:
                DMA("sync", lambda e, dst=dst, srcw=srcw: e.dma_start(out=dst.t[:], in_=srcw), cchB, writes=[dst])
            k.settle(cchB, [d.r for d, _ in cl])
            DMA("gpsimd", lambda e: e.dma_start(out=diag.t[:], in_=c_diag), wchB, writes=[diag])
            load_w_bf(wdo, w_do, 4, D, wchB)
            load_w_bf(wout, w_out, 8, D, wchB)
            load_w_bf(wq, w_q, 8, 2048, wchB)
            skr3 = skr.t.rearrange("p (c d) -> p c d", d=128)
            DMA("gpsimd", lambda e: e.dma_start(out=skr3, in_=subk.rearrange("c k d -> k c d")), wchB, writes=[skr])
            k.settle(wchB, [wdo.r, wout.r, wq.r, skr.r, diag.r])
            V(lambda e: e.memset(c05.t[:], 0.5), writes=[c05])
            V(lambda e: e.memset(nbig.t[:], -30000.0), writes=[nbig])
            for q4 in range(4):
                tp = next_t1()
                for j in range(4):
                    cb = q4 * 4 + j
                    T(lambda e, cb=cb, j=j, tp=tp: e.transpose(tp.t[:, j * 128:(j + 1) * 128], skr3[:, cb, :], identb.t[:]),
                      reads=[skr, identb], writes=[tp])
                A(lambda e, q4=q4, tp=tp: e.activation(out=skT.t[:, q4 * 4:(q4 + 1) * 4, :].rearrange("p c k -> p (c k)"),
                                                       in_=tp.t[:, 0:512], func=AF.Copy), reads=[tp], writes=[skT])
            fence([skr], dsa_views + peer_views)

            x2s = [sbuf(pb, f"x2_{i}", [128, D], F32) for i in range(2)]
            qt_ = sbuf(pb, "qtk", [64, 1024], BF)
            iq_ = sbuf(pb, "iqk", [64, 512], BF)
            iw_ = sbuf(pb, "iwk", [128, 4], F32)
            mr_ = sbuf(pb, "mrk", [128, D], BF)
            gd_ = sbuf(pb, "gdk", [128, D], BF)
            lch = {n: k.chan(f"L{n}") for n in ("qt", "iq", "iw", "mr", "gd")}
            xlch = [k.chan(f"Lx{i}", serial=True) for i in range(2)]
            rel = [sbuf(pb, f"rel{i}", [128, 512], F32) for i in range(2)]
            bst = sbuf(pb, "bst", [128, 16], F32)
            nhd = sbuf(pb, "nhd", [128, NIT], F32)
            sS = sbuf(pb, "sS", [128, 2], F32)
            ind = sbuf(pb, "ind", [128, 2], F32)
            nmid = [sbuf(pb, f"nmid{i}", [128, 1], F32) for i in range(2)]
            lo = sbuf(pb, "lo", [128, 1], F32)
            KTg = [sbuf(pb, f"KTg{i}", [64, 8, 256], BF) for i in range(2)]
            Vg = [sbuf(pb, f"Vg{i}", [128, 2, 520], BF) for i in range(2)]
            kch = [k.chan(f"ktg{i}") for i in range(2)]
            vch = [k.chan(f"vg{i}") for i in range(2)]
            mk = [sbuf(pb, f"mk{i}", [128, 256], BF) for i in range(4)]
            mT = [sbuf(pb, f"mT{i}", [128, 256], BF) for i in range(4)]
            Eb = [sbuf(pb, f"Eb{i}", [128, 512], BF) for i in range(2)]
            rden = sbuf(pb, "rden", [128, 8], F32)
            yd = sbuf(pb, "yd", [128, 512], BF)
            xT = sbuf(pb, "xT", [128, D], BF)
            mtmp = sbuf(pb, "mtmp", [128, 512], F32)
            mg = sbuf(pb, "mg", [128, D], BF)
            junkX = Buf(mtmp.t[:, :].bitcast(BF), mtmp.r)
            h2s = [sbuf(pb, f"h2_{i}", [128, D], BF) for i in range(2)]
            ss3 = sbuf(pb, "ss3", [128, 1], F32)
            std3 = sbuf(pb, "std3", [128, 1], F32)
            rstd3 = sbuf(pb, "rstd3", [128, 1], F32)
            ss2 = sbuf(pb, "ss2", [128, 1], F32)
            std2 = sbuf(pb, "std2", [128, 1], F32)
            rstd2 = sbuf(pb, "rstd2", [128, 1], F32)
            tv = sbuf(pb, "tv", [128, 16, 16], F32)
            ti = sbuf(pb, "ti", [128, 16, 16], U32)
            bv = sbuf(pb, "bv", [128, 8, 16], F32)
            bi = sbuf(pb, "bi", [128, 8, 16], U32)
            pi_ = sbuf(pb, "pi", [128, 128], I32)
            eidxs = [sbuf(pb, f"eidx{i}", [128, 128], I32) for i in range(2)]
            gs = sbuf(pb, "gs", [128, 16], F32)
            gsms = [sbuf(pb, f"gsm{i}", [128, 128], F32) for i in range(2)]
            av = sbuf(pb, "av", [128, 128], F32)
            wgt = sbuf(pb, "wgt", [128, 128], F32)
            junkD = sbuf(pb, "junkD", [128, D], BF)
            uch = [k.chan(f"u{i}", serial=True) for i in range(2 * NU)]
            vch2 = uch
            dg = [sbuf(pb, f"dg{i}", [128, 128], BF) for i in range(4)]
            ochs = [k.chan(f"o{i}", serial=True) for i in range(2)]
            t1_i[0] = 0
            T1.pop()
            Fv = [FB[4], Buf(T1x.t[:, :].bitcast(F32), T1x.r)]
            if debug:
                dbgch = k.chan("dbg")
                dbt = sbuf(pb, "dbt", [128, D], F32)

            Sc3 = Sc.t.rearrange("p (c k) -> p c k", k=128)
            cand3 = cand.t.rearrange("p (h k) -> p h k", k=256)
            qT3 = qT.t.rearrange("p (c k) -> p c k", k=128)
            tmp23 = tmp2.t.rearrange("p (i k) -> p i k", k=256)
            pf3 = pf.t.rearrange("p (i k) -> p i k", k=128)
            isel3 = isel.t.rearrange("p (c k) -> p c k", k=128)

            def stage_X(kt):
                x2 = x2s[kt % 2]; h2 = h2s[kt % 2]; eidx = eidxs[kt % 2]; gsm = gsms[kt % 2]
                G_k = 9 + kt // 4
                N_k = 512 * G_k
                NB_k = 33 + kt
                r = kt % 4
                rows = slice(kt * 128, (kt + 1) * 128)
                fence(peer_views, dsa_views)
                DMA("sync", lambda e, rows=rows: e.dma_start(out=x2.t[:], in_=xo[rows, :]), xlch[kt % 2], writes=[x2])
                DMA("sync", lambda e, kt=kt: e.dma_start(out=qt_.t[:], in_=QTD[kt]), lch["qt"], writes=[qt_])
                DMA("sync", lambda e, kt=kt: e.dma_start(out=iq_.t[:], in_=IQTD[kt]), lch["iq"], writes=[iq_])
                DMA("sync", lambda e, rows=rows: e.dma_start(out=iw_.t[:], in_=IWD[rows, :]), lch["iw"], writes=[iw_])
                DMA("sync", lambda e, rows=rows: e.dma_start(out=mr_.t[:], in_=MRET[rows, :]), lch["mr"], writes=[mr_])
                DMA("sync", lambda e, rows=rows: e.dma_start(out=gd_.t[:], in_=GDSA[rows, :]), lch["gd"], writes=[gd_])

                ri = 0
                for g in range(G_k):
                    gs_ = slice(g * 512, (g + 1) * 512)
                    for h in range(4):
                        bank = FB[(g * 4 + h) % 2]
                        T(lambda e, bank=bank, h=h, gs_=gs_: e.matmul(bank.t[:, :], lhsT=iq_.t[:, h * 128:(h + 1) * 128],
                                                                      rhs=IKT.t[:, gs_], start=True, stop=True),
                          reads=[iq_, IKT], writes=[bank])
                        rl = rel[ri % 2]
                        ri += 1
                        A(lambda e, bank=bank, rl=rl: e.activation(out=rl.t[:], in_=bank.t[:], func=AF.Relu), reads=[bank], writes=[rl])
                        if h == 0:
                            V(lambda e, rl=rl, gs_=gs_: e.tensor_scalar(out=isc.t[:, gs_], in0=rl.t[:], scalar1=iw_.t[:, 0:1],
                                                                        scalar2=None, op0=OP.mult), reads=[rl, iw_], writes=[isc])
                        else:
                            V(lambda e, rl=rl, gs_=gs_, h=h: e.scalar_tensor_tensor(
                                out=isc.t[:, gs_], in0=rl.t[:], scalar=iw_.t[:, h:h + 1], in1=isc.t[:, gs_], op0=OP.mult, op1=OP.add),
                              reads=[rl, iw_, isc], writes=[isc])
                    yield 1.6
                V(lambda e, N_k=N_k: e.tensor_reduce(out=bst.t[:, 0:1], in_=isc.t[:, 0:N_k], axis=AX.X, op=OP.max,
                                                     apply_absolute_value=True), reads=[isc], writes=[bst])
                V(lambda e: e.tensor_scalar(out=bst.t[:, 1:2], in0=bst.t[:, 0:1], scalar1=2.0, scalar2=1.0, op0=OP.mult, op1=OP.add),
                  reads=[bst], writes=[bst])
                V(lambda e: e.tensor_scalar(out=nhd.t[:], in0=cpow.t[:], scalar1=bst.t[:, 1:2], scalar2=None, op0=OP.mult),
                  reads=[cpow, bst], writes=[nhd])
                V(lambda e: e.tensor_scalar(out=isc.t[:, 0:LH], in0=isc.t[:, 0:LH], scalar1=pbias.t[:, 0:1], scalar2=None, op0=OP.add),
                  reads=[isc, pbias], writes=[isc])
                V(lambda e, N_k=N_k, r=r: e.tensor_tensor(out=isc.t[:, N_k - 512:N_k], in0=isc.t[:, N_k - 512:N_k],
                                                          in1=diag.t[:, r * 512:(r + 1) * 512], op=OP.add), reads=[isc, diag], writes=[isc])
                cur = c05
                for it in range(NIT):
                    A(lambda e, N_k=N_k, cur=cur: e.activation(out=junkB.t[:, 0:N_k], in_=isc.t[:, 0:N_k], func=AF.Sign,
                                                               bias=cur.t[:, 0:1], accum_out=sS.t[:, 0:1]),
                      reads=[isc, cur], writes=[junkB, sS])
                    A(lambda e, kt=kt: e.activation(out=ind.t[:, 0:1], in_=sS.t[:, 0:1], func=AF.Sign, bias=cnb.t[:, kt:kt + 1]),
                      reads=[sS, cnb], writes=[ind])
                    nxt = nmid[it % 2]
                    A(lambda e, it=it, cur=cur, nxt=nxt: e.activation(out=nxt.t[:], in_=ind.t[:, 0:1], func=AF.Identity,
                                                                      scale=nhd.t[:, it:it + 1], bias=cur.t[:, 0:1]),
                      reads=[ind, nhd, cur], writes=[nxt])
                    cur = nxt
                    yield 6.9
                A(lambda e, cur=cur: e.activation(out=lo.t[:], in_=cur.t[:], func=AF.Identity, scale=-1.0, bias=nhd.t[:, NIT - 1:NIT]),
                  reads=[cur, nhd], writes=[lo])
                if debug:
                    V(lambda e: e.tensor_copy(out=dbt.t[:, 0:1], in_=lo.t[:]), reads=[lo], writes=[dbt])
                    V(lambda e: e.tensor_copy(out=dbt.t[:, 1:3], in_=bst.t[:, 0:2]), reads=[bst], writes=[dbt])
                    V(lambda e: e.tensor_copy(out=dbt.t[:, 3:4], in_=sS.t[:, 0:1]), reads=[sS], writes=[dbt])
                    DMA("sync", lambda e, rows=rows: e.dma_start(out=DBG_LO[rows, :], in_=dbt.t[:, 0:4]), dbgch, reads=[dbt])
                    k.wait_all("vector", chans=[dbgch], engines=False)

                Fo = [FB[2], FB[3]]
                for hg in range(2):
                    T(lambda e, hg=hg: e.matmul(Fo[hg].t[:, 0:260], lhsT=zerob.t[:, 0:128], rhs=zerob.t[:, 0:260], start=True, stop=False),
                      reads=[zerob], writes=[Fo[hg]])
                ngrp = (NB_k + 1) // 2
                grp = {}

                gmask = {}

                def pre(g):
                    gp = g % 2
                    ktg, vg = KTg[gp], Vg[gp]
                    gs_ = slice(g * 256, (g + 1) * 256)
                    DMA("sync", lambda e: e.dma_start(out=ktg.t[:], in_=KTD[:, :, gs_]), kch[gp], writes=[ktg])
                    DMA("sync", lambda e: e.dma_start(out=vg.t[:], in_=VD[gs_, :].rearrange("(b p) c -> p b c", p=128)),
                        vch[gp], writes=[vg])
                    grp[g] = (ktg, vg)

                def pre_mask(g):
                    gs_ = slice(g * 256, (g + 1) * 256)
                    mkb = mk[g % 4]
                    V(lambda e: e.tensor_scalar(out=mkb.t[:], in0=isc.t[:, gs_], scalar1=lo.t[:, 0:1], scalar2=None, op0=OP.is_gt),
                      reads=[isc, lo], writes=[mkb])
                    nb_here = min(2, NB_k - g * 2)
                    for b in range(nb_here):
                        T(lambda e, b=b: e.transpose(T0.t[:, b * 128:(b + 1) * 128], mkb.t[:, b * 128:(b + 1) * 128], identb.t[:]),
                          reads=[mkb, identb], writes=[T0])
                    mTb = mT[g % 4]
                    A(lambda e: e.activation(out=mTb.t[:, 0:nb_here * 128], in_=T0.t[:, 0:nb_here * 128], func=AF.Identity,
                                             scale=30000.0, bias=nbig.t[:, 0:1]), reads=[T0, nbig], writes=[mTb])
                    gmask[g] = mTb

                units = [(g, b, hg) for g in range(ngrp) for b in range(min(2, NB_k - g * 2)) for hg in range(2)]

                def qk(ui):
                    g, b, hg = units[ui]
                    ktg, vg = grp[g]
                    mTb = gmask[g]
                    Fs = FB[ui % 2]
                    for h4 in range(4):
                        T(lambda e, h4=h4: e.matmul(Fs.t[:, h4 * 128:(h4 + 1) * 128], lhsT=identb.t[:], rhs=mTb.t[:, b * 128:(b + 1) * 128],
                                                    start=(h4 == 0), stop=False), reads=[identb, mTb], writes=[Fs])
                    for h4 in range(4):
                        hh = hg * 4 + h4
                        T(lambda e, h4=h4, hh=hh: e.matmul(
                            Fs.t[:, h4 * 128:(h4 + 1) * 128], lhsT=ktg.t[:, hh, b * 128:(b + 1) * 128],
                            rhs=qt_.t[:, hh * 128:(hh + 1) * 128], start=False, stop=(h4 == 3)), reads=[ktg, qt_], writes=[Fs])

                def smpv(ui):
                    g, b, hg = units[ui]
                    ktg, vg = grp[g]
                    Fs = FB[ui % 2]
                    eb = Eb[ui % 2]
                    last_blk = (g * 2 + b == NB_k - 1)
                    A(lambda e: e.activation(out=eb.t[:], in_=Fs.t[:], func=AF.Exp, scale=0.125), reads=[Fs], writes=[eb])
                    for h4 in range(4):
                        hh = hg * 4 + h4
                        T(lambda e, h4=h4, hh=hh: e.matmul(
                            Fo[hg].t[:, h4 * 65:(h4 + 1) * 65], lhsT=eb.t[:, h4 * 128:(h4 + 1) * 128],
                            rhs=vg.t[:, b, hh * 65:(hh + 1) * 65], start=False, stop=(last_blk and h4 == 3)),
                          reads=[eb, vg], writes=[Fo[hg]])

                pre(0)
                if ngrp > 1:
                    pre(1)
                for g_ in range(min(3, ngrp)):
                    pre_mask(g_)
                qk(0)
                for ui in range(len(units)):
                    g, b, hg = units[ui]
                    if ui + 1 < len(units):
                        qk(ui + 1)
                    smpv(ui)
                    if ui + 1 == len(units) or units[ui + 1][0] != g:
                        if g + 2 < ngrp:
                            pre(g + 2)
                        if g + 3 < ngrp:
                            pre_mask(g + 3)
                    yield 2.5
                for hg in range(2):
                    fo3 = Fo[hg].t[:, 0:260].rearrange("p (h e) -> p h e", e=65)
                    V(lambda e, hg=hg, fo3=fo3: e.reciprocal(out=rden.t[:, hg * 4:(hg + 1) * 4].unsqueeze(2), in_=fo3[:, :, 64:65]),
                      reads=[Fo[hg]], writes=[rden])
                    V(lambda e, hg=hg, fo3=fo3: e.tensor_tensor(
                        out=yd.t[:, hg * 256:(hg + 1) * 256].rearrange("p (h e) -> p h e", e=64), in0=fo3[:, :, 0:64],
                        in1=rden.t[:, hg * 4:(hg + 1) * 4].unsqueeze(2).broadcast_to([128, 4, 64]), op=OP.mult),
                      reads=[Fo[hg], rden], writes=[yd])
                if debug:
                    V(lambda e: e.tensor_copy(out=dbt.t[:, 0:512], in_=yd.t[:]), reads=[yd], writes=[dbt])
                    DMA("sync", lambda e, rows=rows: e.dma_start(out=DBG_YD[rows, :], in_=dbt.t[:, 0:512]), dbgch, reads=[dbt])
                    k.wait_all("vector", chans=[dbgch], engines=False)
                fence(dsa_views, peer_views)
                yield 3.0
                tp = transpose_to(yd, None, 4)
                A(lambda e, tp=tp: e.activation(out=xT.t[:, 0:512], in_=tp.t[:, 0:512], func=AF.Copy), reads=[tp], writes=[xT])
                for nb in range(2):
                    ns = slice(nb * 512, (nb + 1) * 512)
                    bm = FB[nb]
                    for kc in range(4):
                        T(lambda e, kc=kc, ns=ns, bm=bm: e.matmul(bm.t[:, :], lhsT=xT.t[:, kc * 128:(kc + 1) * 128], rhs=wdo.t[:, kc, ns],
                                                                  start=(kc == 0), stop=(kc == 3)), reads=[xT, wdo], writes=[bm])
                    V(lambda e, ns=ns, bm=bm: e.tensor_tensor(out=mtmp.t[:], in0=bm.t[:], in1=gd_.t[:, ns], op=OP.mult),
                      reads=[bm, gd_], writes=[mtmp])
                    V(lambda e, ns=ns: e.tensor_tensor(out=mg.t[:, ns], in0=mtmp.t[:], in1=mr_.t[:, ns], op=OP.add),
                      reads=[mtmp, mr_], writes=[mg])
                tp = transpose_to(mg, None, 8)
                A(lambda e, tp=tp: e.activation(out=xT.t[:], in_=tp.t[:], func=AF.Copy), reads=[tp], writes=[xT])
                for nb in range(2):
                    ns = slice(nb * 512, (nb + 1) * 512)
                    bm = FB[nb]
                    for kc in range(8):
                        T(lambda e, kc=kc, ns=ns, bm=bm: e.matmul(bm.t[:, :], lhsT=xT.t[:, kc * 128:(kc + 1) * 128], rhs=wout.t[:, kc, ns],
                                                                  start=(kc == 0), stop=(kc == 7)), reads=[xT, wout], writes=[bm])
                    V(lambda e, ns=ns, bm=bm: e.tensor_tensor(out=x2.t[:, ns], in0=bm.t[:], in1=x2.t[:, ns], op=OP.add),
                      reads=[bm, x2], writes=[x2])
                if debug:
                    DMA("sync", lambda e, rows=rows: e.dma_start(out=DBG_X2[rows, :], in_=x2.t[:]), dbgch, reads=[x2])
                    k.wait_all("vector", chans=[dbgch], engines=False)

                yield 25.0
                rmsnorm_bf(x2, g2bc, h2, ss2, std2, rstd2, junkX)
                tp = transpose_to(h2, None, 8)
                A(lambda e, tp=tp: e.activation(out=xT.t[:], in_=tp.t[:], func=AF.Copy), reads=[tp], writes=[xT])
                for q4 in range(4):
                    bank = FB[q4 % 2]
                    for j in range(4):
                        cb = q4 * 4 + j
                        for kc in range(8):
                            T(lambda e, bank=bank, j=j, cb=cb, kc=kc: e.matmul(bank.t[:, j * 128:(j + 1) * 128],
                                                                              lhsT=wq.t[:, kc, cb * 128:(cb + 1) * 128],
                                                                              rhs=xT.t[:, kc * 128:(kc + 1) * 128],
                                                                              start=(kc == 0), stop=(kc == 7)), reads=[wq, xT], writes=[bank])
                    A(lambda e, bank=bank, q4=q4: e.activation(out=qT.t[:, q4 * 512:(q4 + 1) * 512], in_=bank.t[:], func=AF.Copy),
                      reads=[bank], writes=[qT])
                for q4 in range(4):
                    bank = FB[q4 % 2]
                    for j in range(4):
                        cb = q4 * 4 + j
                        T(lambda e, bank=bank, j=j, cb=cb: e.matmul(bank.t[:, j * 128:(j + 1) * 128], lhsT=qT3[:, cb, :], rhs=skT.t[:, cb, :],
                                                                    start=True, stop=True), reads=[qT, skT], writes=[bank])
                    A(lambda e, bank=bank, q4=q4: e.activation(out=Sc.t[:, q4 * 512:(q4 + 1) * 512], in_=bank.t[:], func=AF.Copy),
                      reads=[bank], writes=[Sc])

                def top16(vals, vals3, n, w, outv, outi):
                    v2 = Sc2.t.rearrange("p (c k) -> p c k", k=w)
                    for c in range(n):
                        V(lambda e, c=c: e.max(out=outv.t[:, c, 0:8], in_=vals3[:, c, :]), reads=[vals], writes=[outv])
                    yield 0.45 * n
                    for c in range(n):
                        V(lambda e, c=c: e.max_index(out=outi.t[:, c, 0:8], in_max=outv.t[:, c, 0:8], in_values=vals3[:, c, :]),
                          reads=[vals, outv], writes=[outi])
                    yield 0.45 * n
                    for c in range(n):
                        V(lambda e, c=c: e.match_replace(out=v2[:, c, :], in_to_replace=outv.t[:, c, 0:8], in_values=vals3[:, c, :],
                                                         imm_value=-BIG), reads=[vals, outv], writes=[Sc2])
                    yield 0.45 * n
                    for c in range(n):
                        V(lambda e, c=c: e.max(out=outv.t[:, c, 8:16], in_=v2[:, c, :]), reads=[Sc2], writes=[outv])
                    yield 0.45 * n
                    for c in range(n):
                        V(lambda e, c=c: e.max_index(out=outi.t[:, c, 8:16], in_max=outv.t[:, c, 8:16], in_values=v2[:, c, :]),
                          reads=[Sc2, outv], writes=[outi])
                    yield 0.45 * n

                yield from top16(Sc, Sc3, 16, 128, tv, ti)
                tv4 = tv.t[:, :, :].rearrange("p (h c) k -> p h c k", c=2)
                V(lambda e: e.tensor_tensor(out=cand3.rearrange("p h (a b) -> p h a b", b=16),
                                            in0=tv4[:, :, 0, :].unsqueeze(3).broadcast_to([128, 8, 16, 16]),
                                            in1=tv4[:, :, 1, :].unsqueeze(2).broadcast_to([128, 8, 16, 16]), op=OP.add),
                  reads=[tv], writes=[cand])
                yield from top16(cand, cand3, 8, 256, bv, bi)
                bif = bi.t[:, :, :].rearrange("p h k -> p (h k)")
                V(lambda e: e.tensor_copy(out=pf3[:, 0, :], in_=bif), reads=[bi], writes=[pf])
                V(lambda e: e.tensor_scalar(out=pi_.t[:], in0=pf3[:, 0, :], scalar1=1.0 / 16, scalar2=None, op0=OP.mult),
                  reads=[pf], writes=[pi_])
                V(lambda e: e.tensor_copy(out=pf3[:, 1, :], in_=pi_.t[:]), reads=[pi_], writes=[pf])
                V(lambda e: e.scalar_tensor_tensor(out=pf3[:, 2, :], in0=pf3[:, 1, :], scalar=-16.0, in1=pf3[:, 0, :], op0=OP.mult, op1=OP.add),
                  reads=[pf], writes=[pf])
                V(lambda e: e.tensor_scalar(out=pf3[:, 3, :], in0=pf3[:, 2, :], scalar1=0.0, scalar2=None, op0=OP.is_lt),
                  reads=[pf], writes=[pf])
                V(lambda e: e.tensor_tensor(out=pf3[:, 4, :], in0=pf3[:, 1, :], in1=pf3[:, 3, :], op=OP.subtract),
                  reads=[pf], writes=[pf])
                V(lambda e: e.scalar_tensor_tensor(out=pf3[:, 5, :], in0=pf3[:, 4, :], scalar=-16.0, in1=pf3[:, 0, :], op0=OP.mult, op1=OP.add),
                  reads=[pf], writes=[pf])
                V(lambda e: e.tensor_copy(out=tif.t[:, :], in_=ti.t[:, :, :].rearrange("p c k -> p (c k)")), reads=[ti], writes=[tif])
                tif4 = tif.t.rearrange("p (h c k) -> p h c k", c=2, k=16)
                oh4 = oh.t.rearrange("p (h k a) -> p h k a", k=16, a=16)
                for c in range(2):
                    V(lambda e, c=c: e.tensor_tensor(out=oh4, in0=pf3[:, 4 + c, :].rearrange("p (h k) -> p h k", k=16).unsqueeze(3).broadcast_to([128, 8, 16, 16]),
                                                     in1=iota.t[:, :].unsqueeze(1).unsqueeze(1).broadcast_to([128, 8, 16, 16]), op=OP.is_equal),
                      reads=[pf, iota], writes=[oh])
                    V(lambda e, c=c: e.tensor_tensor(out=oh4, in0=oh4, in1=tif4[:, :, c, :].unsqueeze(2).broadcast_to([128, 8, 16, 16]), op=OP.mult),
                      reads=[oh, tif], writes=[oh])
                    V(lambda e, c=c: e.tensor_reduce(out=isel3[:, c, :], in_=oh.t.rearrange("p (hk a) -> p hk a", a=16),
                                                     axis=AX.X, op=OP.add), reads=[oh], writes=[isel])
                V(lambda e: e.scalar_tensor_tensor(out=ef.t[:, :], in0=isel3[:, 0, :], scalar=128.0, in1=isel3[:, 1, :], op0=OP.mult, op1=OP.add),
                  reads=[isel], writes=[ef])
                V(lambda e: e.tensor_copy(out=eidx.t[:], in_=ef.t[:, :]), reads=[ef], writes=[eidx])
                V(lambda e: e.tensor_tensor(out=e0.t.rearrange("p (h k) -> p h k", k=16), in0=bv.t[:, :, :],
                                            in1=bv.t[:, :, 0:1].broadcast_to([128, 8, 16]), op=OP.subtract), reads=[bv], writes=[e0])
                A(lambda e: e.activation(out=e0.t[:, :], in_=e0.t[:, :], func=AF.Exp), reads=[e0], writes=[e0])
                V(lambda e: e.tensor_reduce(out=gs.t[:, 0:8], in_=e0.t.rearrange("p (h k) -> p h k", k=16), axis=AX.X, op=OP.add),
                  reads=[e0], writes=[gs])
                V(lambda e: e.reciprocal(out=gs.t[:, 8:16], in_=gs.t[:, 0:8]), reads=[gs], writes=[gs])
                V(lambda e: e.tensor_tensor(out=gsm.t[:, :].rearrange("p (h k) -> p h k", k=16), in0=e0.t.rearrange("p (h k) -> p h k", k=16),
                                            in1=gs.t[:, 8:16].unsqueeze(2).broadcast_to([128, 8, 16]), op=OP.mult), reads=[e0, gs], writes=[gsm])
                if debug:
                    DMA("sync", lambda e, rows=rows: e.dma_start(out=DBG_E[rows, :], in_=eidx.t[:]), dbgch, reads=[eidx])
            def stage_Y(kt):
                x2 = x2s[kt % 2]; h2 = h2s[kt % 2]; eidx = eidxs[kt % 2]; gsm = gsms[kt % 2]
                rows = slice(kt * 128, (kt + 1) * 128)
                for j in range(128):
                    ub = ubuf[j % (2 * NU)]
                    DMA("gpsimd", lambda e, ub=ub, j=j: e.indirect_dma_start(
                        out=ub.t[:, :], out_offset=None, in_=UB, in_offset=bass.IndirectOffsetOnAxis(ap=eidx.t[:, j:j + 1], axis=0)),
                        uch[j % (2 * NU)], reads=[eidx], writes=[ub])
                    V(lambda e, ub=ub, j=j: e.scalar_tensor_tensor(out=junkD.t[:], in0=ub.t[:, :], scalar=1.0, in1=h2.t[:], op0=OP.mult, op1=OP.mult,
                                                                   accum_out=av.t[:, j:j + 1]), reads=[ub, h2], writes=[junkD, av])
                    yield
                A(lambda e: e.activation(out=wgt.t[:], in_=av.t[:], func=AF.Gelu), reads=[av], writes=[wgt])
                V(lambda e: e.tensor_tensor(out=wgt.t[:], in0=wgt.t[:], in1=gsm.t[:], op=OP.mult), reads=[wgt, gsm], writes=[wgt])
                for j in range(128):
                    vb = vbuf[j % (2 * NU)]
                    DMA("gpsimd", lambda e, vb=vb, j=j: e.indirect_dma_start(
                        out=vb.t[:, :], out_offset=None, in_=VB, in_offset=bass.IndirectOffsetOnAxis(ap=eidx.t[:, j:j + 1], axis=0)),
                        vch2[j % (2 * NU)], reads=[eidx], writes=[vb])
                    dgb = dg[j % 4]
                    A(lambda e, dgb=dgb, j=j: e.activation(out=dgb.t[:], in_=identb.t[:], func=AF.Identity, scale=wgt.t[:, j:j + 1]),
                      reads=[identb, wgt], writes=[dgb])
                    for nb in range(2):
                        T(lambda e, nb=nb, dgb=dgb, vb=vb, j=j: e.matmul(Fv[nb].t[:, :], lhsT=dgb.t[:], rhs=vb.t[:, nb * 512:(nb + 1) * 512],
                                                                         start=(j == 0), stop=(j == 127)), reads=[dgb, vb], writes=[Fv[nb]])
                    yield
                if debug:
                    for nb in range(2):
                        ns = slice(nb * 512, (nb + 1) * 512)
                        V(lambda e, nb=nb, ns=ns: e.tensor_copy(out=dbt.t[:, ns], in_=Fv[nb].t[:]), reads=[Fv[nb]], writes=[dbt])
                    DMA("sync", lambda e, rows=rows: e.dma_start(out=DBG_PEER[rows, :], in_=dbt.t[:]), dbgch, reads=[dbt])
                    k.wait_all("vector", chans=[dbgch], engines=False)
                for nb in range(2):
                    ns = slice(nb * 512, (nb + 1) * 512)
                    V(lambda e, nb=nb, ns=ns: e.tensor_tensor(out=x2.t[:, ns], in0=Fv[nb].t[:], in1=x2.t[:, ns], op=OP.add),
                      reads=[Fv[nb], x2], writes=[x2])
                A(lambda e: e.activation(out=junkD.t[:], in_=x2.t[:], func=AF.Square, accum_out=ss3.t[:, 0:1]), reads=[x2], writes=[junkD, ss3])
                A(lambda e: e.activation(out=std3.t[:], in_=ss3.t[:], func=AF.Sqrt, scale=1.0 / D, bias=epsb.t[:, 0:1]),
                  reads=[ss3, epsb], writes=[std3])
                V(lambda e: e.reciprocal(out=rstd3.t[:], in_=std3.t[:]), reads=[std3], writes=[rstd3])
                V(lambda e: e.scalar_tensor_tensor(out=x2.t[:], in0=x2.t[:], scalar=rstd3.t[:, 0:1], in1=gfbc.t[:], op0=OP.mult, op1=OP.mult),
                  reads=[x2, rstd3, gfbc], writes=[x2])
                DMA("sync", lambda e, rows=rows: e.dma_start(out=out[rows, :], in_=x2.t[:]), ochs[kt % 2], reads=[x2])
                yield

            NY = 257

            def run_pair(gx, gy, wx):
                cx = 0.0
                iy = 0
                for w in gx:
                    cx += w
                    target = min(NY, int(NY * cx / (0.78 * wx)))
                    while gy is not None and iy < target:
                        if next(gy, "end") == "end":
                            gy = None
                        iy += 1
                if gy is not None:
                    for _ in gy:
                        pass

            prevY = None
            for kt in range(nt_b):
                wx = 1.6 * 3 * (9 + kt // 4) + 6.9 * NIT + 2.5 * 2 * (33 + kt) + 28.0 + 0.45 * 120 + 40.0
                run_pair(stage_X(kt), prevY, wx)
                prevY = stage_Y(kt)
            for _ in prevY:
                pass
            k.barrier()
            k.flush()
    return nc


def _consts():
    f32 = np.float32
    lg = np.log(1.0 - 2.0 ** (-5.0 - np.arange(4, dtype=np.float64)))
    j = np.arange(128)
    jj = j % 64
    kd = np.exp(lg[None, :] * (63 - jj)[:, None])
    kdec0 = np.repeat(kd * (j < 64)[:, None], 64, axis=1)
    kdec1 = np.repeat(kd * (j >= 64)[:, None], 64, axis=1)
    c_kdec = np.concatenate([kdec0, kdec1], axis=1).astype(f32)
    qd = np.exp(lg[:, None] * (jj + 1.0)[None, :]) / 8.0
    c_qdec = np.broadcast_to(qd.reshape(1, 512), (64, 512)).astype(f32).copy()
    same = (j[:, None] // 64) == (j[None, :] // 64)
    dm = np.stack([np.exp(lg[h] * np.abs(j[:, None] - j[None, :])) * same / 8.0 for h in range(4)], axis=1)
    c_dmat = dm.reshape(128, 512).astype(f32)
    gd = np.exp(lg * 64)
    c_gdec = np.broadcast_to(np.repeat(gd, 128).reshape(1, 512), (64, 512)).astype(f32).copy()
    diag = np.zeros((128, 4, 4, 128), np.float64)
    for r in range(4):
        for b in range(4):
            if b > r:
                diag[:, r, b, :] = -BIG
            elif b == r:
                diag[:64, r, b, 64:] = -BIG
    c_diag = diag.reshape(128, 2048).astype(f32)
    c_iota = np.broadcast_to(np.arange(16, dtype=f32)[None, :], (128, 16)).copy()
    c_pow = np.broadcast_to((-(2.0 ** -(np.arange(NIT) + 2.0)))[None, :], (128, NIT)).astype(f32).copy()
    nb = np.array([512 * (9 + kt // 4) - 511 for kt in range(NT)], dtype=f32)
    c_nb = np.broadcast_to(nb[None, :], (128, NT)).copy()
    return dict(c_nb=c_nb, c_ident=np.eye(128, dtype=f32), c_kdec=c_kdec, c_qdec=c_qdec, c_dmat=c_dmat, c_gdec=c_gdec,
                c_diag=c_diag, c_iota=c_iota, c_pow=c_pow)


def _ropetab(pos):
    pos = pos.astype(np.float32)
    inv_r = (np.float32(10000.0) ** (-np.arange(32, dtype=np.float32) / np.float32(32))).astype(np.float32)
    inv_d = (np.float32(500000.0) ** (-np.arange(8, dtype=np.float32) / np.float32(8))).astype(np.float32)
    ar = pos[:, None] * inv_r[None, :]
    ad = pos[:, None] * inv_d[None, :]
    return np.concatenate([np.cos(ar), np.sin(ar), np.cos(ad), np.sin(ad)], axis=1).astype(np.float32)


_NC_CACHE = {}


def _make_in_maps(x, attn_norm, w_in, ret_gn, w_ret_o, w_dsa_o, w_out, ffn_norm, peer_wq, peer_subkeys, peer_u, peer_v, final_norm):
    f32 = np.float32
    cs = _consts()
    w_in_p = np.concatenate([w_in[0][:, :3396], np.zeros((D, 60), f32), w_in[0][:, 3396:]], axis=1)
    w_in_p = np.ascontiguousarray(w_in_p, dtype=f32)
    bc = lambda v, n: np.ascontiguousarray(np.broadcast_to(np.asarray(v, f32).reshape(1, n), (128, n)))
    shared = dict(
        w_in=w_in_p, w_ro=np.ascontiguousarray(w_ret_o[0]), w_do=np.ascontiguousarray(w_dsa_o[0]),
        w_out=np.ascontiguousarray(w_out[0]), w_q=np.ascontiguousarray(peer_wq[0]),
        subk=np.ascontiguousarray(peer_subkeys[0].reshape(16, 128, 128)),
        peer_u=np.ascontiguousarray(peer_u[0]), peer_v=np.ascontiguousarray(peer_v[0]),
        g_attn=bc(attn_norm[0], D), g_gn=bc(ret_gn[0], 512), g_ffn=bc(ffn_norm[0], D), g_fin=bc(final_norm, D), **cs)
    in_maps = []
    for c in range(8):
        b, hf = c // 2, c % 2
        xo = np.ascontiguousarray(x[b, hf * LH:(hf + 1) * LH])
        xp = np.ascontiguousarray(x[b, 0:LH]) if hf == 1 else np.zeros((LH, D), f32)
        pos = np.concatenate([np.arange(LH), hf * LH + np.arange(LH)])
        m = dict(shared)
        m.update(xp=xp, xo=xo, ropetab=_ropetab(pos), c_pbias=np.full((128, 1), 0.0 if hf == 1 else -BIG, f32))
        in_maps.append(m)
    return in_maps


def kernel(x, attn_norm, w_in, ret_gn, w_ret_o, w_dsa_o, w_out, ffn_norm, peer_wq, peer_subkeys, peer_u, peer_v, final_norm):
    args = [np.asarray(a) for a in (x, attn_norm, w_in, ret_gn, w_ret_o, w_dsa_o, w_out, ffn_norm, peer_wq,
                                    peer_subkeys, peer_u, peer_v, final_norm)]
    in_maps = _make_in_maps(*args)
    if "nc" not in _NC_CACHE:
        _NC_CACHE["nc"] = build_program(DEBUG)
    res = run_bass_kernel_spmd(_NC_CACHE["nc"], in_maps, core_ids=list(range(8)))
    outp = np.empty((4, L, D), np.float32)
    for c in range(8):
        b, hf = c // 2, c % 2
        outp[b, hf * LH:(hf + 1) * LH] = np.asarray(res.results[c]["out"], np.float32)
    if DEBUG:
        _NC_CACHE["res"] = res
    return outp
```

```python
from contextlib import ExitStack
import numpy as np
import ml_dtypes
import concourse.bass as bass
import concourse.mybir as mybir
from concourse.bass_utils import run_bass_kernel_spmd

F32 = mybir.dt.float32
BF = mybir.dt.bfloat16
I32 = mybir.dt.int32
U32 = mybir.dt.uint32
AF = mybir.ActivationFunctionType
OP = mybir.AluOpType
AX = mybir.AxisListType

ENGS = ("tensor", "vector", "scalar", "gpsimd", "sync")

D = 1024
L = 8192
LH = 4096
NT = 32
NCOL = 5504
EPS = 1e-6
NIT = 16
BIG = 1.0e30
NU = 4
DEBUG = False


class Res:
    __slots__ = ("name", "w", "r", "excl")

    def __init__(self, name):
        self.name = name
        self.w = None
        self.r = []
        self.excl = False


class Chan:
    __slots__ = ("key", "count")

    def __init__(self, key):
        self.key = key
        self.count = 0


class _Rec:
    def __init__(self):
        self.call = None

    def __getattr__(self, name):
        def f(*a, **kw):
            self.call = (name, a, kw)
            return self
        return f


class K:
    def __init__(self, nc, es):
        self.nc = nc
        self.es = es
        self.sems = {}
        self.cnt = {}
        self.clock = {e: {} for e in ENGS}
        self.ops = {e: [] for e in ENGS}
        self.chans = []
        self.serial = set()
        for e in ENGS:
            self._mksem("E_" + e)
        self.nres = 0

    def _mksem(self, key):
        self.sems[key] = self.es.enter_context(self.nc.semaphore(key))
        self.cnt[key] = 0

    def res(self, name=None):
        self.nres += 1
        return Res(name or f"r{self.nres}")

    def chan(self, name, serial=False):
        key = "D_" + name
        self._mksem(key)
        c = Chan(key)
        self.chans.append(c)
        if serial:
            self.serial.add(key)
        return c

    def _need(self, eng, reads, writes):
        mykey = "E_" + eng
        need = {}

        def add(tok, kind):
            if tok is None:
                return
            key, val = tok
            if key == mykey:
                if eng == "tensor":
                    return
            if need.get(key, 0) < val:
                need[key] = val

        for r in reads:
            add(r.w, "raw")
        for w in writes:
            add(w.w, "waw")
            for t in w.r:
                add(t, "war")
        clk = self.clock[eng]
        out = []
        for key, val in need.items():
            if clk.get(key, 0) >= val:
                continue
            if key.startswith("D_") and key not in self.serial:
                assert val == self.cnt[key], f"stale DMA token wait {key} {val} != {self.cnt[key]}"
            clk[key] = val
            out.append((key, val))
        return out

    def op(self, eng, fn, reads=(), writes=()):
        writes = list(writes) + [r for r in reads if r.excl and r not in writes]
        for key, val in self._need(eng, reads, writes):
            self.ops[eng].append(("w", key, val))
        key = "E_" + eng
        self.cnt[key] += 1
        tok = (key, self.cnt[key])
        rec = _Rec()
        fn(rec)
        self.ops[eng].append(("o", rec.call, key, 1))
        for r in reads:
            r.r.append(tok)
        for w in writes:
            w.w = tok
            w.r = []
        return tok

    def dma(self, eng, fn, chan, reads=(), writes=()):
        for key, val in self._need(eng, reads, writes):
            self.ops[eng].append(("w", key, val))
        chan.count += 16
        self.cnt[chan.key] = chan.count
        tok = (chan.key, chan.count)
        rec = _Rec()
        fn(rec)
        self.ops[eng].append(("o", rec.call, chan.key, 16))
        for r in reads:
            r.r.append(tok)
        for w in writes:
            w.w = tok
            w.r = []
        return tok

    def settle(self, chan, ress):
        for r in ress:
            r.w = (chan.key, chan.count)

    def wait_all(self, eng, chans=None, engines=True):
        clk = self.clock[eng]
        keys = []
        if engines:
            keys += ["E_" + e for e in ENGS if e != eng]
        keys += [c.key for c in (chans if chans is not None else self.chans)]
        for key in keys:
            val = self.cnt[key]
            if val > clk.get(key, 0):
                clk[key] = val
                self.ops[eng].append(("w", key, val))

    def barrier(self):
        for e in ENGS:
            self.wait_all(e)

    def flush(self):
        nc = self.nc
        ops = self.ops
        sems = self.sems

        def replay(e, lst):
            for it in lst:
                if it[0] == "w":
                    e.wait_ge(sems[it[1]], it[2])
                else:
                    name, a, kw = it[1]
                    getattr(e, name)(*a, **kw).then_inc(sems[it[2]], it[3])

        with nc.Block() as block:
            @block.tensor
            def _(e):
                replay(e, ops["tensor"])

            @block.vector
            def _(e):
                replay(e, ops["vector"])

            @block.scalar
            def _(e):
                replay(e, ops["scalar"])

            @block.gpsimd
            def _(e):
                replay(e, ops["gpsimd"])

            @block.sync
            def _(e):
                replay(e, ops["sync"])
        self.ops = {e: [] for e in ENGS}


class Buf:
    __slots__ = ("t", "r")

    def __init__(self, t, r):
        self.t = t
        self.r = r


def build_program(debug=False, only_a=False, nt_b=NT, tiles_a=None, tabconv=True):
    nc = bass.Bass("TRN2", target_bir_lowering=False)

    def din(name, shape, dt=F32):
        return nc.dram_tensor(name, list(shape), dt, kind="ExternalInput").ap()

    def dscr(name, shape, dt):
        return nc.dram_tensor(name, list(shape), dt, kind=("ExternalOutput" if debug else "Internal")).ap()

    xp = din("xp", [LH, D])
    xo = din("xo", [LH, D])
    ropetab = din("ropetab", [L, 80])
    w_in = din("w_in", [D, NCOL])
    w_ro = din("w_ro", [512, D])
    w_do = din("w_do", [512, D])
    w_out = din("w_out", [D, D])
    w_q = din("w_q", [D, 2048])
    subk = din("subk", [16, 128, 128])
    peer_u = din("peer_u", [16384, D])
    peer_v = din("peer_v", [16384, D])
    g_attn = din("g_attn", [128, D])
    g_gn = din("g_gn", [128, 512])
    g_ffn = din("g_ffn", [128, D])
    g_fin = din("g_fin", [128, D])
    c_ident = din("c_ident", [128, 128])
    c_kdec = din("c_kdec", [128, 512])
    c_qdec = din("c_qdec", [64, 512])
    c_dmat = din("c_dmat", [128, 512])
    c_gdec = din("c_gdec", [64, 512])
    c_diag = din("c_diag", [128, 2048])
    c_pbias = din("c_pbias", [128, 1])
    c_iota = din("c_iota", [128, 16])
    c_pow = din("c_pow", [128, NIT])
    c_nb = din("c_nb", [128, NT])
    out = nc.dram_tensor("out", [LH, D], F32, kind="ExternalOutput").ap()

    UB = nc.dram_tensor("UB", [16384, D], BF, kind="Internal").ap()
    VB = nc.dram_tensor("VB", [16384, D], BF, kind="Internal").ap()
    KTD = dscr("KTD", [64, 8, L], BF)
    VD = dscr("VD", [L, 8 * 65], BF)
    QTD = dscr("QTD", [NT, 64, 1024], BF)
    IQTD = dscr("IQTD", [NT, 64, 512], BF)
    IWD = dscr("IWD", [LH, 4], F32)
    MRET = dscr("MRET", [LH, D], BF)
    GDSA = dscr("GDSA", [LH, D], BF)
    if debug:
        DBG_X2 = nc.dram_tensor("DBG_X2", [LH, D], F32, kind="ExternalOutput").ap()
        DBG_PEER = nc.dram_tensor("DBG_PEER", [LH, D], F32, kind="ExternalOutput").ap()
        DBG_YD = nc.dram_tensor("DBG_YD", [LH, 512], F32, kind="ExternalOutput").ap()
        DBG_LO = nc.dram_tensor("DBG_LO", [LH, 4], F32, kind="ExternalOutput").ap()
        DBG_E = nc.dram_tensor("DBG_E", [LH, 128], I32, kind="ExternalOutput").ap()

    with ExitStack() as es:
        k = K(nc, es)

        def sbuf(stack, name, shape, dt):
            return Buf(stack.enter_context(nc.sbuf_tensor(name, list(shape), dt)), k.res(name))

        def psum(stack, name, shape, dt):
            b = Buf(stack.enter_context(nc.psum_tensor(name, list(shape), dt)), k.res(name))
            b.r.excl = True
            return b

        def V(fn, reads=(), writes=()):
            return k.op("vector", fn, [b.r for b in reads], [b.r for b in writes])

        def A(fn, reads=(), writes=()):
            return k.op("scalar", fn, [b.r for b in reads], [b.r for b in writes])

        def T(fn, reads=(), writes=()):
            return k.op("tensor", fn, [b.r for b in reads], [b.r for b in writes])

        def G(fn, reads=(), writes=()):
            return k.op("gpsimd", fn, [b.r for b in reads], [b.r for b in writes])

        def DMA(eng, fn, chan, reads=(), writes=()):
            return k.dma(eng, fn, chan, [b.r for b in reads], [b.r for b in writes])

        class DR:
            def __init__(self, name):
                self.r = k.res(name)

        T0 = psum(es, "T0", [128, 1024], BF)
        T1 = [psum(es, f"T1{i}", [128, 1024], BF) for i in range(2)]
        T1x = T1[1]
        FB = [psum(es, f"F{i}", [128, 512], F32) for i in range(5)]

        identb = sbuf(es, "identb", [128, 128], BF)
        zerob = sbuf(es, "zerob", [128, 512], BF)
        IKT = sbuf(es, "IKT", [64, L], BF)
        c_chan = k.chan("const")
        DMA("gpsimd", lambda e: e.dma_start(out=identb.t[:], in_=c_ident), c_chan, writes=[identb])
        cA_chan = k.chan("constA")
        G(lambda e: e.memset(zerob.t[:], 0.0), writes=[zerob])

        t1_i = [0]

        def next_t1():
            t1_i[0] += 1
            return T1[t1_i[0] % len(T1)]

        def load_w_bf(dst, src, nk, ncol, chan, colchunk=1024):
            for kc in range(nk):
                for c0 in range(0, ncol, colchunk):
                    c1 = min(ncol, c0 + colchunk)
                    DMA("gpsimd", lambda e, kc=kc, c0=c0, c1=c1: e.dma_start(
                        out=dst.t[:, kc, c0:c1], in_=src[kc * 128:(kc + 1) * 128, c0:c1]), chan, writes=[dst])

        def rmsnorm_bf(xb, gbc, hb, ss, std, rstd, junk):
            A(lambda e: e.activation(out=junk.t[:], in_=xb.t[:], func=AF.Square, accum_out=ss.t[:, 0:1]),
              reads=[xb], writes=[junk, ss])
            A(lambda e: e.activation(out=std.t[:], in_=ss.t[:], func=AF.Sqrt, scale=1.0 / D, bias=epsb.t[:, 0:1]),
              reads=[ss, epsb], writes=[std])
            V(lambda e: e.reciprocal(out=rstd.t[:], in_=std.t[:]), reads=[std], writes=[rstd])
            V(lambda e: e.scalar_tensor_tensor(out=hb.t[:], in0=xb.t[:], scalar=rstd.t[:, 0:1], in1=gbc.t[:],
                                               op0=OP.mult, op1=OP.mult), reads=[xb, rstd, gbc], writes=[hb])

        def transpose_to(src, dst, nblk, cols=128, parts=128):
            tp = next_t1()
            for j in range(nblk):
                T(lambda e, j=j: e.transpose(tp.t[0:cols, j * parts:(j + 1) * parts],
                                             src.t[0:parts, j * cols:(j + 1) * cols], identb.t[0:parts, 0:parts]),
                  reads=[src, identb], writes=[tp])
            return tp

        epsb = sbuf(es, "epsb", [128, 1], F32)
        V(lambda e: e.memset(epsb.t[:], EPS), writes=[epsb])

        with ExitStack() as pa:
            wsb = sbuf(pa, "wsb", [128, 8, NCOL], BF)
            wro = sbuf(pa, "wro", [128, 4, D], BF)
            gbc = sbuf(pa, "gbc", [128, D], F32)
            gnbc = sbuf(pa, "gnbc", [128, 512], F32)
            kdec = sbuf(pa, "kdec", [128, 512], F32)
            qdec = sbuf(pa, "qdec", [64, 512], F32)
            dmat = sbuf(pa, "dmat", [128, 512], F32)
            gdec = sbuf(pa, "gdec", [64, 512], F32)
            wchan = k.chan("wA")
            DMA("sync", lambda e: e.dma_start(out=gbc.t[:], in_=g_attn), cA_chan, writes=[gbc])
            DMA("sync", lambda e: e.dma_start(out=gnbc.t[:], in_=g_gn), cA_chan, writes=[gnbc])
            DMA("sync", lambda e: e.dma_start(out=kdec.t[:], in_=c_kdec), cA_chan, writes=[kdec])
            DMA("sync", lambda e: e.dma_start(out=qdec.t[:], in_=c_qdec), cA_chan, writes=[qdec])
            DMA("sync", lambda e: e.dma_start(out=dmat.t[:], in_=c_dmat), cA_chan, writes=[dmat])
            DMA("sync", lambda e: e.dma_start(out=gdec.t[:], in_=c_gdec), cA_chan, writes=[gdec])
            wsbA = Buf(wsb.t, k.res("wsbA"))
            wsbB = Buf(wsb.t, k.res("wsbB"))
            wchanB = k.chan("wA2")
            for (rngs, wres, wch) in (([(0, 1024), (2048, 3396)], wsbA, wchan), ([(1024, 2048), (3396, NCOL)], wsbB, wchanB)):
                for (ra, rb) in rngs:
                    for c0 in range(ra, rb, 1024):
                        c1 = min(rb, c0 + 1024)
                        for kc in range(8):
                            DMA("gpsimd", lambda e, kc=kc, c0=c0, c1=c1: e.dma_start(
                                out=wsb.t[:, kc, c0:c1], in_=w_in[kc * 128:(kc + 1) * 128, c0:c1]), wch, writes=[wres])
            load_w_bf(wro, w_ro, 4, D, wchanB)
            k.settle(cA_chan, [b.r for b in (gbc, gnbc, kdec, qdec, dmat, gdec)])
            k.settle(wchan, [wsbA.r])
            k.settle(wchanB, [wsbB.r, wro.r])

            tstage = [sbuf(pa, f"tstage{i}", [128, 2, D], BF) for i in range(2)]
            tch_in = [k.chan(f"tin{i}") for i in range(2)]
            tch_out = [k.chan(f"tout{i}") for i in range(2)]
            r_UB = DR("UB")
            r_VB = DR("VB")
            def conv_gen():
                ci = 0
                for (src, dst, rr) in ((peer_u, UB, r_UB), (peer_v, VB, r_VB)):
                    for c in range(64 if tabconv else 0):
                        st = tstage[ci % 2]
                        sv = src[c * 256:(c + 1) * 256, :].rearrange("(r p) c -> p r c", p=128)
                        dv = dst[c * 256:(c + 1) * 256, :].rearrange("(r p) c -> p r c", p=128)
                        DMA("gpsimd", lambda e, st=st, sv=sv: e.dma_start(out=st.t[:], in_=sv), tch_in[ci % 2], writes=[st])
                        DMA("sync", lambda e, st=st, dv=dv: e.dma_start(out=dv, in_=st.t[:]), tch_out[ci % 2], reads=[st])
                        ci += 1
                        yield 1

            xbuf = [sbuf(pa, f"xbuf{i}", [128, D], F32) for i in range(2)]
            xch = [k.chan(f"x{i}") for i in range(2)]
            rtb = [sbuf(pa, f"rtb{i}", [128, 80], F32) for i in range(2)]
            rch = [k.chan(f"rt{i}") for i in range(2)]
            junkA = sbuf(pa, "junkA", [128, D], BF)
            ss = sbuf(pa, "ss", [128, 1], F32)
            std = sbuf(pa, "std", [128, 1], F32)
            rstd = sbuf(pa, "rstd", [128, 1], F32)
            hb = sbuf(pa, "hb", [128, D], BF)
            hT = [sbuf(pa, f"hT{i}", [128, D], BF) for i in range(2)]
            rtmp = sbuf(pa, "rtmp", [128, 4, 256], F32)
            RQKs = [sbuf(pa, f"RQK{i}", [128, 512], BF) for i in range(2)]
            Vt = [sbuf(pa, f"Vt{i}", [128, 512], BF) for i in range(2)]
            Kd = [sbuf(pa, f"Kd{i}", [128, 256], BF) for i in range(2)]
            S32 = [sbuf(pa, f"S32_{i}", [64, 512], F32) for i in range(5)]
            Sbf = [sbuf(pa, f"Sbf_{i}", [64, 512], BF) for i in range(5)]
            Stmp = sbuf(pa, "Stmp", [64, 512], F32)
            KTs = sbuf(pa, "KTs", [64, 512], BF)
            QTs = sbuf(pa, "QTs", [64, 512], BF)
            QW = sbuf(pa, "QW", [64, 4, 192], BF)
            sTm = sbuf(pa, "sTm", [128, 512], BF)
            gst = sbuf(pa, "gst", [128, 32], F32)
            yc = sbuf(pa, "yc", [128, 512], F32)
            ysq = yc
            sw = sbuf(pa, "sw", [128, 512], F32)
            yret = sbuf(pa, "yret", [128, 512], BF)
            yT = sbuf(pa, "yT", [128, 512], BF)
            gr = sbuf(pa, "gr", [128, D], BF)
            mst = [sbuf(pa, "mst0", [128, D], BF)]
            gdst = [sbuf(pa, "gdst0", [128, D], BF)]
            DQ = sbuf(pa, "DQ", [128, 512], BF)
            DKb = sbuf(pa, "DKb", [128, 512], BF)
            qtst = [sbuf(pa, "qtst0", [64, 1024], BF)]
            ktst = [sbuf(pa, "ktst0", [64, 1024], BF)]
            vast = [sbuf(pa, f"vast{i}", [128, 8, 65], BF) for i in range(2)]
            IQI = sbuf(pa, "IQI", [128, 320], BF)
            iqst = [sbuf(pa, f"iqst{i}", [64, 512], BF) for i in range(2)]
            iwst = [sbuf(pa, f"iwst{i}", [128, 4], F32) for i in range(2)]
            sch = {n: [k.chan(f"{n}{i}") for i in range(2)] for n in ("m", "gd", "qt", "kt", "va", "iq", "iw")}
            r_scr = DR("scratchA")

            G(lambda e: e.memset(QW.t[:], 0.0), writes=[QW])
            for i in range(2):
                G(lambda e, i=i: e.memset(vast[i].t[:], 1.0), writes=[vast[i]])
            G(lambda e: e.memset(S32[0].t[:], 0.0), writes=[S32[0]])
            G(lambda e: e.memset(Sbf[0].t[:], 0.0), writes=[Sbf[0]])

            fb_i = [0]

            def next_fb():
                fb_i[0] += 1
                return FB[fb_i[0] % 3]

            def proj(hTt, c0, ncol):
                wres = wsbA if (c0 < 1024 or 2048 <= c0 < 3396) else wsbB
                bank = next_fb()
                for kc in range(8):
                    T(lambda e, kc=kc: e.matmul(bank.t[:, 0:ncol], lhsT=hTt.t[:, kc * 128:(kc + 1) * 128],
                                               rhs=wsb.t[:, kc, c0:c0 + ncol], start=(kc == 0), stop=(kc == 7)),
                      reads=[hTt, wres], writes=[bank])
                return bank

            def rope_tok(bank, c0, H, half, rt, cos0, dst, d0):
                src3 = bank.t[:, c0:c0 + H * 64].rearrange("p (h e) -> p h e", e=64)
                dst3 = dst.t[:, d0:d0 + H * 64].rearrange("p (h e) -> p h e", e=64)
                x1 = src3[:, :, 0:half]
                x2 = src3[:, :, half:2 * half]
                cb = rt.t[:, cos0:cos0 + half].unsqueeze(1).broadcast_to([128, H, half])
                sb_ = rt.t[:, cos0 + half:cos0 + 2 * half].unsqueeze(1).broadcast_to([128, H, half])
                tm = [rtmp.t[:, i, 0:H * half].rearrange("p (h e) -> p h e", e=half) for i in range(4)]
                V(lambda e: e.tensor_tensor(out=tm[0], in0=x1, in1=cb, op=OP.mult), reads=[bank, rt], writes=[rtmp])
                V(lambda e: e.tensor_tensor(out=tm[1], in0=x2, in1=sb_, op=OP.mult), reads=[bank, rt], writes=[rtmp])
                V(lambda e: e.tensor_tensor(out=tm[2], in0=x1, in1=sb_, op=OP.mult), reads=[bank, rt], writes=[rtmp])
                V(lambda e: e.tensor_tensor(out=tm[3], in0=x2, in1=cb, op=OP.mult), reads=[bank, rt], writes=[rtmp])
                V(lambda e: e.tensor_tensor(out=dst3[:, :, 0:half], in0=tm[0], in1=tm[1], op=OP.subtract),
                  reads=[rtmp], writes=[dst])
                V(lambda e: e.tensor_tensor(out=dst3[:, :, half:2 * half], in0=tm[2], in1=tm[3], op=OP.add),
                  reads=[rtmp], writes=[dst])
                if 2 * half < 64:
                    A(lambda e: e.activation(out=dst3[:, :, 2 * half:64], in_=src3[:, :, 2 * half:64], func=AF.Copy),
                      reads=[bank], writes=[dst])

            def tileA(t):
                own = t >= 32
                tl = t % 32
                par = t % 2
                xb = xbuf[par]
                rt = rtb[par]
                RQK = RQKs[par]
                src = xo if own else xp
                DMA("sync", lambda e, xb=xb, src=src, tl=tl: e.dma_start(out=xb.t[:], in_=src[tl * 128:(tl + 1) * 128, :]),
                    xch[par], writes=[xb])
                DMA("sync", lambda e, rt=rt, t=t: e.dma_start(out=rt.t[:], in_=ropetab[t * 128:(t + 1) * 128, :]),
                    rch[par], writes=[rt])
                rmsnorm_bf(xb, gbc, hb, ss, std, rstd, junkA)
                for kc in range(8):
                    T(lambda e, kc=kc: e.transpose(T0.t[:, kc * 128:(kc + 1) * 128], hb.t[:, kc * 128:(kc + 1) * 128],
                                                   identb.t[:]), reads=[hb, identb], writes=[T0])
                hTt = hT[par]
                A(lambda e, hTt=hTt: e.activation(out=hTt.t[:], in_=T0.t[:], func=AF.Copy), reads=[T0], writes=[hTt])
                yield 1

                b0 = proj(hTt, 0, 512)
                rope_tok(b0, 0, 8, 32, rt, 0, RQK, 0)
                yield 1
                b1 = proj(hTt, 512, 512)
                vt = Vt[par]
                A(lambda e, b1=b1, vt=vt: e.activation(out=vt.t[:], in_=b1.t[:], func=AF.Copy), reads=[b1], writes=[vt])
                yield 1
                ia, ib, inx = (2 * t) % 5, (2 * t + 1) % 5, (2 * t + 2) % 5
                for c in range(2):
                    V(lambda e, c=c: e.tensor_tensor(out=Kd[c].t[:], in0=RQK.t[:, 256:512], in1=kdec.t[:, c * 256:(c + 1) * 256],
                                                     op=OP.mult), reads=[RQK, kdec], writes=[Kd[c]])
                for c, (si, so) in enumerate(((ia, ib), (ib, inx))):
                    kvb = FB[3]
                    for h in range(4):
                        T(lambda e, c=c, h=h: e.matmul(kvb.t[0:64, h * 128:(h + 1) * 128], lhsT=Kd[c].t[:, h * 64:(h + 1) * 64],
                                                       rhs=vt.t[:, h * 128:(h + 1) * 128], start=True, stop=True),
                          reads=[Kd[c], vt], writes=[kvb])
                    V(lambda e, si=si: e.tensor_tensor(out=Stmp.t[:], in0=S32[si].t[:], in1=gdec.t[:], op=OP.mult),
                      reads=[S32[si], gdec], writes=[Stmp])
                    V(lambda e, so=so: e.tensor_tensor(out=S32[so].t[:], in0=Stmp.t[:], in1=kvb.t[0:64, :], op=OP.add),
                      reads=[Stmp, kvb], writes=[S32[so]])
                    A(lambda e, so=so: e.activation(out=Sbf[so].t[:], in_=S32[so].t[:], func=AF.Copy),
                      reads=[S32[so]], writes=[Sbf[so]])
                yield 'half'

                if own:
                    tp = transpose_to(RQK, None, 8, cols=64)
                    A(lambda e, tp=tp: e.activation(out=KTs.t[:], in_=tp.t[0:64, 512:1024], func=AF.Copy), reads=[tp], writes=[KTs])
                    A(lambda e, tp=tp: e.activation(out=QTs.t[:], in_=tp.t[0:64, 0:512], func=AF.Copy), reads=[tp], writes=[QTs])
                    tq = tp.t[0:64, 0:512].rearrange("p (h e) -> p h e", e=128)
                    qd3 = qdec.t[:, :].rearrange("p (h e) -> p h e", e=128)
                    V(lambda e: e.tensor_tensor(out=QW.t[:, :, 0:64], in0=tq[:, :, 0:64], in1=qd3[:, :, 0:64], op=OP.mult),
                      reads=[tp, qdec], writes=[QW])
                    V(lambda e: e.tensor_tensor(out=QW.t[:, :, 128:192], in0=tq[:, :, 64:128], in1=qd3[:, :, 64:128], op=OP.mult),
                      reads=[tp, qdec], writes=[QW])
                    sTb = FB[3]
                    for h in range(4):
                        T(lambda e, h=h: e.matmul(sTb.t[:, h * 128:(h + 1) * 128], lhsT=KTs.t[:, h * 128:(h + 1) * 128],
                                                  rhs=QTs.t[:, h * 128:(h + 1) * 128], start=True, stop=True),
                          reads=[KTs, QTs], writes=[sTb])
                    V(lambda e: e.tensor_tensor(out=sTm.t[:], in0=sTb.t[:], in1=dmat.t[:], op=OP.mult),
                      reads=[sTb, dmat], writes=[sTm])
                    yb = FB[4]
                    for h in range(4):
                        hs = slice(h * 128, (h + 1) * 128)
                        T(lambda e, hs=hs: e.matmul(yb.t[:, hs], lhsT=sTm.t[:, hs], rhs=vt.t[:, hs], start=True, stop=False),
                          reads=[sTm, vt], writes=[yb])
                        T(lambda e, hs=hs, h=h: e.matmul(yb.t[:, hs], lhsT=QW.t[:, h, 0:128], rhs=Sbf[ia].t[:, hs], start=False, stop=False),
                          reads=[QW, Sbf[ia]], writes=[yb])
                        T(lambda e, hs=hs, h=h: e.matmul(yb.t[:, hs], lhsT=QW.t[:, h, 64:192], rhs=Sbf[ib].t[:, hs], start=False, stop=True),
                          reads=[QW, Sbf[ib]], writes=[yb])
                    yield 1
                    y3 = yb.t[:, :].rearrange("p (h e) -> p h e", e=128)
                    V(lambda e: e.tensor_reduce(out=gst.t[:, 0:4], in_=y3, axis=AX.X, op=OP.add), reads=[yb], writes=[gst])
                    A(lambda e: e.activation(out=ysq.t[:], in_=yb.t[:], func=AF.Square), reads=[yb], writes=[ysq])
                    V(lambda e: e.tensor_reduce(out=gst.t[:, 4:8], in_=ysq.t[:, :].rearrange("p (h e) -> p h e", e=128),
                                                axis=AX.X, op=OP.add), reads=[ysq], writes=[gst])
                    V(lambda e: e.tensor_scalar(out=gst.t[:, 8:12], in0=gst.t[:, 0:4], scalar1=1.0 / 128, scalar2=None, op0=OP.mult),
                      reads=[gst], writes=[gst])
                    V(lambda e: e.tensor_tensor(out=gst.t[:, 12:16], in0=gst.t[:, 8:12], in1=gst.t[:, 8:12], op=OP.mult),
                      reads=[gst], writes=[gst])
                    V(lambda e: e.scalar_tensor_tensor(out=gst.t[:, 16:20], in0=gst.t[:, 4:8], scalar=1.0 / 128, in1=gst.t[:, 12:16],
                                                       op0=OP.mult, op1=OP.subtract), reads=[gst], writes=[gst])
                    A(lambda e: e.activation(out=gst.t[:, 20:24], in_=gst.t[:, 16:20], func=AF.Sqrt, bias=epsb.t[:, 0:1]),
                      reads=[gst, epsb], writes=[gst])
                    V(lambda e: e.reciprocal(out=gst.t[:, 24:28], in_=gst.t[:, 20:24]), reads=[gst], writes=[gst])
                    yc3 = yc.t[:, :].rearrange("p (h e) -> p h e", e=128)
                    V(lambda e: e.tensor_tensor(out=yc3, in0=y3, in1=gst.t[:, 8:12].unsqueeze(2).broadcast_to([128, 4, 128]),
                                                op=OP.subtract), reads=[yb, gst], writes=[yc])
                    V(lambda e: e.tensor_tensor(out=yc3, in0=yc3, in1=gst.t[:, 24:28].unsqueeze(2).broadcast_to([128, 4, 128]),
                                                op=OP.mult), reads=[yc, gst], writes=[yc])
                    V(lambda e: e.tensor_tensor(out=yc.t[:], in0=yc.t[:], in1=gnbc.t[:], op=OP.mult), reads=[yc, gnbc], writes=[yc])
                    yield 1
                    b2 = proj(hTt, 1024, 512)
                    A(lambda e, b2=b2: e.activation(out=sw.t[:], in_=b2.t[:], func=AF.Silu), reads=[b2], writes=[sw])
                    V(lambda e: e.tensor_tensor(out=yret.t[:], in0=yc.t[:], in1=sw.t[:], op=OP.mult), reads=[yc, sw], writes=[yret])
                    tp = transpose_to(yret, None, 4)
                    A(lambda e, tp=tp: e.activation(out=yT.t[:], in_=tp.t[:, 0:512], func=AF.Copy), reads=[tp], writes=[yT])
                    ms = mst[0]
                    for nb in range(2):
                        bg = proj(hTt, 3456 + nb * 512, 512)
                        A(lambda e, bg=bg, nb=nb: e.activation(out=gr.t[:, nb * 512:(nb + 1) * 512], in_=bg.t[:], func=AF.Sigmoid),
                          reads=[bg], writes=[gr])
                        bm = next_fb()
                        for kc in range(4):
                            T(lambda e, kc=kc, nb=nb, bm=bm: e.matmul(bm.t[:, :], lhsT=yT.t[:, kc * 128:(kc + 1) * 128],
                                                                      rhs=wro.t[:, kc, nb * 512:(nb + 1) * 512],
                                                                      start=(kc == 0), stop=(kc == 3)), reads=[yT, wro], writes=[bm])
                        V(lambda e, nb=nb, bm=bm, ms=ms: e.tensor_tensor(out=ms.t[:, nb * 512:(nb + 1) * 512], in0=bm.t[:],
                                                                         in1=gr.t[:, nb * 512:(nb + 1) * 512], op=OP.mult),
                          reads=[bm, gr], writes=[ms])
                    DMA("sync", lambda e, ms=ms, tl=tl: e.dma_start(out=MRET[tl * 128:(tl + 1) * 128, :], in_=ms.t[:]),
                        sch["m"][0], reads=[ms])
                    yield 1
                    gd = gdst[0]
                    for nb in range(2):
                        bg = proj(hTt, 4480 + nb * 512, 512)
                        A(lambda e, bg=bg, nb=nb, gd=gd: e.activation(out=gd.t[:, nb * 512:(nb + 1) * 512], in_=bg.t[:], func=AF.Sigmoid),
                          reads=[bg], writes=[gd])
                    DMA("sync", lambda e, gd=gd, tl=tl: e.dma_start(out=GDSA[tl * 128:(tl + 1) * 128, :], in_=gd.t[:]),
                        sch["gd"][0], reads=[gd])
                    yield 1
                    b3 = proj(hTt, 1536, 512)
                    rope_tok(b3, 0, 8, 8, rt, 64, DQ, 0)
                    tp = transpose_to(DQ, None, 8, cols=64)
                    qs = qtst[0]
                    A(lambda e, tp=tp, qs=qs: e.activation(out=qs.t[:], in_=tp.t[0:64, :], func=AF.Copy), reads=[tp], writes=[qs])
                    DMA("sync", lambda e, qs=qs, tl=tl: e.dma_start(out=QTD[tl], in_=qs.t[:]), sch["qt"][0], reads=[qs])
                    yield 1

                b4 = proj(hTt, 2048, 512)
                rope_tok(b4, 0, 8, 8, rt, 64, DKb, 0)
                tp = transpose_to(DKb, None, 8, cols=64)
                ks = ktst[0]
                A(lambda e, tp=tp, ks=ks: e.activation(out=ks.t[:], in_=tp.t[0:64, :], func=AF.Copy), reads=[tp], writes=[ks])
                DMA("sync", lambda e, ks=ks, t=t: e.dma_start(out=KTD[:, :, t * 128:(t + 1) * 128],
                                                             in_=ks.t[:, :].rearrange("p (h e) -> p h e", e=128)),
                    sch["kt"][0], reads=[ks])
                yield 1
                b5 = proj(hTt, 2560, 512)
                va = vast[par]
                A(lambda e, b5=b5, va=va: e.activation(out=va.t[:, :, 0:64], in_=b5.t[:, :].rearrange("p (h e) -> p h e", e=64),
                                                       func=AF.Copy), reads=[b5], writes=[va])
                DMA("sync", lambda e, va=va, t=t: e.dma_start(out=VD[t * 128:(t + 1) * 128, :],
                                                             in_=va.t[:, :, :].rearrange("p h e -> p (h e)")),
                    sch["va"][par], reads=[va])
                yield 1
                b6 = proj(hTt, 3072, 324)
                rope_tok(b6, 0, 5, 8, rt, 64, IQI, 0)
                tp = transpose_to(IQI, None, 5, cols=64)
                A(lambda e, tp=tp, t=t: e.activation(out=IKT.t[:, t * 128:(t + 1) * 128], in_=tp.t[0:64, 512:640], func=AF.Copy),
                  reads=[tp], writes=[IKT])
                if own:
                    iqs = iqst[par]
                    A(lambda e, tp=tp, iqs=iqs: e.activation(out=iqs.t[:], in_=tp.t[0:64, 0:512], func=AF.Copy), reads=[tp], writes=[iqs])
                    DMA("sync", lambda e, iqs=iqs, tl=tl: e.dma_start(out=IQTD[tl], in_=iqs.t[:]), sch["iq"][par], reads=[iqs])
                    iws = iwst[par]
                    V(lambda e, b6=b6, iws=iws: e.tensor_scalar(out=iws.t[:], in0=b6.t[:, 320:324], scalar1=1.0 / 16, scalar2=None, op0=OP.mult),
                      reads=[b6], writes=[iws])
                    DMA("sync", lambda e, iws=iws, tl=tl: e.dma_start(out=IWD[tl * 128:(tl + 1) * 128, :], in_=iws.t[:]),
                        sch["iw"][par], reads=[iws])
            prevA = None
            cgen = conv_gen()
            for ti_, t in enumerate(tiles_a if tiles_a is not None else range(64)):
                if ti_ >= 12:
                    for _ in range(3):
                        next(cgen, None)
                cur = tileA(t)
                while True:
                    r = next(cur)
                    if prevA is not None and next(prevA, "end") == "end":
                        prevA = None
                    if r == "half":
                        break
                if prevA is not None:
                    for _ in prevA:
                        pass
                prevA = cur
            for _ in prevA:
                pass
            for _ in cgen:
                pass
            k.barrier()
            k.flush()

        with ExitStack() as pb:
            if only_a:
                return nc
            wdo = sbuf(pb, "wdo", [128, 4, D], BF)
            wout = sbuf(pb, "wout", [128, 8, D], BF)
            wq = sbuf(pb, "wq", [128, 8, 2048], BF)
            skT = sbuf(pb, "skT", [128, 16, 128], BF)
            g2bc = sbuf(pb, "g2bc", [128, D], F32)
            gfbc = sbuf(pb, "gfbc", [128, D], F32)
            diag = sbuf(pb, "diag", [128, 2048], BF)
            pbias = sbuf(pb, "pbias", [128, 1], F32)
            iota = sbuf(pb, "iota", [128, 16], F32)
            cpow = sbuf(pb, "cpow", [128, NIT], F32)
            cnb = sbuf(pb, "cnb", [128, NT], F32)
            c05 = sbuf(pb, "c05", [128, 1], F32)
            nbig = sbuf(pb, "nbig", [128, 1], F32)
            dummy = sbuf(pb, "fdummy", [128, 8], F32)
            POOLW = 10240
            pool_t = pb.enter_context(nc.sbuf_tensor("pool", [128, POOLW], F32))

            def view(name, w0, w1, dt=None, shape=None):
                ap = pool_t[:, w0:w1]
                if dt is not None:
                    ap = ap.bitcast(dt)
                return Buf(ap, k.res(name))

            isc = view("isc", 0, 8192)
            junkB = view("junkB", 8192, 10240, mybir.dt.int8)
            skr = view("skr", 0, 1024, BF)
            Sc = view("Sc", 0, 2048)
            cand = view("cand", 2048, 4096)
            qT = view("qT", 4096, 5120, BF)
            oh = view("oh", 5120, 6144, BF)
            tmp2 = view("tmp2", 6144, 6656)
            Sc2 = view("Sc2", 8192, 10240)
            pf = view("pf", 6656, 7424)
            tif = view("tif", 7424, 7680)
            isel = view("isel", 7680, 7936)
            ef = view("ef", 7936, 8064)
            e0 = view("e0", 8064, 8192)
            ubuf = [sbuf(pb, f"gbuf{i}", [128, D], BF) for i in range(2 * NU)]
            vbuf = ubuf
            dsa_views = [isc, junkB]
            peer_views = [Sc, cand, qT, oh, tmp2, pf, tif, isel, ef, e0, Sc2]

            def fence(frm, to):
                V(lambda e: e.memset(dummy.t[:, 0:1], 0.0), writes=list(frm) + list(to) + [dummy])

            wchB = k.chan("wB")
            cchB = k.chan("cB")
            cl = ((g2bc, g_ffn), (gfbc, g_fin), (pbias, c_pbias), (iota, c_iota), (cpow, c_pow), (cnb, c_nb))
            for dst, srcw in cl:
                DMA("sync", lambda e, dst=dst, srcw=srcw: e.dma_start(out=dst.t[:], in_=srcw), cchB, writes=[dst])
            k.settle(cchB, [d.r for d, _ in cl])
            DMA("gpsimd", lambda e: e.dma_start(out=diag.t[:], in_=c_diag), wchB, writes=[diag])
            load_w_bf(wdo, w_do, 4, D, wchB)
            load_w_bf(wout, w_out, 8, D, wchB)
            load_w_bf(wq, w_q, 8, 2048, wchB)
            skr3 = skr.t.rearrange("p (c d) -> p c d", d=128)
            DMA("gpsimd", lambda e: e.dma_start(out=skr3, in_=subk.rearrange("c k d -> k c d")), wchB, writes=[skr])
            k.settle(wchB, [wdo.r, wout.r, wq.r, skr.r, diag.r])
            V(lambda e: e.memset(c05.t[:], 0.5), writes=[c05])
            V(lambda e: e.memset(nbig.t[:], -30000.0), writes=[nbig])
            for q4 in range(4):
                tp = next_t1()
                for j in range(4):
                    cb = q4 * 4 + j
                    T(lambda e, cb=cb, j=j, tp=tp: e.transpose(tp.t[:, j * 128:(j + 1) * 128], skr3[:, cb, :], identb.t[:]),
                      reads=[skr, identb], writes=[tp])
                A(lambda e, q4=q4, tp=tp: e.activation(out=skT.t[:, q4 * 4:(q4 + 1) * 4, :].rearrange("p c k -> p (c k)"),
                                                       in_=tp.t[:, 0:512], func=AF.Copy), reads=[tp], writes=[skT])
            fence([skr], dsa_views + peer_views)

            x2s = [sbuf(pb, f"x2_{i}", [128, D], F32) for i in range(2)]
            qt_ = sbuf(pb, "qtk", [64, 1024], BF)
            iq_ = sbuf(pb, "iqk", [64, 512], BF)
            iw_ = sbuf(pb, "iwk", [128, 4], F32)
            mr_ = sbuf(pb, "mrk", [128, D], BF)
            gd_ = sbuf(pb, "gdk", [128, D], BF)
            lch = {n: k.chan(f"L{n}") for n in ("qt", "iq", "iw", "mr", "gd")}
            xlch = [k.chan(f"Lx{i}", serial=True) for i in range(2)]
            rel = [sbuf(pb, f"rel{i}", [128, 512], F32) for i in range(2)]
            bst = sbuf(pb, "bst", [128, 16], F32)
            nhd = sbuf(pb, "nhd", [128, NIT], F32)
            sS = sbuf(pb, "sS", [128, 2], F32)
            ind = sbuf(pb, "ind", [128, 2], F32)
            nmid = [sbuf(pb, f"nmid{i}", [128, 1], F32) for i in range(2)]
            lo = sbuf(pb, "lo", [128, 1], F32)
            KTg = [sbuf(pb, f"KTg{i}", [64, 8, 256], BF) for i in range(2)]
            Vg = [sbuf(pb, f"Vg{i}", [128, 2, 520], BF) for i in range(2)]
            kch = [k.chan(f"ktg{i}") for i in range(2)]
            vch = [k.chan(f"vg{i}") for i in range(2)]
            mk = [sbuf(pb, f"mk{i}", [128, 256], BF) for i in range(4)]
            mT = [sbuf(pb, f"mT{i}", [128, 256], BF) for i in range(4)]
            Eb = [sbuf(pb, f"Eb{i}", [128, 512], BF) for i in range(2)]
            rden = sbuf(pb, "rden", [128, 8], F32)
            yd = sbuf(pb, "yd", [128, 512], BF)
            xT = sbuf(pb, "xT", [128, D], BF)
            mtmp = sbuf(pb, "mtmp", [128, 512], F32)
            mg = sbuf(pb, "mg", [128, D], BF)
            junkX = Buf(mtmp.t[:, :].bitcast(BF), mtmp.r)
            h2s = [sbuf(pb, f"h2_{i}", [128, D], BF) for i in range(2)]
            ss3 = sbuf(pb, "ss3", [128, 1], F32)
            std3 = sbuf(pb, "std3", [128, 1], F32)
            rstd3 = sbuf(pb, "rstd3", [128, 1], F32)
            ss2 = sbuf(pb, "ss2", [128, 1], F32)
            std2 = sbuf(pb, "std2", [128, 1], F32)
            rstd2 = sbuf(pb, "rstd2", [128, 1], F32)
            tv = sbuf(pb, "tv", [128, 16, 16], F32)
            ti = sbuf(pb, "ti", [128, 16, 16], U32)
            bv = sbuf(pb, "bv", [128, 8, 16], F32)
            bi = sbuf(pb, "bi", [128, 8, 16], U32)
            pi_ = sbuf(pb, "pi", [128, 128], I32)
            eidxs = [sbuf(pb, f"eidx{i}", [128, 128], I32) for i in range(2)]
            gs = sbuf(pb, "gs", [128, 16], F32)
            gsms = [sbuf(pb, f"gsm{i}", [128, 128], F32) for i in range(2)]
            av = sbuf(pb, "av", [128, 128], F32)
            wgt = sbuf(pb, "wgt", [128, 128], F32)
            junkD = sbuf(pb, "junkD", [128, D], BF)
            uch = [k.chan(f"u{i}", serial=True) for i in range(2 * NU)]
            vch2 = uch
            dg = [sbuf(pb, f"dg{i}", [128, 128], BF) for i in range(8)]
            ochs = [k.chan(f"o{i}", serial=True) for i in range(2)]
            t1_i[0] = 0
            T1.pop()
            Fv = [FB[4], Buf(T1x.t[:, :].bitcast(F32), T1x.r)]
            if debug:
                dbgch = k.chan("dbg")
                dbt = sbuf(pb, "dbt", [128, D], F32)

            Sc3 = Sc.t.rearrange("p (c k) -> p c k", k=128)
            cand3 = cand.t.rearrange("p (h k) -> p h k", k=256)
            qT3 = qT.t.rearrange("p (c k) -> p c k", k=128)
            tmp23 = tmp2.t.rearrange("p (i k) -> p i k", k=256)
            pf3 = pf.t.rearrange("p (i k) -> p i k", k=128)
            isel3 = isel.t.rearrange("p (c k) -> p c k", k=128)

            def stage_X(kt):
                x2 = x2s[kt % 2]; h2 = h2s[kt % 2]; eidx = eidxs[kt % 2]; gsm = gsms[kt % 2]
                G_k = 9 + kt // 4
                N_k = 512 * G_k
                NB_k = 33 + kt
                r = kt % 4
                rows = slice(kt * 128, (kt + 1) * 128)
                fence(peer_views, dsa_views)
                DMA("sync", lambda e, rows=rows: e.dma_start(out=x2.t[:], in_=xo[rows, :]), xlch[kt % 2], writes=[x2])
                DMA("sync", lambda e, kt=kt: e.dma_start(out=qt_.t[:], in_=QTD[kt]), lch["qt"], writes=[qt_])
                DMA("sync", lambda e, kt=kt: e.dma_start(out=iq_.t[:], in_=IQTD[kt]), lch["iq"], writes=[iq_])
                DMA("sync", lambda e, rows=rows: e.dma_start(out=iw_.t[:], in_=IWD[rows, :]), lch["iw"], writes=[iw_])
                DMA("sync", lambda e, rows=rows: e.dma_start(out=mr_.t[:], in_=MRET[rows, :]), lch["mr"], writes=[mr_])
                DMA("sync", lambda e, rows=rows: e.dma_start(out=gd_.t[:], in_=GDSA[rows, :]), lch["gd"], writes=[gd_])

                ri = 0
                for g in range(G_k):
                    gs_ = slice(g * 512, (g + 1) * 512)
                    for h in range(4):
                        bank = FB[(g * 4 + h) % 2]
                        T(lambda e, bank=bank, h=h, gs_=gs_: e.matmul(bank.t[:, :], lhsT=iq_.t[:, h * 128:(h + 1) * 128],
                                                                      rhs=IKT.t[:, gs_], start=True, stop=True),
                          reads=[iq_, IKT], writes=[bank])
                        rl = rel[ri % 2]
                        ri += 1
                        A(lambda e, bank=bank, rl=rl: e.activation(out=rl.t[:], in_=bank.t[:], func=AF.Relu), reads=[bank], writes=[rl])
                        if h == 0:
                            V(lambda e, rl=rl, gs_=gs_: e.tensor_scalar(out=isc.t[:, gs_], in0=rl.t[:], scalar1=iw_.t[:, 0:1],
                                                                        scalar2=None, op0=OP.mult), reads=[rl, iw_], writes=[isc])
                        else:
                            V(lambda e, rl=rl, gs_=gs_, h=h: e.scalar_tensor_tensor(
                                out=isc.t[:, gs_], in0=rl.t[:], scalar=iw_.t[:, h:h + 1], in1=isc.t[:, gs_], op0=OP.mult, op1=OP.add),
                              reads=[rl, iw_, isc], writes=[isc])
                    yield 1.6
                V(lambda e, N_k=N_k: e.tensor_reduce(out=bst.t[:, 0:1], in_=isc.t[:, 0:N_k], axis=AX.X, op=OP.max,
                                                     apply_absolute_value=True), reads=[isc], writes=[bst])
                V(lambda e: e.tensor_scalar(out=bst.t[:, 1:2], in0=bst.t[:, 0:1], scalar1=2.0, scalar2=1.0, op0=OP.mult, op1=OP.add),
                  reads=[bst], writes=[bst])
                V(lambda e: e.tensor_scalar(out=nhd.t[:], in0=cpow.t[:], scalar1=bst.t[:, 1:2], scalar2=None, op0=OP.mult),
                  reads=[cpow, bst], writes=[nhd])
                V(lambda e: e.tensor_scalar(out=isc.t[:, 0:LH], in0=isc.t[:, 0:LH], scalar1=pbias.t[:, 0:1], scalar2=None, op0=OP.add),
                  reads=[isc, pbias], writes=[isc])
                V(lambda e, N_k=N_k, r=r: e.tensor_tensor(out=isc.t[:, N_k - 512:N_k], in0=isc.t[:, N_k - 512:N_k],
                                                          in1=diag.t[:, r * 512:(r + 1) * 512], op=OP.add), reads=[isc, diag], writes=[isc])
                cur = c05
                for it in range(NIT):
                    A(lambda e, N_k=N_k, cur=cur: e.activation(out=junkB.t[:, 0:N_k], in_=isc.t[:, 0:N_k], func=AF.Sign,
                                                               bias=cur.t[:, 0:1], accum_out=sS.t[:, 0:1]),
                      reads=[isc, cur], writes=[junkB, sS])
                    A(lambda e, kt=kt: e.activation(out=ind.t[:, 0:1], in_=sS.t[:, 0:1], func=AF.Sign, bias=cnb.t[:, kt:kt + 1]),
                      reads=[sS, cnb], writes=[ind])
                    nxt = nmid[it % 2]
                    A(lambda e, it=it, cur=cur, nxt=nxt: e.activation(out=nxt.t[:], in_=ind.t[:, 0:1], func=AF.Identity,
                                                                      scale=nhd.t[:, it:it + 1], bias=cur.t[:, 0:1]),
                      reads=[ind, nhd, cur], writes=[nxt])
                    cur = nxt
                    yield 6.9
                A(lambda e, cur=cur: e.activation(out=lo.t[:], in_=cur.t[:], func=AF.Identity, scale=-1.0, bias=nhd.t[:, NIT - 1:NIT]),
                  reads=[cur, nhd], writes=[lo])
                if debug:
                    V(lambda e: e.tensor_copy(out=dbt.t[:, 0:1], in_=lo.t[:]), reads=[lo], writes=[dbt])
                    V(lambda e: e.tensor_copy(out=dbt.t[:, 1:3], in_=bst.t[:, 0:2]), reads=[bst], writes=[dbt])
                    V(lambda e: e.tensor_copy(out=dbt.t[:, 3:4], in_=sS.t[:, 0:1]), reads=[sS], writes=[dbt])
                    DMA("sync", lambda e, rows=rows: e.dma_start(out=DBG_LO[rows, :], in_=dbt.t[:, 0:4]), dbgch, reads=[dbt])
                    k.wait_all("vector", chans=[dbgch], engines=False)

                Fo = [FB[2], FB[3]]
                for hg in range(2):
                    T(lambda e, hg=hg: e.matmul(Fo[hg].t[:, 0:260], lhsT=zerob.t[:, 0:128], rhs=zerob.t[:, 0:260], start=True, stop=False),
                      reads=[zerob], writes=[Fo[hg]])
                ngrp = (NB_k + 1) // 2
                grp = {}

                gmask = {}

                def pre(g):
                    gp = g % 2
                    ktg, vg = KTg[gp], Vg[gp]
                    gs_ = slice(g * 256, (g + 1) * 256)
                    DMA("sync", lambda e: e.dma_start(out=ktg.t[:], in_=KTD[:, :, gs_]), kch[gp], writes=[ktg])
                    DMA("sync", lambda e: e.dma_start(out=vg.t[:], in_=VD[gs_, :].rearrange("(b p) c -> p b c", p=128)),
                        vch[gp], writes=[vg])
                    grp[g] = (ktg, vg)

                def pre_mask(g):
                    gs_ = slice(g * 256, (g + 1) * 256)
                    mkb = mk[g % 4]
                    V(lambda e: e.tensor_scalar(out=mkb.t[:], in0=isc.t[:, gs_], scalar1=lo.t[:, 0:1], scalar2=None, op0=OP.is_gt),
                      reads=[isc, lo], writes=[mkb])
                    nb_here = min(2, NB_k - g * 2)
                    for b in range(nb_here):
                        T(lambda e, b=b: e.transpose(T0.t[:, b * 128:(b + 1) * 128], mkb.t[:, b * 128:(b + 1) * 128], identb.t[:]),
                          reads=[mkb, identb], writes=[T0])
                    mTb = mT[g % 4]
                    A(lambda e: e.activation(out=mTb.t[:, 0:nb_here * 128], in_=T0.t[:, 0:nb_here * 128], func=AF.Identity,
                                             scale=30000.0, bias=nbig.t[:, 0:1]), reads=[T0, nbig], writes=[mTb])
                    gmask[g] = mTb

                units = [(g, b, hg) for g in range(ngrp) for b in range(min(2, NB_k - g * 2)) for hg in range(2)]

                def qk(ui):
                    g, b, hg = units[ui]
                    ktg, vg = grp[g]
                    mTb = gmask[g]
                    Fs = FB[ui % 2]
                    for h4 in range(4):
                        T(lambda e, h4=h4: e.matmul(Fs.t[:, h4 * 128:(h4 + 1) * 128], lhsT=identb.t[:], rhs=mTb.t[:, b * 128:(b + 1) * 128],
                                                    start=(h4 == 0), stop=False), reads=[identb, mTb], writes=[Fs])
                    for h4 in range(4):
                        hh = hg * 4 + h4
                        T(lambda e, h4=h4, hh=hh: e.matmul(
                            Fs.t[:, h4 * 128:(h4 + 1) * 128], lhsT=ktg.t[:, hh, b * 128:(b + 1) * 128],
                            rhs=qt_.t[:, hh * 128:(hh + 1) * 128], start=False, stop=(h4 == 3)), reads=[ktg, qt_], writes=[Fs])

                def smpv(ui):
                    g, b, hg = units[ui]
                    ktg, vg = grp[g]
                    Fs = FB[ui % 2]
                    eb = Eb[ui % 2]
                    last_blk = (g * 2 + b == NB_k - 1)
                    A(lambda e: e.activation(out=eb.t[:], in_=Fs.t[:], func=AF.Exp, scale=0.125), reads=[Fs], writes=[eb])
                    for h4 in range(4):
                        hh = hg * 4 + h4
                        T(lambda e, h4=h4, hh=hh: e.matmul(
                            Fo[hg].t[:, h4 * 65:(h4 + 1) * 65], lhsT=eb.t[:, h4 * 128:(h4 + 1) * 128],
                            rhs=vg.t[:, b, hh * 65:(hh + 1) * 65], start=False, stop=(last_blk and h4 == 3)),
                          reads=[eb, vg], writes=[Fo[hg]])

                pre(0)
                if ngrp > 1:
                    pre(1)
                for g_ in range(min(3, ngrp)):
                    pre_mask(g_)
                qk(0)
                for ui in range(len(units)):
                    g, b, hg = units[ui]
                    if ui + 1 < len(units):
                        qk(ui + 1)
                    smpv(ui)
                    if ui + 1 == len(units) or units[ui + 1][0] != g:
                        if g + 2 < ngrp:
                            pre(g + 2)
                        if g + 3 < ngrp:
                            pre_mask(g + 3)
                    yield 2.5
                for hg in range(2):
                    fo3 = Fo[hg].t[:, 0:260].rearrange("p (h e) -> p h e", e=65)
                    V(lambda e, hg=hg, fo3=fo3: e.reciprocal(out=rden.t[:, hg * 4:(hg + 1) * 4].unsqueeze(2), in_=fo3[:, :, 64:65]),
                      reads=[Fo[hg]], writes=[rden])
                    V(lambda e, hg=hg, fo3=fo3: e.tensor_tensor(
                        out=yd.t[:, hg * 256:(hg + 1) * 256].rearrange("p (h e) -> p h e", e=64), in0=fo3[:, :, 0:64],
                        in1=rden.t[:, hg * 4:(hg + 1) * 4].unsqueeze(2).broadcast_to([128, 4, 64]), op=OP.mult),
                      reads=[Fo[hg], rden], writes=[yd])
                if debug:
                    V(lambda e: e.tensor_copy(out=dbt.t[:, 0:512], in_=yd.t[:]), reads=[yd], writes=[dbt])
                    DMA("sync", lambda e, rows=rows: e.dma_start(out=DBG_YD[rows, :], in_=dbt.t[:, 0:512]), dbgch, reads=[dbt])
                    k.wait_all("vector", chans=[dbgch], engines=False)
                fence(dsa_views, peer_views)
                yield 3.0
                tp = transpose_to(yd, None, 4)
                A(lambda e, tp=tp: e.activation(out=xT.t[:, 0:512], in_=tp.t[:, 0:512], func=AF.Copy), reads=[tp], writes=[xT])
                for nb in range(2):
                    ns = slice(nb * 512, (nb + 1) * 512)
                    bm = FB[nb]
                    for kc in range(4):
                        T(lambda e, kc=kc, ns=ns, bm=bm: e.matmul(bm.t[:, :], lhsT=xT.t[:, kc * 128:(kc + 1) * 128], rhs=wdo.t[:, kc, ns],
                                                                  start=(kc == 0), stop=(kc == 3)), reads=[xT, wdo], writes=[bm])
                    V(lambda e, ns=ns, bm=bm: e.tensor_tensor(out=mtmp.t[:], in0=bm.t[:], in1=gd_.t[:, ns], op=OP.mult),
                      reads=[bm, gd_], writes=[mtmp])
                    V(lambda e, ns=ns: e.tensor_tensor(out=mg.t[:, ns], in0=mtmp.t[:], in1=mr_.t[:, ns], op=OP.add),
                      reads=[mtmp, mr_], writes=[mg])
                tp = transpose_to(mg, None, 8)
                A(lambda e, tp=tp: e.activation(out=xT.t[:], in_=tp.t[:], func=AF.Copy), reads=[tp], writes=[xT])
                for nb in range(2):
                    ns = slice(nb * 512, (nb + 1) * 512)
                    bm = FB[nb]
                    for kc in range(8):
                        T(lambda e, kc=kc, ns=ns, bm=bm: e.matmul(bm.t[:, :], lhsT=xT.t[:, kc * 128:(kc + 1) * 128], rhs=wout.t[:, kc, ns],
                                                                  start=(kc == 0), stop=(kc == 7)), reads=[xT, wout], writes=[bm])
                    V(lambda e, ns=ns, bm=bm: e.tensor_tensor(out=x2.t[:, ns], in0=bm.t[:], in1=x2.t[:, ns], op=OP.add),
                      reads=[bm, x2], writes=[x2])
                if debug:
                    DMA("sync", lambda e, rows=rows: e.dma_start(out=DBG_X2[rows, :], in_=x2.t[:]), dbgch, reads=[x2])
                    k.wait_all("vector", chans=[dbgch], engines=False)

                yield 25.0
                rmsnorm_bf(x2, g2bc, h2, ss2, std2, rstd2, junkX)
                tp = transpose_to(h2, None, 8)
                A(lambda e, tp=tp: e.activation(out=xT.t[:], in_=tp.t[:], func=AF.Copy), reads=[tp], writes=[xT])
                for q4 in range(4):
                    bank = FB[q4 % 2]
                    for j in range(4):
                        cb = q4 * 4 + j
                        for kc in range(8):
                            T(lambda e, bank=bank, j=j, cb=cb, kc=kc: e.matmul(bank.t[:, j * 128:(j + 1) * 128],
                                                                              lhsT=wq.t[:, kc, cb * 128:(cb + 1) * 128],
                                                                              rhs=xT.t[:, kc * 128:(kc + 1) * 128],
                                                                              start=(kc == 0), stop=(kc == 7)), reads=[wq, xT], writes=[bank])
                    A(lambda e, bank=bank, q4=q4: e.activation(out=qT.t[:, q4 * 512:(q4 + 1) * 512], in_=bank.t[:], func=AF.Copy),
                      reads=[bank], writes=[qT])
                for q4 in range(4):
                    bank = FB[q4 % 2]
                    for j in range(4):
                        cb = q4 * 4 + j
                        T(lambda e, bank=bank, j=j, cb=cb: e.matmul(bank.t[:, j * 128:(j + 1) * 128], lhsT=qT3[:, cb, :], rhs=skT.t[:, cb, :],
                                                                    start=True, stop=True), reads=[qT, skT], writes=[bank])
                    A(lambda e, bank=bank, q4=q4: e.activation(out=Sc.t[:, q4 * 512:(q4 + 1) * 512], in_=bank.t[:], func=AF.Copy),
                      reads=[bank], writes=[Sc])

                def top16(vals, vals3, n, w, outv, outi):
                    v2 = Sc2.t.rearrange("p (c k) -> p c k", k=w)
                    for c in range(n):
                        V(lambda e, c=c: e.max(out=outv.t[:, c, 0:8], in_=vals3[:, c, :]), reads=[vals], writes=[outv])
                    yield 0.45 * n
                    for c in range(n):
                        V(lambda e, c=c: e.max_index(out=outi.t[:, c, 0:8], in_max=outv.t[:, c, 0:8], in_values=vals3[:, c, :]),
                          reads=[vals, outv], writes=[outi])
                    yield 0.45 * n
                    for c in range(n):
                        V(lambda e, c=c: e.match_replace(out=v2[:, c, :], in_to_replace=outv.t[:, c, 0:8], in_values=vals3[:, c, :],
                                                         imm_value=-BIG), reads=[vals, outv], writes=[Sc2])
                    yield 0.45 * n
                    for c in range(n):
                        V(lambda e, c=c: e.max(out=outv.t[:, c, 8:16], in_=v2[:, c, :]), reads=[Sc2], writes=[outv])
                    yield 0.45 * n
                    for c in range(n):
                        V(lambda e, c=c: e.max_index(out=outi.t[:, c, 8:16], in_max=outv.t[:, c, 8:16], in_values=v2[:, c, :]),
                          reads=[Sc2, outv], writes=[outi])
                    yield 0.45 * n

                yield from top16(Sc, Sc3, 16, 128, tv, ti)
                tv4 = tv.t[:, :, :].rearrange("p (h c) k -> p h c k", c=2)
                V(lambda e: e.tensor_tensor(out=cand3.rearrange("p h (a b) -> p h a b", b=16),
                                            in0=tv4[:, :, 0, :].unsqueeze(3).broadcast_to([128, 8, 16, 16]),
                                            in1=tv4[:, :, 1, :].unsqueeze(2).broadcast_to([128, 8, 16, 16]), op=OP.add),
                  reads=[tv], writes=[cand])
                yield from top16(cand, cand3, 8, 256, bv, bi)
                bif = bi.t[:, :, :].rearrange("p h k -> p (h k)")
                V(lambda e: e.tensor_copy(out=pf3[:, 0, :], in_=bif), reads=[bi], writes=[pf])
                V(lambda e: e.tensor_scalar(out=pi_.t[:], in0=pf3[:, 0, :], scalar1=1.0 / 16, scalar2=None, op0=OP.mult),
                  reads=[pf], writes=[pi_])
                V(lambda e: e.tensor_copy(out=pf3[:, 1, :], in_=pi_.t[:]), reads=[pi_], writes=[pf])
                V(lambda e: e.scalar_tensor_tensor(out=pf3[:, 2, :], in0=pf3[:, 1, :], scalar=-16.0, in1=pf3[:, 0, :], op0=OP.mult, op1=OP.add),
                  reads=[pf], writes=[pf])
                V(lambda e: e.tensor_scalar(out=pf3[:, 3, :], in0=pf3[:, 2, :], scalar1=0.0, scalar2=None, op0=OP.is_lt),
                  reads=[pf], writes=[pf])
                V(lambda e: e.tensor_tensor(out=pf3[:, 4, :], in0=pf3[:, 1, :], in1=pf3[:, 3, :], op=OP.subtract),
                  reads=[pf], writes=[pf])
                V(lambda e: e.scalar_tensor_tensor(out=pf3[:, 5, :], in0=pf3[:, 4, :], scalar=-16.0, in1=pf3[:, 0, :], op0=OP.mult, op1=OP.add),
                  reads=[pf], writes=[pf])
                V(lambda e: e.tensor_copy(out=tif.t[:, :], in_=ti.t[:, :, :].rearrange("p c k -> p (c k)")), reads=[ti], writes=[tif])
                tif4 = tif.t.rearrange("p (h c k) -> p h c k", c=2, k=16)
                oh4 = oh.t.rearrange("p (h k a) -> p h k a", k=16, a=16)
                for c in range(2):
                    V(lambda e, c=c: e.tensor_tensor(out=oh4, in0=pf3[:, 4 + c, :].rearrange("p (h k) -> p h k", k=16).unsqueeze(3).broadcast_to([128, 8, 16, 16]),
                                                     in1=iota.t[:, :].unsqueeze(1).unsqueeze(1).broadcast_to([128, 8, 16, 16]), op=OP.is_equal),
                      reads=[pf, iota], writes=[oh])
                    V(lambda e, c=c: e.tensor_tensor(out=oh4, in0=oh4, in1=tif4[:, :, c, :].unsqueeze(2).broadcast_to([128, 8, 16, 16]), op=OP.mult),
                      reads=[oh, tif], writes=[oh])
                    V(lambda e, c=c: e.tensor_reduce(out=isel3[:, c, :], in_=oh.t.rearrange("p (hk a) -> p hk a", a=16),
                                                     axis=AX.X, op=OP.add), reads=[oh], writes=[isel])
                V(lambda e: e.scalar_tensor_tensor(out=ef.t[:, :], in0=isel3[:, 0, :], scalar=128.0, in1=isel3[:, 1, :], op0=OP.mult, op1=OP.add),
                  reads=[isel], writes=[ef])
                V(lambda e: e.tensor_copy(out=eidx.t[:], in_=ef.t[:, :]), reads=[ef], writes=[eidx])
                V(lambda e: e.tensor_tensor(out=e0.t.rearrange("p (h k) -> p h k", k=16), in0=bv.t[:, :, :],
                                            in1=bv.t[:, :, 0:1].broadcast_to([128, 8, 16]), op=OP.subtract), reads=[bv], writes=[e0])
                A(lambda e: e.activation(out=e0.t[:, :], in_=e0.t[:, :], func=AF.Exp), reads=[e0], writes=[e0])
                V(lambda e: e.tensor_reduce(out=gs.t[:, 0:8], in_=e0.t.rearrange("p (h k) -> p h k", k=16), axis=AX.X, op=OP.add),
                  reads=[e0], writes=[gs])
                V(lambda e: e.reciprocal(out=gs.t[:, 8:16], in_=gs.t[:, 0:8]), reads=[gs], writes=[gs])
                V(lambda e: e.tensor_tensor(out=gsm.t[:, :].rearrange("p (h k) -> p h k", k=16), in0=e0.t.rearrange("p (h k) -> p h k", k=16),
                                            in1=gs.t[:, 8:16].unsqueeze(2).broadcast_to([128, 8, 16]), op=OP.mult), reads=[e0, gs], writes=[gsm])
                if debug:
                    DMA("sync", lambda e, rows=rows: e.dma_start(out=DBG_E[rows, :], in_=eidx.t[:]), dbgch, reads=[eidx])
            def stage_Y(kt):
                x2 = x2s[kt % 2]; h2 = h2s[kt % 2]; eidx = eidxs[kt % 2]; gsm = gsms[kt % 2]
                rows = slice(kt * 128, (kt + 1) * 128)
                for j in range(128):
                    ub = ubuf[j % (2 * NU)]
                    DMA("gpsimd", lambda e, ub=ub, j=j: e.indirect_dma_start(
                        out=ub.t[:, :], out_offset=None, in_=UB, in_offset=bass.IndirectOffsetOnAxis(ap=eidx.t[:, j:j + 1], axis=0)),
                        uch[j % (2 * NU)], reads=[eidx], writes=[ub])
                    V(lambda e, ub=ub, j=j: e.scalar_tensor_tensor(out=junkD.t[:], in0=ub.t[:, :], scalar=1.0, in1=h2.t[:], op0=OP.mult, op1=OP.mult,
                                                                   accum_out=av.t[:, j:j + 1]), reads=[ub, h2], writes=[junkD, av])
                    yield
                A(lambda e: e.activation(out=wgt.t[:], in_=av.t[:], func=AF.Gelu), reads=[av], writes=[wgt])
                V(lambda e: e.tensor_tensor(out=wgt.t[:], in0=wgt.t[:], in1=gsm.t[:], op=OP.mult), reads=[wgt, gsm], writes=[wgt])
                for j in range(128):
                    vb = vbuf[j % (2 * NU)]
                    DMA("gpsimd", lambda e, vb=vb, j=j: e.indirect_dma_start(
                        out=vb.t[:, :], out_offset=None, in_=VB, in_offset=bass.IndirectOffsetOnAxis(ap=eidx.t[:, j:j + 1], axis=0)),
                        vch2[j % (2 * NU)], reads=[eidx], writes=[vb])
                    dgb = dg[j % 8]
                    A(lambda e, dgb=dgb, j=j: e.activation(out=dgb.t[:], in_=identb.t[:], func=AF.Identity, scale=wgt.t[:, j:j + 1]),
                      reads=[identb, wgt], writes=[dgb])
                    for nb in range(2):
                        T(lambda e, nb=nb, dgb=dgb, vb=vb, j=j: e.matmul(Fv[nb].t[:, :], lhsT=dgb.t[:], rhs=vb.t[:, nb * 512:(nb + 1) * 512],
                                                                         start=(j == 0), stop=(j == 127)), reads=[dgb, vb], writes=[Fv[nb]])
                    yield
                if debug:
                    for nb in range(2):
                        ns = slice(nb * 512, (nb + 1) * 512)
                        V(lambda e, nb=nb, ns=ns: e.tensor_copy(out=dbt.t[:, ns], in_=Fv[nb].t[:]), reads=[Fv[nb]], writes=[dbt])
                    DMA("sync", lambda e, rows=rows: e.dma_start(out=DBG_PEER[rows, :], in_=dbt.t[:]), dbgch, reads=[dbt])
                    k.wait_all("vector", chans=[dbgch], engines=False)
                for nb in range(2):
                    ns = slice(nb * 512, (nb + 1) * 512)
                    V(lambda e, nb=nb, ns=ns: e.tensor_tensor(out=x2.t[:, ns], in0=Fv[nb].t[:], in1=x2.t[:, ns], op=OP.add),
                      reads=[Fv[nb], x2], writes=[x2])
                A(lambda e: e.activation(out=junkD.t[:], in_=x2.t[:], func=AF.Square, accum_out=ss3.t[:, 0:1]), reads=[x2], writes=[junkD, ss3])
                A(lambda e: e.activation(out=std3.t[:], in_=ss3.t[:], func=AF.Sqrt, scale=1.0 / D, bias=epsb.t[:, 0:1]),
                  reads=[ss3, epsb], writes=[std3])
                V(lambda e: e.reciprocal(out=rstd3.t[:], in_=std3.t[:]), reads=[std3], writes=[rstd3])
                V(lambda e: e.scalar_tensor_tensor(out=x2.t[:], in0=x2.t[:], scalar=rstd3.t[:, 0:1], in1=gfbc.t[:], op0=OP.mult, op1=OP.mult),
                  reads=[x2, rstd3, gfbc], writes=[x2])
                DMA("sync", lambda e, rows=rows: e.dma_start(out=out[rows, :], in_=x2.t[:]), ochs[kt % 2], reads=[x2])
                yield

            NY = 257

            def run_pair(gx, gy, wx):
                cx = 0.0
                iy = 0
                for w in gx:
                    cx += w
                    target = min(NY, int(NY * cx / (0.85 * wx)))
                    while gy is not None and iy < target:
                        if next(gy, "end") == "end":
                            gy = None
                        iy += 1
                if gy is not None:
                    for _ in gy:
                        pass

            prevY = None
            for kt in range(nt_b):
                wx = 1.6 * 3 * (9 + kt // 4) + 6.9 * NIT + 2.5 * 2 * (33 + kt) + 28.0 + 0.45 * 120 + 40.0
                run_pair(stage_X(kt), prevY, wx)
                prevY = stage_Y(kt)
            for _ in prevY:
                pass
            k.barrier()
            k.flush()
    return nc


def _consts():
    f32 = np.float32
    lg = np.log(1.0 - 2.0 ** (-5.0 - np.arange(4, dtype=np.float64)))
    j = np.arange(128)
    jj = j % 64
    kd = np.exp(lg[None, :] * (63 - jj)[:, None])
    kdec0 = np.repeat(kd * (j < 64)[:, None], 64, axis=1)
    kdec1 = np.repeat(kd * (j >= 64)[:, None], 64, axis=1)
    c_kdec = np.concatenate([kdec0, kdec1], axis=1).astype(f32)
    qd = np.exp(lg[:, None] * (jj + 1.0)[None, :]) / 8.0
    c_qdec = np.broadcast_to(qd.reshape(1, 512), (64, 512)).astype(f32).copy()
    same = (j[:, None] // 64) == (j[None, :] // 64)
    dm = np.stack([np.exp(lg[h] * np.abs(j[:, None] - j[None, :])) * same / 8.0 for h in range(4)], axis=1)
    c_dmat = dm.reshape(128, 512).astype(f32)
    gd = np.exp(lg * 64)
    c_gdec = np.broadcast_to(np.repeat(gd, 128).reshape(1, 512), (64, 512)).astype(f32).copy()
    diag = np.zeros((128, 4, 4, 128), np.float64)
    for r in range(4):
        for b in range(4):
            if b > r:
                diag[:, r, b, :] = -BIG
            elif b == r:
                diag[:64, r, b, 64:] = -BIG
    c_diag = diag.reshape(128, 2048).astype(f32)
    c_iota = np.broadcast_to(np.arange(16, dtype=f32)[None, :], (128, 16)).copy()
    c_pow = np.broadcast_to((-(2.0 ** -(np.arange(NIT) + 2.0)))[None, :], (128, NIT)).astype(f32).copy()
    nb = np.array([512 * (9 + kt // 4) - 511 for kt in range(NT)], dtype=f32)
    c_nb = np.broadcast_to(nb[None, :], (128, NT)).copy()
    return dict(c_nb=c_nb, c_ident=np.eye(128, dtype=f32), c_kdec=c_kdec, c_qdec=c_qdec, c_dmat=c_dmat, c_gdec=c_gdec,
                c_diag=c_diag, c_iota=c_iota, c_pow=c_pow)


def _ropetab(pos):
    pos = pos.astype(np.float32)
    inv_r = (np.float32(10000.0) ** (-np.arange(32, dtype=np.float32) / np.float32(32))).astype(np.float32)
    inv_d = (np.float32(500000.0) ** (-np.arange(8, dtype=np.float32) / np.float32(8))).astype(np.float32)
    ar = pos[:, None] * inv_r[None, :]
    ad = pos[:, None] * inv_d[None, :]
    return np.concatenate([np.cos(ar), np.sin(ar), np.cos(ad), np.sin(ad)], axis=1).astype(np.float32)


_NC_CACHE = {}


def _make_in_maps(x, attn_norm, w_in, ret_gn, w_ret_o, w_dsa_o, w_out, ffn_norm, peer_wq, peer_subkeys, peer_u, peer_v, final_norm):
    f32 = np.float32
    cs = _consts()
    w_in_p = np.concatenate([w_in[0][:, :3396], np.zeros((D, 60), f32), w_in[0][:, 3396:]], axis=1)
    w_in_p = np.ascontiguousarray(w_in_p, dtype=f32)
    bc = lambda v, n: np.ascontiguousarray(np.broadcast_to(np.asarray(v, f32).reshape(1, n), (128, n)))
    shared = dict(
        w_in=w_in_p, w_ro=np.ascontiguousarray(w_ret_o[0]), w_do=np.ascontiguousarray(w_dsa_o[0]),
        w_out=np.ascontiguousarray(w_out[0]), w_q=np.ascontiguousarray(peer_wq[0]),
        subk=np.ascontiguousarray(peer_subkeys[0].reshape(16, 128, 128)),
        peer_u=np.ascontiguousarray(peer_u[0]), peer_v=np.ascontiguousarray(peer_v[0]),
        g_attn=bc(attn_norm[0], D), g_gn=bc(ret_gn[0], 512), g_ffn=bc(ffn_norm[0], D), g_fin=bc(final_norm, D), **cs)
    in_maps = []
    for c in range(8):
        b, hf = c // 2, c % 2
        xo = np.ascontiguousarray(x[b, hf * LH:(hf + 1) * LH])
        xp = np.ascontiguousarray(x[b, 0:LH]) if hf == 1 else np.zeros((LH, D), f32)
        pos = np.concatenate([np.arange(LH), hf * LH + np.arange(LH)])
        m = dict(shared)
        m.update(xp=xp, xo=xo, ropetab=_ropetab(pos), c_pbias=np.full((128, 1), 0.0 if hf == 1 else -BIG, f32))
        in_maps.append(m)
    return in_maps


def kernel(x, attn_norm, w_in, ret_gn, w_ret_o, w_dsa_o, w_out, ffn_norm, peer_wq, peer_subkeys, peer_u, peer_v, final_norm):
    args = [np.asarray(a) for a in (x, attn_norm, w_in, ret_gn, w_ret_o, w_dsa_o, w_out, ffn_norm, peer_wq,
                                    peer_subkeys, peer_u, peer_v, final_norm)]
    in_maps = _make_in_maps(*args)
    if "nc" not in _NC_CACHE:
        _NC_CACHE["nc"] = build_program(DEBUG)
    res = run_bass_kernel_spmd(_NC_CACHE["nc"], in_maps, core_ids=list(range(8)))
    outp = np.empty((4, L, D), np.float32)
    for c in range(8):
        b, hf = c // 2, c % 2
        outp[b, hf * LH:(hf + 1) * LH] = np.asarray(res.results[c]["out"], np.float32)
    if DEBUG:
        _NC_CACHE["res"] = res
    return outp
```

```python
from contextlib import ExitStack
import numpy as np
import ml_dtypes
import concourse.bass as bass
import concourse.mybir as mybir
from concourse.bass_utils import run_bass_kernel_spmd

F32 = mybir.dt.float32
BF = mybir.dt.bfloat16
I32 = mybir.dt.int32
U32 = mybir.dt.uint32
AF = mybir.ActivationFunctionType
OP = mybir.AluOpType
AX = mybir.AxisListType

ENGS = ("tensor", "vector", "scalar", "gpsimd", "sync")

D = 1024
L = 8192
LH = 4096
NT = 32
NCOL = 5504
EPS = 1e-6
NIT = 16
BIG = 1.0e30
NU = 4
DEBUG = False


class Res:
    __slots__ = ("name", "w", "r", "excl")

    def __init__(self, name):
        self.name = name
        self.w = None
        self.r = []
        self.excl = False


class Chan:
    __slots__ = ("key", "count")

    def __init__(self, key):
        self.key = key
        self.count = 0


class _Rec:
    def __init__(self):
        self.call = None

    def __getattr__(self, name):
        def f(*a, **kw):
            self.call = (name, a, kw)
            return self
        return f


class K:
    def __init__(self, nc, es):
        self.nc = nc
        self.es = es
        self.sems = {}
        self.cnt = {}
        self.clock = {e: {} for e in ENGS}
        self.ops = {e: [] for e in ENGS}
        self.chans = []
        self.serial = set()
        for e in ENGS:
            self._mksem("E_" + e)
        self.nres = 0

    def _mksem(self, key):
        self.sems[key] = self.es.enter_context(self.nc.semaphore(key))
        self.cnt[key] = 0

    def res(self, name=None):
        self.nres += 1
        return Res(name or f"r{self.nres}")

    def chan(self, name, serial=False):
        key = "D_" + name
        self._mksem(key)
        c = Chan(key)
        self.chans.append(c)
        if serial:
            self.serial.add(key)
        return c

    def _need(self, eng, reads, writes):
        mykey = "E_" + eng
        need = {}

        def add(tok, kind):
            if tok is None:
                return
            key, val = tok
            if key == mykey:
                if eng == "tensor":
                    return
                if kind == "war" and eng in ("vector", "scalar"):
                    return
            if need.get(key, 0) < val:
                need[key] = val

        for r in reads:
            add(r.w, "raw")
        for w in writes:
            add(w.w, "waw")
            for t in w.r:
                add(t, "war")
        clk = self.clock[eng]
        out = []
        for key, val in need.items():
            if clk.get(key, 0) >= val:
                continue
            if key.startswith("D_") and key not in self.serial:
                assert val == self.cnt[key], f"stale DMA token wait {key} {val} != {self.cnt[key]}"
            clk[key] = val
            out.append((key, val))
        return out

    def op(self, eng, fn, reads=(), writes=()):
        writes = list(writes) + [r for r in reads if r.excl and r not in writes]
        for key, val in self._need(eng, reads, writes):
            self.ops[eng].append(("w", key, val))
        key = "E_" + eng
        self.cnt[key] += 1
        tok = (key, self.cnt[key])
        rec = _Rec()
        fn(rec)
        self.ops[eng].append(("o", rec.call, key, 1))
        for r in reads:
            r.r.append(tok)
        for w in writes:
            w.w = tok
            w.r = []
        return tok

    def dma(self, eng, fn, chan, reads=(), writes=()):
        for key, val in self._need(eng, reads, writes):
            self.ops[eng].append(("w", key, val))
        chan.count += 16
        self.cnt[chan.key] = chan.count
        tok = (chan.key, chan.count)
        rec = _Rec()
        fn(rec)
        self.ops[eng].append(("o", rec.call, chan.key, 16))
        for r in reads:
            r.r.append(tok)
        for w in writes:
            w.w = tok
            w.r = []
        return tok

    def settle(self, chan, ress):
        for r in ress:
            r.w = (chan.key, chan.count)

    def wait_all(self, eng, chans=None, engines=True):
        clk = self.clock[eng]
        keys = []
        if engines:
            keys += ["E_" + e for e in ENGS if e != eng]
        keys += [c.key for c in (chans if chans is not None else self.chans)]
        for key in keys:
            val = self.cnt[key]
            if val > clk.get(key, 0):
                clk[key] = val
                self.ops[eng].append(("w", key, val))

    def barrier(self):
        for e in ENGS:
            self.wait_all(e)

    def flush(self):
        nc = self.nc
        ops = self.ops
        sems = self.sems

        def replay(e, lst):
            for it in lst:
                if it[0] == "w":
                    e.wait_ge(sems[it[1]], it[2])
                else:
                    name, a, kw = it[1]
                    getattr(e, name)(*a, **kw).then_inc(sems[it[2]], it[3])

        with nc.Block() as block:
            @block.tensor
            def _(e):
                replay(e, ops["tensor"])

            @block.vector
            def _(e):
                replay(e, ops["vector"])

            @block.scalar
            def _(e):
                replay(e, ops["scalar"])

            @block.gpsimd
            def _(e):
                replay(e, ops["gpsimd"])

            @block.sync
            def _(e):
                replay(e, ops["sync"])
        self.ops = {e: [] for e in ENGS}


class Buf:
    __slots__ = ("t", "r")

    def __init__(self, t, r):
        self.t = t
        self.r = r


def build_program(debug=False, only_a=False, nt_b=NT, tiles_a=None, tabconv=True):
    nc = bass.Bass("TRN2", target_bir_lowering=False)

    def din(name, shape, dt=F32):
        return nc.dram_tensor(name, list(shape), dt, kind="ExternalInput").ap()

    def dscr(name, shape, dt):
        return nc.dram_tensor(name, list(shape), dt, kind=("ExternalOutput" if debug else "Internal")).ap()

    xp = din("xp", [LH, D])
    xo = din("xo", [LH, D])
    ropetab = din("ropetab", [L, 80])
    w_in = din("w_in", [D, NCOL])
    w_ro = din("w_ro", [512, D])
    w_do = din("w_do", [512, D])
    w_out = din("w_out", [D, D])
    w_q = din("w_q", [D, 2048])
    subk = din("subk", [16, 128, 128])
    peer_u = din("peer_u", [16384, D])
    peer_v = din("peer_v", [16384, D])
    g_attn = din("g_attn", [128, D])
    g_gn = din("g_gn", [128, 512])
    g_ffn = din("g_ffn", [128, D])
    g_fin = din("g_fin", [128, D])
    c_ident = din("c_ident", [128, 128])
    c_kdec = din("c_kdec", [128, 512])
    c_qdec = din("c_qdec", [64, 512])
    c_dmat = din("c_dmat", [128, 512])
    c_gdec = din("c_gdec", [64, 512])
    c_diag = din("c_diag", [128, 2048])
    c_pbias = din("c_pbias", [128, 1])
    c_iota = din("c_iota", [128, 16])
    c_pow = din("c_pow", [128, NIT])
    c_nb = din("c_nb", [128, NT])
    out = nc.dram_tensor("out", [LH, D], F32, kind="ExternalOutput").ap()

    UB = nc.dram_tensor("UB", [16384, D], BF, kind="Internal").ap()
    VB = nc.dram_tensor("VB", [16384, D], BF, kind="Internal").ap()
    KTD = dscr("KTD", [64, 8, L], BF)
    VD = dscr("VD", [L, 8 * 65], BF)
    QTD = dscr("QTD", [NT, 64, 1024], BF)
    IQTD = dscr("IQTD", [NT, 64, 512], BF)
    IWD = dscr("IWD", [LH, 4], F32)
    MRET = dscr("MRET", [LH, D], BF)
    GDSA = dscr("GDSA", [LH, D], BF)
    if debug:
        DBG_X2 = nc.dram_tensor("DBG_X2", [LH, D], F32, kind="ExternalOutput").ap()
        DBG_PEER = nc.dram_tensor("DBG_PEER", [LH, D], F32, kind="ExternalOutput").ap()
        DBG_YD = nc.dram_tensor("DBG_YD", [LH, 512], F32, kind="ExternalOutput").ap()
        DBG_LO = nc.dram_tensor("DBG_LO", [LH, 4], F32, kind="ExternalOutput").ap()
        DBG_E = nc.dram_tensor("DBG_E", [LH, 128], I32, kind="ExternalOutput").ap()

    with ExitStack() as es:
        k = K(nc, es)

        def sbuf(stack, name, shape, dt):
            return Buf(stack.enter_context(nc.sbuf_tensor(name, list(shape), dt)), k.res(name))

        def psum(stack, name, shape, dt):
            b = Buf(stack.enter_context(nc.psum_tensor(name, list(shape), dt)), k.res(name))
            b.r.excl = True
            return b

        def V(fn, reads=(), writes=()):
            return k.op("vector", fn, [b.r for b in reads], [b.r for b in writes])

        def A(fn, reads=(), writes=()):
            return k.op("scalar", fn, [b.r for b in reads], [b.r for b in writes])

        def T(fn, reads=(), writes=()):
            return k.op("tensor", fn, [b.r for b in reads], [b.r for b in writes])

        def G(fn, reads=(), writes=()):
            return k.op("gpsimd", fn, [b.r for b in reads], [b.r for b in writes])

        def DMA(eng, fn, chan, reads=(), writes=()):
            return k.dma(eng, fn, chan, [b.r for b in reads], [b.r for b in writes])

        class DR:
            def __init__(self, name):
                self.r = k.res(name)

        T0 = psum(es, "T0", [128, 1024], BF)
        T1 = [psum(es, f"T1{i}", [128, 1024], BF) for i in range(2)]
        T1x = T1[1]
        FB = [psum(es, f"F{i}", [128, 512], F32) for i in range(5)]

        identb = sbuf(es, "identb", [128, 128], BF)
        zerob = sbuf(es, "zerob", [128, 512], BF)
        IKT = sbuf(es, "IKT", [64, L], BF)
        c_chan = k.chan("const")
        DMA("gpsimd", lambda e: e.dma_start(out=identb.t[:], in_=c_ident), c_chan, writes=[identb])
        cA_chan = k.chan("constA")
        G(lambda e: e.memset(zerob.t[:], 0.0), writes=[zerob])

        t1_i = [0]

        def next_t1():
            t1_i[0] += 1
            return T1[t1_i[0] % len(T1)]

        def load_w_bf(dst, src, nk, ncol, chan, colchunk=1024):
            for kc in range(nk):
                for c0 in range(0, ncol, colchunk):
                    c1 = min(ncol, c0 + colchunk)
                    DMA("gpsimd", lambda e, kc=kc, c0=c0, c1=c1: e.dma_start(
                        out=dst.t[:, kc, c0:c1], in_=src[kc * 128:(kc + 1) * 128, c0:c1]), chan, writes=[dst])

        def rmsnorm_bf(xb, gbc, hb, ss, std, rstd, junk):
            A(lambda e: e.activation(out=junk.t[:], in_=xb.t[:], func=AF.Square, accum_out=ss.t[:, 0:1]),
              reads=[xb], writes=[junk, ss])
            A(lambda e: e.activation(out=std.t[:], in_=ss.t[:], func=AF.Sqrt, scale=1.0 / D, bias=epsb.t[:, 0:1]),
              reads=[ss, epsb], writes=[std])
            V(lambda e: e.reciprocal(out=rstd.t[:], in_=std.t[:]), reads=[std], writes=[rstd])
            V(lambda e: e.scalar_tensor_tensor(out=hb.t[:], in0=xb.t[:], scalar=rstd.t[:, 0:1], in1=gbc.t[:],
                                               op0=OP.mult, op1=OP.mult), reads=[xb, rstd, gbc], writes=[hb])

        def transpose_to(src, dst, nblk, cols=128, parts=128):
            tp = next_t1()
            for j in range(nblk):
                T(lambda e, j=j: e.transpose(tp.t[0:cols, j * parts:(j + 1) * parts],
                                             src.t[0:parts, j * cols:(j + 1) * cols], identb.t[0:parts, 0:parts]),
                  reads=[src, identb], writes=[tp])
            return tp

        epsb = sbuf(es, "epsb", [128, 1], F32)
        V(lambda e: e.memset(epsb.t[:], EPS), writes=[epsb])

        with ExitStack() as pa:
            wsb = sbuf(pa, "wsb", [128, 8, NCOL], BF)
            wro = sbuf(pa, "wro", [128, 4, D], BF)
            gbc = sbuf(pa, "gbc", [128, D], F32)
            gnbc = sbuf(pa, "gnbc", [128, 512], F32)
            kdec = sbuf(pa, "kdec", [128, 512], F32)
            qdec = sbuf(pa, "qdec", [64, 512], F32)
            dmat = sbuf(pa, "dmat", [128, 512], F32)
            gdec = sbuf(pa, "gdec", [64, 512], F32)
            wchan = k.chan("wA")
            DMA("sync", lambda e: e.dma_start(out=gbc.t[:], in_=g_attn), cA_chan, writes=[gbc])
            DMA("sync", lambda e: e.dma_start(out=gnbc.t[:], in_=g_gn), cA_chan, writes=[gnbc])
            DMA("sync", lambda e: e.dma_start(out=kdec.t[:], in_=c_kdec), cA_chan, writes=[kdec])
            DMA("sync", lambda e: e.dma_start(out=qdec.t[:], in_=c_qdec), cA_chan, writes=[qdec])
            DMA("sync", lambda e: e.dma_start(out=dmat.t[:], in_=c_dmat), cA_chan, writes=[dmat])
            DMA("sync", lambda e: e.dma_start(out=gdec.t[:], in_=c_gdec), cA_chan, writes=[gdec])
            wsbA = Buf(wsb.t, k.res("wsbA"))
            wsbB = Buf(wsb.t, k.res("wsbB"))
            wchanB = k.chan("wA2")
            for (rngs, wres, wch) in (([(0, 1024), (2048, 3396)], wsbA, wchan), ([(1024, 2048), (3396, NCOL)], wsbB, wchanB)):
                for (ra, rb) in rngs:
                    for c0 in range(ra, rb, 1024):
                        c1 = min(rb, c0 + 1024)
                        for kc in range(8):
                            DMA("gpsimd", lambda e, kc=kc, c0=c0, c1=c1: e.dma_start(
                                out=wsb.t[:, kc, c0:c1], in_=w_in[kc * 128:(kc + 1) * 128, c0:c1]), wch, writes=[wres])
            load_w_bf(wro, w_ro, 4, D, wchanB)
            k.settle(cA_chan, [b.r for b in (gbc, gnbc, kdec, qdec, dmat, gdec)])
            k.settle(wchan, [wsbA.r])
            k.settle(wchanB, [wsbB.r, wro.r])

            tstage = [sbuf(pa, f"tstage{i}", [128, 2, D], BF) for i in range(2)]
            tch_in = [k.chan(f"tin{i}") for i in range(2)]
            tch_out = [k.chan(f"tout{i}") for i in range(2)]
            r_UB = DR("UB")
            r_VB = DR("VB")
            def conv_gen():
                ci = 0
                for (src, dst, rr) in ((peer_u, UB, r_UB), (peer_v, VB, r_VB)):
                    for c in range(64 if tabconv else 0):
                        st = tstage[ci % 2]
                        sv = src[c * 256:(c + 1) * 256, :].rearrange("(r p) c -> p r c", p=128)
                        dv = dst[c * 256:(c + 1) * 256, :].rearrange("(r p) c -> p r c", p=128)
                        DMA("gpsimd", lambda e, st=st, sv=sv: e.dma_start(out=st.t[:], in_=sv), tch_in[ci % 2], writes=[st])
                        DMA("sync", lambda e, st=st, dv=dv: e.dma_start(out=dv, in_=st.t[:]), tch_out[ci % 2], reads=[st])
                        ci += 1
                        yield 1

            xbuf = [sbuf(pa, f"xbuf{i}", [128, D], F32) for i in range(2)]
            xch = [k.chan(f"x{i}") for i in range(2)]
            rtb = [sbuf(pa, f"rtb{i}", [128, 80], F32) for i in range(2)]
            rch = [k.chan(f"rt{i}") for i in range(2)]
            junkA = sbuf(pa, "junkA", [128, D], BF)
            ss = sbuf(pa, "ss", [128, 1], F32)
            std = sbuf(pa, "std", [128, 1], F32)
            rstd = sbuf(pa, "rstd", [128, 1], F32)
            hb = sbuf(pa, "hb", [128, D], BF)
            hT = [sbuf(pa, f"hT{i}", [128, D], BF) for i in range(2)]
            rtmp = sbuf(pa, "rtmp", [128, 4, 256], F32)
            RQKs = [sbuf(pa, f"RQK{i}", [128, 512], BF) for i in range(2)]
            Vt = [sbuf(pa, f"Vt{i}", [128, 512], BF) for i in range(2)]
            Kd = [sbuf(pa, f"Kd{i}", [128, 256], BF) for i in range(2)]
            S32 = [sbuf(pa, f"S32_{i}", [64, 512], F32) for i in range(5)]
            Sbf = [sbuf(pa, f"Sbf_{i}", [64, 512], BF) for i in range(5)]
            Stmp = sbuf(pa, "Stmp", [64, 512], F32)
            KTs = sbuf(pa, "KTs", [64, 512], BF)
            QTs = sbuf(pa, "QTs", [64, 512], BF)
            QW = sbuf(pa, "QW", [64, 4, 192], BF)
            sTm = sbuf(pa, "sTm", [128, 512], BF)
            gst = sbuf(pa, "gst", [128, 32], F32)
            yc = sbuf(pa, "yc", [128, 512], F32)
            ysq = yc
            sw = sbuf(pa, "sw", [128, 512], F32)
            yret = sbuf(pa, "yret", [128, 512], BF)
            yT = sbuf(pa, "yT", [128, 512], BF)
            gr = sbuf(pa, "gr", [128, D], BF)
            mst = [sbuf(pa, "mst0", [128, D], BF)]
            gdst = [sbuf(pa, "gdst0", [128, D], BF)]
            DQ = sbuf(pa, "DQ", [128, 512], BF)
            DKb = sbuf(pa, "DKb", [128, 512], BF)
            qtst = [sbuf(pa, "qtst0", [64, 1024], BF)]
            ktst = [sbuf(pa, "ktst0", [64, 1024], BF)]
            vast = [sbuf(pa, f"vast{i}", [128, 8, 65], BF) for i in range(2)]
            IQI = sbuf(pa, "IQI", [128, 320], BF)
            iqst = [sbuf(pa, f"iqst{i}", [64, 512], BF) for i in range(2)]
            iwst = [sbuf(pa, f"iwst{i}", [128, 4], F32) for i in range(2)]
            sch = {n: [k.chan(f"{n}{i}") for i in range(2)] for n in ("m", "gd", "qt", "kt", "va", "iq", "iw")}
            r_scr = DR("scratchA")

            G(lambda e: e.memset(QW.t[:], 0.0), writes=[QW])
            for i in range(2):
                G(lambda e, i=i: e.memset(vast[i].t[:], 1.0), writes=[vast[i]])
            G(lambda e: e.memset(S32[0].t[:], 0.0), writes=[S32[0]])
            G(lambda e: e.memset(Sbf[0].t[:], 0.0), writes=[Sbf[0]])

            fb_i = [0]

            def next_fb():
                fb_i[0] += 1
                return FB[fb_i[0] % 3]

            def proj(hTt, c0, ncol):
                wres = wsbA if (c0 < 1024 or 2048 <= c0 < 3396) else wsbB
                bank = next_fb()
                for kc in range(8):
                    T(lambda e, kc=kc: e.matmul(bank.t[:, 0:ncol], lhsT=hTt.t[:, kc * 128:(kc + 1) * 128],
                                               rhs=wsb.t[:, kc, c0:c0 + ncol], start=(kc == 0), stop=(kc == 7)),
                      reads=[hTt, wres], writes=[bank])
                return bank

            def rope_tok(bank, c0, H, half, rt, cos0, dst, d0):
                src3 = bank.t[:, c0:c0 + H * 64].rearrange("p (h e) -> p h e", e=64)
                dst3 = dst.t[:, d0:d0 + H * 64].rearrange("p (h e) -> p h e", e=64)
                x1 = src3[:, :, 0:half]
                x2 = src3[:, :, half:2 * half]
                cb = rt.t[:, cos0:cos0 + half].unsqueeze(1).broadcast_to([128, H, half])
                sb_ = rt.t[:, cos0 + half:cos0 + 2 * half].unsqueeze(1).broadcast_to([128, H, half])
                tm = [rtmp.t[:, i, 0:H * half].rearrange("p (h e) -> p h e", e=half) for i in range(4)]
                V(lambda e: e.tensor_tensor(out=tm[0], in0=x1, in1=cb, op=OP.mult), reads=[bank, rt], writes=[rtmp])
                V(lambda e: e.tensor_tensor(out=tm[1], in0=x2, in1=sb_, op=OP.mult), reads=[bank, rt], writes=[rtmp])
                V(lambda e: e.tensor_tensor(out=tm[2], in0=x1, in1=sb_, op=OP.mult), reads=[bank, rt], writes=[rtmp])
                V(lambda e: e.tensor_tensor(out=tm[3], in0=x2, in1=cb, op=OP.mult), reads=[bank, rt], writes=[rtmp])
                V(lambda e: e.tensor_tensor(out=dst3[:, :, 0:half], in0=tm[0], in1=tm[1], op=OP.subtract),
                  reads=[rtmp], writes=[dst])
                V(lambda e: e.tensor_tensor(out=dst3[:, :, half:2 * half], in0=tm[2], in1=tm[3], op=OP.add),
                  reads=[rtmp], writes=[dst])
                if 2 * half < 64:
                    A(lambda e: e.activation(out=dst3[:, :, 2 * half:64], in_=src3[:, :, 2 * half:64], func=AF.Copy),
                      reads=[bank], writes=[dst])

            def tileA(t):
                own = t >= 32
                tl = t % 32
                par = t % 2
                xb = xbuf[par]
                rt = rtb[par]
                RQK = RQKs[par]
                src = xo if own else xp
                DMA("sync", lambda e, xb=xb, src=src, tl=tl: e.dma_start(out=xb.t[:], in_=src[tl * 128:(tl + 1) * 128, :]),
                    xch[par], writes=[xb])
                DMA("sync", lambda e, rt=rt, t=t: e.dma_start(out=rt.t[:], in_=ropetab[t * 128:(t + 1) * 128, :]),
                    rch[par], writes=[rt])
                rmsnorm_bf(xb, gbc, hb, ss, std, rstd, junkA)
                for kc in range(8):
                    T(lambda e, kc=kc: e.transpose(T0.t[:, kc * 128:(kc + 1) * 128], hb.t[:, kc * 128:(kc + 1) * 128],
                                                   identb.t[:]), reads=[hb, identb], writes=[T0])
                hTt = hT[par]
                A(lambda e, hTt=hTt: e.activation(out=hTt.t[:], in_=T0.t[:], func=AF.Copy), reads=[T0], writes=[hTt])
                yield 1

                b0 = proj(hTt, 0, 512)
                rope_tok(b0, 0, 8, 32, rt, 0, RQK, 0)
                yield 1
                b1 = proj(hTt, 512, 512)
                vt = Vt[par]
                A(lambda e, b1=b1, vt=vt: e.activation(out=vt.t[:], in_=b1.t[:], func=AF.Copy), reads=[b1], writes=[vt])
                yield 1
                ia, ib, inx = (2 * t) % 5, (2 * t + 1) % 5, (2 * t + 2) % 5
                for c in range(2):
                    V(lambda e, c=c: e.tensor_tensor(out=Kd[c].t[:], in0=RQK.t[:, 256:512], in1=kdec.t[:, c * 256:(c + 1) * 256],
                                                     op=OP.mult), reads=[RQK, kdec], writes=[Kd[c]])
                for c, (si, so) in enumerate(((ia, ib), (ib, inx))):
                    kvb = FB[3]
                    for h in range(4):
                        T(lambda e, c=c, h=h: e.matmul(kvb.t[0:64, h * 128:(h + 1) * 128], lhsT=Kd[c].t[:, h * 64:(h + 1) * 64],
                                                       rhs=vt.t[:, h * 128:(h + 1) * 128], start=True, stop=True),
                          reads=[Kd[c], vt], writes=[kvb])
                    V(lambda e, si=si: e.tensor_tensor(out=Stmp.t[:], in0=S32[si].t[:], in1=gdec.t[:], op=OP.mult),
                      reads=[S32[si], gdec], writes=[Stmp])
                    V(lambda e, so=so: e.tensor_tensor(out=S32[so].t[:], in0=Stmp.t[:], in1=kvb.t[0:64, :], op=OP.add),
                      reads=[Stmp, kvb], writes=[S32[so]])
                    A(lambda e, so=so: e.activation(out=Sbf[so].t[:], in_=S32[so].t[:], func=AF.Copy),
                      reads=[S32[so]], writes=[Sbf[so]])
                yield 'half'

                if own:
                    tp = transpose_to(RQK, None, 8, cols=64)
                    A(lambda e, tp=tp: e.activation(out=KTs.t[:], in_=tp.t[0:64, 512:1024], func=AF.Copy), reads=[tp], writes=[KTs])
                    A(lambda e, tp=tp: e.activation(out=QTs.t[:], in_=tp.t[0:64, 0:512], func=AF.Copy), reads=[tp], writes=[QTs])
                    tq = tp.t[0:64, 0:512].rearrange("p (h e) -> p h e", e=128)
                    qd3 = qdec.t[:, :].rearrange("p (h e) -> p h e", e=128)
                    V(lambda e: e.tensor_tensor(out=QW.t[:, :, 0:64], in0=tq[:, :, 0:64], in1=qd3[:, :, 0:64], op=OP.mult),
                      reads=[tp, qdec], writes=[QW])
                    V(lambda e: e.tensor_tensor(out=QW.t[:, :, 128:192], in0=tq[:, :, 64:128], in1=qd3[:, :, 64:128], op=OP.mult),
                      reads=[tp, qdec], writes=[QW])
                    sTb = FB[3]
                    for h in range(4):
                        T(lambda e, h=h: e.matmul(sTb.t[:, h * 128:(h + 1) * 128], lhsT=KTs.t[:, h * 128:(h + 1) * 128],
                                                  rhs=QTs.t[:, h * 128:(h + 1) * 128], start=True, stop=True),
                          reads=[KTs, QTs], writes=[sTb])
                    V(lambda e: e.tensor_tensor(out=sTm.t[:], in0=sTb.t[:], in1=dmat.t[:], op=OP.mult),
                      reads=[sTb, dmat], writes=[sTm])
                    yb = FB[4]
                    for h in range(4):
                        hs = slice(h * 128, (h + 1) * 128)
                        T(lambda e, hs=hs: e.matmul(yb.t[:, hs], lhsT=sTm.t[:, hs], rhs=vt.t[:, hs], start=True, stop=False),
                          reads=[sTm, vt], writes=[yb])
                        T(lambda e, hs=hs, h=h: e.matmul(yb.t[:, hs], lhsT=QW.t[:, h, 0:128], rhs=Sbf[ia].t[:, hs], start=False, stop=False),
                          reads=[QW, Sbf[ia]], writes=[yb])
                        T(lambda e, hs=hs, h=h: e.matmul(yb.t[:, hs], lhsT=QW.t[:, h, 64:192], rhs=Sbf[ib].t[:, hs], start=False, stop=True),
                          reads=[QW, Sbf[ib]], writes=[yb])
                    yield 1
                    y3 = yb.t[:, :].rearrange("p (h e) -> p h e", e=128)
                    V(lambda e: e.tensor_reduce(out=gst.t[:, 0:4], in_=y3, axis=AX.X, op=OP.add), reads=[yb], writes=[gst])
                    A(lambda e: e.activation(out=ysq.t[:], in_=yb.t[:], func=AF.Square), reads=[yb], writes=[ysq])
                    V(lambda e: e.tensor_reduce(out=gst.t[:, 4:8], in_=ysq.t[:, :].rearrange("p (h e) -> p h e", e=128),
                                                axis=AX.X, op=OP.add), reads=[ysq], writes=[gst])
                    V(lambda e: e.tensor_scalar(out=gst.t[:, 8:12], in0=gst.t[:, 0:4], scalar1=1.0 / 128, scalar2=None, op0=OP.mult),
                      reads=[gst], writes=[gst])
                    V(lambda e: e.tensor_tensor(out=gst.t[:, 12:16], in0=gst.t[:, 8:12], in1=gst.t[:, 8:12], op=OP.mult),
                      reads=[gst], writes=[gst])
                    V(lambda e: e.scalar_tensor_tensor(out=gst.t[:, 16:20], in0=gst.t[:, 4:8], scalar=1.0 / 128, in1=gst.t[:, 12:16],
                                                       op0=OP.mult, op1=OP.subtract), reads=[gst], writes=[gst])
                    A(lambda e: e.activation(out=gst.t[:, 20:24], in_=gst.t[:, 16:20], func=AF.Sqrt, bias=epsb.t[:, 0:1]),
                      reads=[gst, epsb], writes=[gst])
                    V(lambda e: e.reciprocal(out=gst.t[:, 24:28], in_=gst.t[:, 20:24]), reads=[gst], writes=[gst])
                    yc3 = yc.t[:, :].rearrange("p (h e) -> p h e", e=128)
                    V(lambda e: e.tensor_tensor(out=yc3, in0=y3, in1=gst.t[:, 8:12].unsqueeze(2).broadcast_to([128, 4, 128]),
                                                op=OP.subtract), reads=[yb, gst], writes=[yc])
                    V(lambda e: e.tensor_tensor(out=yc3, in0=yc3, in1=gst.t[:, 24:28].unsqueeze(2).broadcast_to([128, 4, 128]),
                                                op=OP.mult), reads=[yc, gst], writes=[yc])
                    V(lambda e: e.tensor_tensor(out=yc.t[:], in0=yc.t[:], in1=gnbc.t[:], op=OP.mult), reads=[yc, gnbc], writes=[yc])
                    yield 1
                    b2 = proj(hTt, 1024, 512)
                    A(lambda e, b2=b2: e.activation(out=sw.t[:], in_=b2.t[:], func=AF.Silu), reads=[b2], writes=[sw])
                    V(lambda e: e.tensor_tensor(out=yret.t[:], in0=yc.t[:], in1=sw.t[:], op=OP.mult), reads=[yc, sw], writes=[yret])
                    tp = transpose_to(yret, None, 4)
                    A(lambda e, tp=tp: e.activation(out=yT.t[:], in_=tp.t[:, 0:512], func=AF.Copy), reads=[tp], writes=[yT])
                    ms = mst[0]
                    for nb in range(2):
                        bg = proj(hTt, 3456 + nb * 512, 512)
                        A(lambda e, bg=bg, nb=nb: e.activation(out=gr.t[:, nb * 512:(nb + 1) * 512], in_=bg.t[:], func=AF.Sigmoid),
                          reads=[bg], writes=[gr])
                        bm = next_fb()
                        for kc in range(4):
                            T(lambda e, kc=kc, nb=nb, bm=bm: e.matmul(bm.t[:, :], lhsT=yT.t[:, kc * 128:(kc + 1) * 128],
                                                                      rhs=wro.t[:, kc, nb * 512:(nb + 1) * 512],
                                                                      start=(kc == 0), stop=(kc == 3)), reads=[yT, wro], writes=[bm])
                        V(lambda e, nb=nb, bm=bm, ms=ms: e.tensor_tensor(out=ms.t[:, nb * 512:(nb + 1) * 512], in0=bm.t[:],
                                                                         in1=gr.t[:, nb * 512:(nb + 1) * 512], op=OP.mult),
                          reads=[bm, gr], writes=[ms])
                    DMA("sync", lambda e, ms=ms, tl=tl: e.dma_start(out=MRET[tl * 128:(tl + 1) * 128, :], in_=ms.t[:]),
                        sch["m"][0], reads=[ms])
                    yield 1
                    gd = gdst[0]
                    for nb in range(2):
                        bg = proj(hTt, 4480 + nb * 512, 512)
                        A(lambda e, bg=bg, nb=nb, gd=gd: e.activation(out=gd.t[:, nb * 512:(nb + 1) * 512], in_=bg.t[:], func=AF.Sigmoid),
                          reads=[bg], writes=[gd])
                    DMA("sync", lambda e, gd=gd, tl=tl: e.dma_start(out=GDSA[tl * 128:(tl + 1) * 128, :], in_=gd.t[:]),
                        sch["gd"][0], reads=[gd])
                    yield 1
                    b3 = proj(hTt, 1536, 512)
                    rope_tok(b3, 0, 8, 8, rt, 64, DQ, 0)
                    tp = transpose_to(DQ, None, 8, cols=64)
                    qs = qtst[0]
                    A(lambda e, tp=tp, qs=qs: e.activation(out=qs.t[:], in_=tp.t[0:64, :], func=AF.Copy), reads=[tp], writes=[qs])
                    DMA("sync", lambda e, qs=qs, tl=tl: e.dma_start(out=QTD[tl], in_=qs.t[:]), sch["qt"][0], reads=[qs])
                    yield 1

                b4 = proj(hTt, 2048, 512)
                rope_tok(b4, 0, 8, 8, rt, 64, DKb, 0)
                tp = transpose_to(DKb, None, 8, cols=64)
                ks = ktst[0]
                A(lambda e, tp=tp, ks=ks: e.activation(out=ks.t[:], in_=tp.t[0:64, :], func=AF.Copy), reads=[tp], writes=[ks])
                DMA("sync", lambda e, ks=ks, t=t: e.dma_start(out=KTD[:, :, t * 128:(t + 1) * 128],
                                                             in_=ks.t[:, :].rearrange("p (h e) -> p h e", e=128)),
                    sch["kt"][0], reads=[ks])
                yield 1
                b5 = proj(hTt, 2560, 512)
                va = vast[par]
                A(lambda e, b5=b5, va=va: e.activation(out=va.t[:, :, 0:64], in_=b5.t[:, :].rearrange("p (h e) -> p h e", e=64),
                                                       func=AF.Copy), reads=[b5], writes=[va])
                DMA("sync", lambda e, va=va, t=t: e.dma_start(out=VD[t * 128:(t + 1) * 128, :],
                                                             in_=va.t[:, :, :].rearrange("p h e -> p (h e)")),
                    sch["va"][par], reads=[va])
                yield 1
                b6 = proj(hTt, 3072, 324)
                rope_tok(b6, 0, 5, 8, rt, 64, IQI, 0)
                tp = transpose_to(IQI, None, 5, cols=64)
                A(lambda e, tp=tp, t=t: e.activation(out=IKT.t[:, t * 128:(t + 1) * 128], in_=tp.t[0:64, 512:640], func=AF.Copy),
                  reads=[tp], writes=[IKT])
                if own:
                    iqs = iqst[par]
                    A(lambda e, tp=tp, iqs=iqs: e.activation(out=iqs.t[:], in_=tp.t[0:64, 0:512], func=AF.Copy), reads=[tp], writes=[iqs])
                    DMA("sync", lambda e, iqs=iqs, tl=tl: e.dma_start(out=IQTD[tl], in_=iqs.t[:]), sch["iq"][par], reads=[iqs])
                    iws = iwst[par]
                    V(lambda e, b6=b6, iws=iws: e.tensor_scalar(out=iws.t[:], in0=b6.t[:, 320:324], scalar1=1.0 / 16, scalar2=None, op0=OP.mult),
                      reads=[b6], writes=[iws])
                    DMA("sync", lambda e, iws=iws, tl=tl: e.dma_start(out=IWD[tl * 128:(tl + 1) * 128, :], in_=iws.t[:]),
                        sch["iw"][par], reads=[iws])
            prevA = None
            cgen = conv_gen()
            for ti_, t in enumerate(tiles_a if tiles_a is not None else range(64)):
                if ti_ >= 12:
                    for _ in range(3):
                        next(cgen, None)
                cur = tileA(t)
                while True:
                    r = next(cur)
                    if prevA is not None and next(prevA, "end") == "end":
                        prevA = None
                    if r == "half":
                        break
                if prevA is not None:
                    for _ in prevA:
                        pass
                prevA = cur
            for _ in prevA:
                pass
            for _ in cgen:
                pass
            k.barrier()
            k.flush()

        with ExitStack() as pb:
            if only_a:
                return nc
            wdo = sbuf(pb, "wdo", [128, 4, D], BF)
            wout = sbuf(pb, "wout", [128, 8, D], BF)
            wq = sbuf(pb, "wq", [128, 8, 2048], BF)
            skT = sbuf(pb, "skT", [128, 16, 128], BF)
            g2bc = sbuf(pb, "g2bc", [128, D], F32)
            gfbc = sbuf(pb, "gfbc", [128, D], F32)
            diag = sbuf(pb, "diag", [128, 2048], BF)
            pbias = sbuf(pb, "pbias", [128, 1], F32)
            iota = sbuf(pb, "iota", [128, 16], F32)
            cpow = sbuf(pb, "cpow", [128, NIT], F32)
            cnb = sbuf(pb, "cnb", [128, NT], F32)
            c05 = sbuf(pb, "c05", [128, 1], F32)
            nbig = sbuf(pb, "nbig", [128, 1], F32)
            dummy = sbuf(pb, "fdummy", [128, 8], F32)
            POOLW = 10240
            pool_t = pb.enter_context(nc.sbuf_tensor("pool", [128, POOLW], F32))

            def view(name, w0, w1, dt=None, shape=None):
                ap = pool_t[:, w0:w1]
                if dt is not None:
                    ap = ap.bitcast(dt)
                return Buf(ap, k.res(name))

            isc = view("isc", 0, 8192)
            junkB = view("junkB", 8192, 10240, mybir.dt.int8)
            skr = view("skr", 0, 1024, BF)
            Sc = view("Sc", 0, 2048)
            cand = view("cand", 2048, 4096)
            qT = view("qT", 4096, 5120, BF)
            oh = view("oh", 5120, 6144, BF)
            tmp2 = view("tmp2", 6144, 6656)
            Sc2 = view("Sc2", 8192, 10240)
            pf = view("pf", 6656, 7424)
            tif = view("tif", 7424, 7680)
            isel = view("isel", 7680, 7936)
            ef = view("ef", 7936, 8064)
            e0 = view("e0", 8064, 8192)
            ubuf = [sbuf(pb, f"gbuf{i}", [128, D], BF) for i in range(2 * NU)]
            vbuf = ubuf
            dsa_views = [isc, junkB]
            peer_views = [Sc, cand, qT, oh, tmp2, pf, tif, isel, ef, e0, Sc2]

            def fence(frm, to):
                V(lambda e: e.memset(dummy.t[:, 0:1], 0.0), writes=list(frm) + list(to) + [dummy])

            wchB = k.chan("wB")
            cchB = k.chan("cB")
            cl = ((g2bc, g_ffn), (gfbc, g_fin), (pbias, c_pbias), (iota, c_iota), (cpow, c_pow), (cnb, c_nb))
            for dst, srcw in cl:
                DMA("sync", lambda e, dst=dst, srcw=srcw: e.dma_start(out=dst.t[:], in_=srcw), cchB, writes=[dst])
            k.settle(cchB, [d.r for d, _ in cl])
            DMA("gpsimd", lambda e: e.dma_start(out=diag.t[:], in_=c_diag), wchB, writes=[diag])
            load_w_bf(wdo, w_do, 4, D, wchB)
            load_w_bf(wout, w_out, 8, D, wchB)
            load_w_bf(wq, w_q, 8, 2048, wchB)
            skr3 = skr.t.rearrange("p (c d) -> p c d", d=128)
            DMA("gpsimd", lambda e: e.dma_start(out=skr3, in_=subk.rearrange("c k d -> k c d")), wchB, writes=[skr])
            k.settle(wchB, [wdo.r, wout.r, wq.r, skr.r, diag.r])
            V(lambda e: e.memset(c05.t[:], 0.5), writes=[c05])
            V(lambda e: e.memset(nbig.t[:], -30000.0), writes=[nbig])
            for q4 in range(4):
                tp = next_t1()
                for j in range(4):
                    cb = q4 * 4 + j
                    T(lambda e, cb=cb, j=j, tp=tp: e.transpose(tp.t[:, j * 128:(j + 1) * 128], skr3[:, cb, :], identb.t[:]),
                      reads=[skr, identb], writes=[tp])
                A(lambda e, q4=q4, tp=tp: e.activation(out=skT.t[:, q4 * 4:(q4 + 1) * 4, :].rearrange("p c k -> p (c k)"),
                                                       in_=tp.t[:, 0:512], func=AF.Copy), reads=[tp], writes=[skT])
            fence([skr], dsa_views + peer_views)

            x2s = [sbuf(pb, f"x2_{i}", [128, D], F32) for i in range(2)]
            qt_ = sbuf(pb, "qtk", [64, 1024], BF)
            iq_ = sbuf(pb, "iqk", [64, 512], BF)
            iw_ = sbuf(pb, "iwk", [128, 4], F32)
            mr_ = sbuf(pb, "mrk", [128, D], BF)
            gd_ = sbuf(pb, "gdk", [128, D], BF)
            lch = {n: k.chan(f"L{n}") for n in ("qt", "iq", "iw", "mr", "gd")}
            xlch = [k.chan(f"Lx{i}", serial=True) for i in range(2)]
            rel = [sbuf(pb, f"rel{i}", [128, 512], F32) for i in range(2)]
            bst = sbuf(pb, "bst", [128, 16], F32)
            nhd = sbuf(pb, "nhd", [128, NIT], F32)
            sS = sbuf(pb, "sS", [128, 2], F32)
            ind = sbuf(pb, "ind", [128, 2], F32)
            nmid = [sbuf(pb, f"nmid{i}", [128, 1], F32) for i in range(2)]
            lo = sbuf(pb, "lo", [128, 1], F32)
            KTg = [sbuf(pb, f"KTg{i}", [64, 8, 256], BF) for i in range(2)]
            Vg = [sbuf(pb, f"Vg{i}", [128, 2, 520], BF) for i in range(2)]
            kch = [k.chan(f"ktg{i}") for i in range(2)]
            vch = [k.chan(f"vg{i}") for i in range(2)]
            mk = [sbuf(pb, f"mk{i}", [128, 256], BF) for i in range(4)]
            mT = [sbuf(pb, f"mT{i}", [128, 256], BF) for i in range(4)]
            Eb = [sbuf(pb, f"Eb{i}", [128, 512], BF) for i in range(2)]
            rden = sbuf(pb, "rden", [128, 8], F32)
            yd = sbuf(pb, "yd", [128, 512], BF)
            xT = sbuf(pb, "xT", [128, D], BF)
            mtmp = sbuf(pb, "mtmp", [128, 512], F32)
            mg = sbuf(pb, "mg", [128, D], BF)
            junkX = Buf(mtmp.t[:, :].bitcast(BF), mtmp.r)
            h2s = [sbuf(pb, f"h2_{i}", [128, D], BF) for i in range(2)]
            ss3 = sbuf(pb, "ss3", [128, 1], F32)
            std3 = sbuf(pb, "std3", [128, 1], F32)
            rstd3 = sbuf(pb, "rstd3", [128, 1], F32)
            ss2 = sbuf(pb, "ss2", [128, 1], F32)
            std2 = sbuf(pb, "std2", [128, 1], F32)
            rstd2 = sbuf(pb, "rstd2", [128, 1], F32)
            tv = sbuf(pb, "tv", [128, 16, 16], F32)
            ti = sbuf(pb, "ti", [128, 16, 16], U32)
            bv = sbuf(pb, "bv", [128, 8, 16], F32)
            bi = sbuf(pb, "bi", [128, 8, 16], U32)
            pi_ = sbuf(pb, "pi", [128, 128], I32)
            eidxs = [sbuf(pb, f"eidx{i}", [128, 128], I32) for i in range(2)]
            gs = sbuf(pb, "gs", [128, 16], F32)
            gsms = [sbuf(pb, f"gsm{i}", [128, 128], F32) for i in range(2)]
            av = sbuf(pb, "av", [128, 128], F32)
            wgt = sbuf(pb, "wgt", [128, 128], F32)
            junkD = sbuf(pb, "junkD", [128, D], BF)
            uch = [k.chan(f"u{i}", serial=True) for i in range(2 * NU)]
            vch2 = uch
            dg = [sbuf(pb, f"dg{i}", [128, 128], BF) for i in range(4)]
            ochs = [k.chan(f"o{i}", serial=True) for i in range(2)]
            t1_i[0] = 0
            T1.pop()
            Fv = [FB[4], Buf(T1x.t[:, :].bitcast(F32), T1x.r)]
            if debug:
                dbgch = k.chan("dbg")
                dbt = sbuf(pb, "dbt", [128, D], F32)

            Sc3 = Sc.t.rearrange("p (c k) -> p c k", k=128)
            cand3 = cand.t.rearrange("p (h k) -> p h k", k=256)
            qT3 = qT.t.rearrange("p (c k) -> p c k", k=128)
            tmp23 = tmp2.t.rearrange("p (i k) -> p i k", k=256)
            pf3 = pf.t.rearrange("p (i k) -> p i k", k=128)
            isel3 = isel.t.rearrange("p (c k) -> p c k", k=128)

            def stage_X(kt):
                x2 = x2s[kt % 2]; h2 = h2s[kt % 2]; eidx = eidxs[kt % 2]; gsm = gsms[kt % 2]
                G_k = 9 + kt // 4
                N_k = 512 * G_k
                NB_k = 33 + kt
                r = kt % 4
                rows = slice(kt * 128, (kt + 1) * 128)
                fence(peer_views, dsa_views)
                DMA("sync", lambda e, rows=rows: e.dma_start(out=x2.t[:], in_=xo[rows, :]), xlch[kt % 2], writes=[x2])
                DMA("sync", lambda e, kt=kt: e.dma_start(out=qt_.t[:], in_=QTD[kt]), lch["qt"], writes=[qt_])
                DMA("sync", lambda e, kt=kt: e.dma_start(out=iq_.t[:], in_=IQTD[kt]), lch["iq"], writes=[iq_])
                DMA("sync", lambda e, rows=rows: e.dma_start(out=iw_.t[:], in_=IWD[rows, :]), lch["iw"], writes=[iw_])
                DMA("sync", lambda e, rows=rows: e.dma_start(out=mr_.t[:], in_=MRET[rows, :]), lch["mr"], writes=[mr_])
                DMA("sync", lambda e, rows=rows: e.dma_start(out=gd_.t[:], in_=GDSA[rows, :]), lch["gd"], writes=[gd_])

                ri = 0
                for g in range(G_k):
                    gs_ = slice(g * 512, (g + 1) * 512)
                    for h in range(4):
                        bank = FB[(g * 4 + h) % 2]
                        T(lambda e, bank=bank, h=h, gs_=gs_: e.matmul(bank.t[:, :], lhsT=iq_.t[:, h * 128:(h + 1) * 128],
                                                                      rhs=IKT.t[:, gs_], start=True, stop=True),
                          reads=[iq_, IKT], writes=[bank])
                        rl = rel[ri % 2]
                        ri += 1
                        A(lambda e, bank=bank, rl=rl: e.activation(out=rl.t[:], in_=bank.t[:], func=AF.Relu), reads=[bank], writes=[rl])
                        if h == 0:
                            V(lambda e, rl=rl, gs_=gs_: e.tensor_scalar(out=isc.t[:, gs_], in0=rl.t[:], scalar1=iw_.t[:, 0:1],
                                                                        scalar2=None, op0=OP.mult), reads=[rl, iw_], writes=[isc])
                        else:
                            V(lambda e, rl=rl, gs_=gs_, h=h: e.scalar_tensor_tensor(
                                out=isc.t[:, gs_], in0=rl.t[:], scalar=iw_.t[:, h:h + 1], in1=isc.t[:, gs_], op0=OP.mult, op1=OP.add),
                              reads=[rl, iw_, isc], writes=[isc])
                    yield 1.6
                V(lambda e, N_k=N_k: e.tensor_reduce(out=bst.t[:, 0:1], in_=isc.t[:, 0:N_k], axis=AX.X, op=OP.max,
                                                     apply_absolute_value=True), reads=[isc], writes=[bst])
                V(lambda e: e.tensor_scalar(out=bst.t[:, 1:2], in0=bst.t[:, 0:1], scalar1=2.0, scalar2=1.0, op0=OP.mult, op1=OP.add),
                  reads=[bst], writes=[bst])
                V(lambda e: e.tensor_scalar(out=nhd.t[:], in0=cpow.t[:], scalar1=bst.t[:, 1:2], scalar2=None, op0=OP.mult),
                  reads=[cpow, bst], writes=[nhd])
                V(lambda e: e.tensor_scalar(out=isc.t[:, 0:LH], in0=isc.t[:, 0:LH], scalar1=pbias.t[:, 0:1], scalar2=None, op0=OP.add),
                  reads=[isc, pbias], writes=[isc])
                V(lambda e, N_k=N_k, r=r: e.tensor_tensor(out=isc.t[:, N_k - 512:N_k], in0=isc.t[:, N_k - 512:N_k],
                                                          in1=diag.t[:, r * 512:(r + 1) * 512], op=OP.add), reads=[isc, diag], writes=[isc])
                cur = c05
                for it in range(NIT):
                    A(lambda e, N_k=N_k, cur=cur: e.activation(out=junkB.t[:, 0:N_k], in_=isc.t[:, 0:N_k], func=AF.Sign,
                                                               bias=cur.t[:, 0:1], accum_out=sS.t[:, 0:1]),
                      reads=[isc, cur], writes=[junkB, sS])
                    A(lambda e, kt=kt: e.activation(out=ind.t[:, 0:1], in_=sS.t[:, 0:1], func=AF.Sign, bias=cnb.t[:, kt:kt + 1]),
                      reads=[sS, cnb], writes=[ind])
                    nxt = nmid[it % 2]
                    A(lambda e, it=it, cur=cur, nxt=nxt: e.activation(out=nxt.t[:], in_=ind.t[:, 0:1], func=AF.Identity,
                                                                      scale=nhd.t[:, it:it + 1], bias=cur.t[:, 0:1]),
                      reads=[ind, nhd, cur], writes=[nxt])
                    cur = nxt
                    yield 6.9
                A(lambda e, cur=cur: e.activation(out=lo.t[:], in_=cur.t[:], func=AF.Identity, scale=-1.0, bias=nhd.t[:, NIT - 1:NIT]),
                  reads=[cur, nhd], writes=[lo])
                if debug:
                    V(lambda e: e.tensor_copy(out=dbt.t[:, 0:1], in_=lo.t[:]), reads=[lo], writes=[dbt])
                    V(lambda e: e.tensor_copy(out=dbt.t[:, 1:3], in_=bst.t[:, 0:2]), reads=[bst], writes=[dbt])
                    V(lambda e: e.tensor_copy(out=dbt.t[:, 3:4], in_=sS.t[:, 0:1]), reads=[sS], writes=[dbt])
                    DMA("sync", lambda e, rows=rows: e.dma_start(out=DBG_LO[rows, :], in_=dbt.t[:, 0:4]), dbgch, reads=[dbt])
                    k.wait_all("vector", chans=[dbgch], engines=False)

                Fo = [FB[2], FB[3]]
                for hg in range(2):
                    T(lambda e, hg=hg: e.matmul(Fo[hg].t[:, 0:260], lhsT=zerob.t[:, 0:128], rhs=zerob.t[:, 0:260], start=True, stop=False),
                      reads=[zerob], writes=[Fo[hg]])
                ngrp = (NB_k + 1) // 2
                grp = {}

                gmask = {}

                def pre(g):
                    gp = g % 2
                    ktg, vg = KTg[gp], Vg[gp]
                    gs_ = slice(g * 256, (g + 1) * 256)
                    DMA("sync", lambda e: e.dma_start(out=ktg.t[:], in_=KTD[:, :, gs_]), kch[gp], writes=[ktg])
                    DMA("sync", lambda e: e.dma_start(out=vg.t[:], in_=VD[gs_, :].rearrange("(b p) c -> p b c", p=128)),
                        vch[gp], writes=[vg])
                    grp[g] = (ktg, vg)

                def pre_mask(g):
                    gs_ = slice(g * 256, (g + 1) * 256)
                    mkb = mk[g % 4]
                    V(lambda e: e.tensor_scalar(out=mkb.t[:], in0=isc.t[:, gs_], scalar1=lo.t[:, 0:1], scalar2=None, op0=OP.is_gt),
                      reads=[isc, lo], writes=[mkb])
                    nb_here = min(2, NB_k - g * 2)
                    for b in range(nb_here):
                        T(lambda e, b=b: e.transpose(T0.t[:, b * 128:(b + 1) * 128], mkb.t[:, b * 128:(b + 1) * 128], identb.t[:]),
                          reads=[mkb, identb], writes=[T0])
                    mTb = mT[g % 4]
                    A(lambda e: e.activation(out=mTb.t[:, 0:nb_here * 128], in_=T0.t[:, 0:nb_here * 128], func=AF.Identity,
                                             scale=30000.0, bias=nbig.t[:, 0:1]), reads=[T0, nbig], writes=[mTb])
                    gmask[g] = mTb

                units = [(g, b, hg) for g in range(ngrp) for b in range(min(2, NB_k - g * 2)) for hg in range(2)]

                def qk(ui):
                    g, b, hg = units[ui]
                    ktg, vg = grp[g]
                    mTb = gmask[g]
                    Fs = FB[ui % 2]
                    for h4 in range(4):
                        T(lambda e, h4=h4: e.matmul(Fs.t[:, h4 * 128:(h4 + 1) * 128], lhsT=identb.t[:], rhs=mTb.t[:, b * 128:(b + 1) * 128],
                                                    start=(h4 == 0), stop=False), reads=[identb, mTb], writes=[Fs])
                    for h4 in range(4):
                        hh = hg * 4 + h4
                        T(lambda e, h4=h4, hh=hh: e.matmul(
                            Fs.t[:, h4 * 128:(h4 + 1) * 128], lhsT=ktg.t[:, hh, b * 128:(b + 1) * 128],
                            rhs=qt_.t[:, hh * 128:(hh + 1) * 128], start=False, stop=(h4 == 3)), reads=[ktg, qt_], writes=[Fs])

                def smpv(ui):
                    g, b, hg = units[ui]
                    ktg, vg = grp[g]
                    Fs = FB[ui % 2]
                    eb = Eb[ui % 2]
                    last_blk = (g * 2 + b == NB_k - 1)
                    A(lambda e: e.activation(out=eb.t[:], in_=Fs.t[:], func=AF.Exp, scale=0.125), reads=[Fs], writes=[eb])
                    for h4 in range(4):
                        hh = hg * 4 + h4
                        T(lambda e, h4=h4, hh=hh: e.matmul(
                            Fo[hg].t[:, h4 * 65:(h4 + 1) * 65], lhsT=eb.t[:, h4 * 128:(h4 + 1) * 128],
                            rhs=vg.t[:, b, hh * 65:(hh + 1) * 65], start=False, stop=(last_blk and h4 == 3)),
                          reads=[eb, vg], writes=[Fo[hg]])

                pre(0)
                if ngrp > 1:
                    pre(1)
                for g_ in range(min(3, ngrp)):
                    pre_mask(g_)
                qk(0)
                for ui in range(len(units)):
                    g, b, hg = units[ui]
                    if ui + 1 < len(units):
                        qk(ui + 1)
                    smpv(ui)
                    if ui + 1 == len(units) or units[ui + 1][0] != g:
                        if g + 2 < ngrp:
                            pre(g + 2)
                        if g + 3 < ngrp:
                            pre_mask(g + 3)
                    yield 2.5
                for hg in range(2):
                    fo3 = Fo[hg].t[:, 0:260].rearrange("p (h e) -> p h e", e=65)
                    V(lambda e, hg=hg, fo3=fo3: e.reciprocal(out=rden.t[:, hg * 4:(hg + 1) * 4].unsqueeze(2), in_=fo3[:, :, 64:65]),
                      reads=[Fo[hg]], writes=[rden])
                    V(lambda e, hg=hg, fo3=fo3: e.tensor_tensor(
                        out=yd.t[:, hg * 256:(hg + 1) * 256].rearrange("p (h e) -> p h e", e=64), in0=fo3[:, :, 0:64],
                        in1=rden.t[:, hg * 4:(hg + 1) * 4].unsqueeze(2).broadcast_to([128, 4, 64]), op=OP.mult),
                      reads=[Fo[hg], rden], writes=[yd])
                if debug:
                    V(lambda e: e.tensor_copy(out=dbt.t[:, 0:512], in_=yd.t[:]), reads=[yd], writes=[dbt])
                    DMA("sync", lambda e, rows=rows: e.dma_start(out=DBG_YD[rows, :], in_=dbt.t[:, 0:512]), dbgch, reads=[dbt])
                    k.wait_all("vector", chans=[dbgch], engines=False)
                fence(dsa_views, peer_views)
                yield 3.0
                tp = transpose_to(yd, None, 4)
                A(lambda e, tp=tp: e.activation(out=xT.t[:, 0:512], in_=tp.t[:, 0:512], func=AF.Copy), reads=[tp], writes=[xT])
                for nb in range(2):
                    ns = slice(nb * 512, (nb + 1) * 512)
                    bm = FB[nb]
                    for kc in range(4):
                        T(lambda e, kc=kc, ns=ns, bm=bm: e.matmul(bm.t[:, :], lhsT=xT.t[:, kc * 128:(kc + 1) * 128], rhs=wdo.t[:, kc, ns],
                                                                  start=(kc == 0), stop=(kc == 3)), reads=[xT, wdo], writes=[bm])
                    V(lambda e, ns=ns, bm=bm: e.tensor_tensor(out=mtmp.t[:], in0=bm.t[:], in1=gd_.t[:, ns], op=OP.mult),
                      reads=[bm, gd_], writes=[mtmp])
                    V(lambda e, ns=ns: e.tensor_tensor(out=mg.t[:, ns], in0=mtmp.t[:], in1=mr_.t[:, ns], op=OP.add),
                      reads=[mtmp, mr_], writes=[mg])
                tp = transpose_to(mg, None, 8)
                A(lambda e, tp=tp: e.activation(out=xT.t[:], in_=tp.t[:], func=AF.Copy), reads=[tp], writes=[xT])
                for nb in range(2):
                    ns = slice(nb * 512, (nb + 1) * 512)
                    bm = FB[nb]
                    for kc in range(8):
                        T(lambda e, kc=kc, ns=ns, bm=bm: e.matmul(bm.t[:, :], lhsT=xT.t[:, kc * 128:(kc + 1) * 128], rhs=wout.t[:, kc, ns],
                                                                  start=(kc == 0), stop=(kc == 7)), reads=[xT, wout], writes=[bm])
                    V(lambda e, ns=ns, bm=bm: e.tensor_tensor(out=x2.t[:, ns], in0=bm.t[:], in1=x2.t[:, ns], op=OP.add),
                      reads=[bm, x2], writes=[x2])
                if debug:
                    DMA("sync", lambda e, rows=rows: e.dma_start(out=DBG_X2[rows, :], in_=x2.t[:]), dbgch, reads=[x2])
                    k.wait_all("vector", chans=[dbgch], engines=False)

                yield 25.0
                rmsnorm_bf(x2, g2bc, h2, ss2, std2, rstd2, junkX)
                tp = transpose_to(h2, None, 8)
                A(lambda e, tp=tp: e.activation(out=xT.t[:], in_=tp.t[:], func=AF.Copy), reads=[tp], writes=[xT])
                for q4 in range(4):
                    bank = FB[q4 % 2]
                    for j in range(4):
                        cb = q4 * 4 + j
                        for kc in range(8):
                            T(lambda e, bank=bank, j=j, cb=cb, kc=kc: e.matmul(bank.t[:, j * 128:(j + 1) * 128],
                                                                              lhsT=wq.t[:, kc, cb * 128:(cb + 1) * 128],
                                                                              rhs=xT.t[:, kc * 128:(kc + 1) * 128],
                                                                              start=(kc == 0), stop=(kc == 7)), reads=[wq, xT], writes=[bank])
                    A(lambda e, bank=bank, q4=q4: e.activation(out=qT.t[:, q4 * 512:(q4 + 1) * 512], in_=bank.t[:], func=AF.Copy),
                      reads=[bank], writes=[qT])
                for q4 in range(4):
                    bank = FB[q4 % 2]
                    for j in range(4):
                        cb = q4 * 4 + j
                        T(lambda e, bank=bank, j=j, cb=cb: e.matmul(bank.t[:, j * 128:(j + 1) * 128], lhsT=qT3[:, cb, :], rhs=skT.t[:, cb, :],
                                                                    start=True, stop=True), reads=[qT, skT], writes=[bank])
                    A(lambda e, bank=bank, q4=q4: e.activation(out=Sc.t[:, q4 * 512:(q4 + 1) * 512], in_=bank.t[:], func=AF.Copy),
                      reads=[bank], writes=[Sc])

                def top16(vals, vals3, n, w, outv, outi):
                    v2 = Sc2.t.rearrange("p (c k) -> p c k", k=w)
                    for c in range(n):
                        V(lambda e, c=c: e.max(out=outv.t[:, c, 0:8], in_=vals3[:, c, :]), reads=[vals], writes=[outv])
                    yield 0.45 * n
                    for c in range(n):
                        V(lambda e, c=c: e.max_index(out=outi.t[:, c, 0:8], in_max=outv.t[:, c, 0:8], in_values=vals3[:, c, :]),
                          reads=[vals, outv], writes=[outi])
                    yield 0.45 * n
                    for c in range(n):
                        V(lambda e, c=c: e.match_replace(out=v2[:, c, :], in_to_replace=outv.t[:, c, 0:8], in_values=vals3[:, c, :],
                                                         imm_value=-BIG), reads=[vals, outv], writes=[Sc2])
                    yield 0.45 * n
                    for c in range(n):
                        V(lambda e, c=c: e.max(out=outv.t[:, c, 8:16], in_=v2[:, c, :]), reads=[Sc2], writes=[outv])
                    yield 0.45 * n
                    for c in range(n):
                        V(lambda e, c=c: e.max_index(out=outi.t[:, c, 8:16], in_max=outv.t[:, c, 8:16], in_values=v2[:, c, :]),
                          reads=[Sc2, outv], writes=[outi])
                    yield 0.45 * n

                yield from top16(Sc, Sc3, 16, 128, tv, ti)
                tv4 = tv.t[:, :, :].rearrange("p (h c) k -> p h c k", c=2)
                V(lambda e: e.tensor_tensor(out=cand3.rearrange("p h (a b) -> p h a b", b=16),
                                            in0=tv4[:, :, 0, :].unsqueeze(3).broadcast_to([128, 8, 16, 16]),
                                            in1=tv4[:, :, 1, :].unsqueeze(2).broadcast_to([128, 8, 16, 16]), op=OP.add),
                  reads=[tv], writes=[cand])
                yield from top16(cand, cand3, 8, 256, bv, bi)
                bif = bi.t[:, :, :].rearrange("p h k -> p (h k)")
                V(lambda e: e.tensor_copy(out=pf3[:, 0, :], in_=bif), reads=[bi], writes=[pf])
                V(lambda e: e.tensor_scalar(out=pi_.t[:], in0=pf3[:, 0, :], scalar1=1.0 / 16, scalar2=None, op0=OP.mult),
                  reads=[pf], writes=[pi_])
                V(lambda e: e.tensor_copy(out=pf3[:, 1, :], in_=pi_.t[:]), reads=[pi_], writes=[pf])
                V(lambda e: e.scalar_tensor_tensor(out=pf3[:, 2, :], in0=pf3[:, 1, :], scalar=-16.0, in1=pf3[:, 0, :], op0=OP.mult, op1=OP.add),
                  reads=[pf], writes=[pf])
                V(lambda e: e.tensor_scalar(out=pf3[:, 3, :], in0=pf3[:, 2, :], scalar1=0.0, scalar2=None, op0=OP.is_lt),
                  reads=[pf], writes=[pf])
                V(lambda e: e.tensor_tensor(out=pf3[:, 4, :], in0=pf3[:, 1, :], in1=pf3[:, 3, :], op=OP.subtract),
                  reads=[pf], writes=[pf])
                V(lambda e: e.scalar_tensor_tensor(out=pf3[:, 5, :], in0=pf3[:, 4, :], scalar=-16.0, in1=pf3[:, 0, :], op0=OP.mult, op1=OP.add),
                  reads=[pf], writes=[pf])
                V(lambda e: e.tensor_copy(out=tif.t[:, :], in_=ti.t[:, :, :].rearrange("p c k -> p (c k)")), reads=[ti], writes=[tif])
                tif4 = tif.t.rearrange("p (h c k) -> p h c k", c=2, k=16)
                oh4 = oh.t.rearrange("p (h k a) -> p h k a", k=16, a=16)
                for c in range(2):
                    V(lambda e, c=c: e.tensor_tensor(out=oh4, in0=pf3[:, 4 + c, :].rearrange("p (h k) -> p h k", k=16).unsqueeze(3).broadcast_to([128, 8, 16, 16]),
                                                     in1=iota.t[:, :].unsqueeze(1).unsqueeze(1).broadcast_to([128, 8, 16, 16]), op=OP.is_equal),
                      reads=[pf, iota], writes=[oh])
                    V(lambda e, c=c: e.tensor_tensor(out=oh4, in0=oh4, in1=tif4[:, :, c, :].unsqueeze(2).broadcast_to([128, 8, 16, 16]), op=OP.mult),
                      reads=[oh, tif], writes=[oh])
                    V(lambda e, c=c: e.tensor_reduce(out=isel3[:, c, :], in_=oh.t.rearrange("p (hk a) -> p hk a", a=16),
                                                     axis=AX.X, op=OP.add), reads=[oh], writes=[isel])
                V(lambda e: e.scalar_tensor_tensor(out=ef.t[:, :], in0=isel3[:, 0, :], scalar=128.0, in1=isel3[:, 1, :], op0=OP.mult, op1=OP.add),
                  reads=[isel], writes=[ef])
                V(lambda e: e.tensor_copy(out=eidx.t[:], in_=ef.t[:, :]), reads=[ef], writes=[eidx])
                V(lambda e: e.tensor_tensor(out=e0.t.rearrange("p (h k) -> p h k", k=16), in0=bv.t[:, :, :],
                                            in1=bv.t[:, :, 0:1].broadcast_to([128, 8, 16]), op=OP.subtract), reads=[bv], writes=[e0])
                A(lambda e: e.activation(out=e0.t[:, :], in_=e0.t[:, :], func=AF.Exp), reads=[e0], writes=[e0])
                V(lambda e: e.tensor_reduce(out=gs.t[:, 0:8], in_=e0.t.rearrange("p (h k) -> p h k", k=16), axis=AX.X, op=OP.add),
                  reads=[e0], writes=[gs])
                V(lambda e: e.reciprocal(out=gs.t[:, 8:16], in_=gs.t[:, 0:8]), reads=[gs], writes=[gs])
                V(lambda e: e.tensor_tensor(out=gsm.t[:, :].rearrange("p (h k) -> p h k", k=16), in0=e0.t.rearrange("p (h k) -> p h k", k=16),
                                            in1=gs.t[:, 8:16].unsqueeze(2).broadcast_to([128, 8, 16]), op=OP.mult), reads=[e0, gs], writes=[gsm])
                if debug:
                    DMA("sync", lambda e, rows=rows: e.dma_start(out=DBG_E[rows, :], in_=eidx.t[:]), dbgch, reads=[eidx])
            def stage_Y(kt):
                x2 = x2s[kt % 2]; h2 = h2s[kt % 2]; eidx = eidxs[kt % 2]; gsm = gsms[kt % 2]
                rows = slice(kt * 128, (kt + 1) * 128)
                for j in range(128):
                    ub = ubuf[j % (2 * NU)]
                    DMA("gpsimd", lambda e, ub=ub, j=j: e.indirect_dma_start(
                        out=ub.t[:, :], out_offset=None, in_=UB, in_offset=bass.IndirectOffsetOnAxis(ap=eidx.t[:, j:j + 1], axis=0)),
                        uch[j % (2 * NU)], reads=[eidx], writes=[ub])
                    V(lambda e, ub=ub, j=j: e.scalar_tensor_tensor(out=junkD.t[:], in0=ub.t[:, :], scalar=1.0, in1=h2.t[:], op0=OP.mult, op1=OP.mult,
                                                                   accum_out=av.t[:, j:j + 1]), reads=[ub, h2], writes=[junkD, av])
                    yield
                A(lambda e: e.activation(out=wgt.t[:], in_=av.t[:], func=AF.Gelu), reads=[av], writes=[wgt])
                V(lambda e: e.tensor_tensor(out=wgt.t[:], in0=wgt.t[:], in1=gsm.t[:], op=OP.mult), reads=[wgt, gsm], writes=[wgt])
                for j in range(128):
                    vb = vbuf[j % (2 * NU)]
                    DMA("gpsimd", lambda e, vb=vb, j=j: e.indirect_dma_start(
                        out=vb.t[:, :], out_offset=None, in_=VB, in_offset=bass.IndirectOffsetOnAxis(ap=eidx.t[:, j:j + 1], axis=0)),
                        vch2[j % (2 * NU)], reads=[eidx], writes=[vb])
                    dgb = dg[j % 4]
                    A(lambda e, dgb=dgb, j=j: e.activation(out=dgb.t[:], in_=identb.t[:], func=AF.Identity, scale=wgt.t[:, j:j + 1]),
                      reads=[identb, wgt], writes=[dgb])
                    for nb in range(2):
                        T(lambda e, nb=nb, dgb=dgb, vb=vb, j=j: e.matmul(Fv[nb].t[:, :], lhsT=dgb.t[:], rhs=vb.t[:, nb * 512:(nb + 1) * 512],
                                                                         start=(j == 0), stop=(j == 127)), reads=[dgb, vb], writes=[Fv[nb]])
                    yield
                if debug:
                    for nb in range(2):
                        ns = slice(nb * 512, (nb + 1) * 512)
                        V(lambda e, nb=nb, ns=ns: e.tensor_copy(out=dbt.t[:, ns], in_=Fv[nb].t[:]), reads=[Fv[nb]], writes=[dbt])
                    DMA("sync", lambda e, rows=rows: e.dma_start(out=DBG_PEER[rows, :], in_=dbt.t[:]), dbgch, reads=[dbt])
                    k.wait_all("vector", chans=[dbgch], engines=False)
                for nb in range(2):
                    ns = slice(nb * 512, (nb + 1) * 512)
                    V(lambda e, nb=nb, ns=ns: e.tensor_tensor(out=x2.t[:, ns], in0=Fv[nb].t[:], in1=x2.t[:, ns], op=OP.add),
                      reads=[Fv[nb], x2], writes=[x2])
                A(lambda e: e.activation(out=junkD.t[:], in_=x2.t[:], func=AF.Square, accum_out=ss3.t[:, 0:1]), reads=[x2], writes=[junkD, ss3])
                A(lambda e: e.activation(out=std3.t[:], in_=ss3.t[:], func=AF.Sqrt, scale=1.0 / D, bias=epsb.t[:, 0:1]),
                  reads=[ss3, epsb], writes=[std3])
                V(lambda e: e.reciprocal(out=rstd3.t[:], in_=std3.t[:]), reads=[std3], writes=[rstd3])
                V(lambda e: e.scalar_tensor_tensor(out=x2.t[:], in0=x2.t[:], scalar=rstd3.t[:, 0:1], in1=gfbc.t[:], op0=OP.mult, op1=OP.mult),
                  reads=[x2, rstd3, gfbc], writes=[x2])
                DMA("sync", lambda e, rows=rows: e.dma_start(out=out[rows, :], in_=x2.t[:]), ochs[kt % 2], reads=[x2])
                yield

            NY = 257

            def run_pair(gx, gy, wx):
                cx = 0.0
                iy = 0
                for w in gx:
                    cx += w
                    target = min(NY, int(NY * cx / (0.85 * wx)))
                    while gy is not None and iy < target:
                        if next(gy, "end") == "end":
                            gy = None
                        iy += 1
                if gy is not None:
                    for _ in gy:
                        pass

            prevY = None
            for kt in range(nt_b):
                wx = 1.6 * 3 * (9 + kt // 4) + 6.9 * NIT + 2.5 * 2 * (33 + kt) + 28.0 + 0.45 * 120 + 40.0
                run_pair(stage_X(kt), prevY, wx)
                prevY = stage_Y(kt)
            for _ in prevY:
                pass
            k.barrier()
            k.flush()
    return nc


def _consts():
    f32 = np.float32
    lg = np.log(1.0 - 2.0 ** (-5.0 - np.arange(4, dtype=np.float64)))
    j = np.arange(128)
    jj = j % 64
    kd = np.exp(lg[None, :] * (63 - jj)[:, None])
    kdec0 = np.repeat(kd * (j < 64)[:, None], 64, axis=1)
    kdec1 = np.repeat(kd * (j >= 64)[:, None], 64, axis=1)
    c_kdec = np.concatenate([kdec0, kdec1], axis=1).astype(f32)
    qd = np.exp(lg[:, None] * (jj + 1.0)[None, :]) / 8.0
    c_qdec = np.broadcast_to(qd.reshape(1, 512), (64, 512)).astype(f32).copy()
    same = (j[:, None] // 64) == (j[None, :] // 64)
    dm = np.stack([np.exp(lg[h] * np.abs(j[:, None] - j[None, :])) * same / 8.0 for h in range(4)], axis=1)
    c_dmat = dm.reshape(128, 512).astype(f32)
    gd = np.exp(lg * 64)
    c_gdec = np.broadcast_to(np.repeat(gd, 128).reshape(1, 512), (64, 512)).astype(f32).copy()
    diag = np.zeros((128, 4, 4, 128), np.float64)
    for r in range(4):
        for b in range(4):
            if b > r:
                diag[:, r, b, :] = -BIG
            elif b == r:
                diag[:64, r, b, 64:] = -BIG
    c_diag = diag.reshape(128, 2048).astype(f32)
    c_iota = np.broadcast_to(np.arange(16, dtype=f32)[None, :], (128, 16)).copy()
    c_pow = np.broadcast_to((-(2.0 ** -(np.arange(NIT) + 2.0)))[None, :], (128, NIT)).astype(f32).copy()
    nb = np.array([512 * (9 + kt // 4) - 511 for kt in range(NT)], dtype=f32)
    c_nb = np.broadcast_to(nb[None, :], (128, NT)).copy()
    return dict(c_nb=c_nb, c_ident=np.eye(128, dtype=f32), c_kdec=c_kdec, c_qdec=c_qdec, c_dmat=c_dmat, c_gdec=c_gdec,
                c_diag=c_diag, c_iota=c_iota, c_pow=c_pow)


def _ropetab(pos):
    pos = pos.astype(np.float32)
    inv_r = (np.float32(10000.0) ** (-np.arange(32, dtype=np.float32) / np.float32(32))).astype(np.float32)
    inv_d = (np.float32(500000.0) ** (-np.arange(8, dtype=np.float32) / np.float32(8))).astype(np.float32)
    ar = pos[:, None] * inv_r[None, :]
    ad = pos[:, None] * inv_d[None, :]
    return np.concatenate([np.cos(ar), np.sin(ar), np.cos(ad), np.sin(ad)], axis=1).astype(np.float32)


_NC_CACHE = {}


def _make_in_maps(x, attn_norm, w_in, ret_gn, w_ret_o, w_dsa_o, w_out, ffn_norm, peer_wq, peer_subkeys, peer_u, peer_v, final_norm):
    f32 = np.float32
    cs = _consts()
    w_in_p = np.concatenate([w_in[0][:, :3396], np.zeros((D, 60), f32), w_in[0][:, 3396:]], axis=1)
    w_in_p = np.ascontiguousarray(w_in_p, dtype=f32)
    bc = lambda v, n: np.ascontiguousarray(np.broadcast_to(np.asarray(v, f32).reshape(1, n), (128, n)))
    shared = dict(
        w_in=w_in_p, w_ro=np.ascontiguousarray(w_ret_o[0]), w_do=np.ascontiguousarray(w_dsa_o[0]),
        w_out=np.ascontiguousarray(w_out[0]), w_q=np.ascontiguousarray(peer_wq[0]),
        subk=np.ascontiguousarray(peer_subkeys[0].reshape(16, 128, 128)),
        peer_u=np.ascontiguousarray(peer_u[0]), peer_v=np.ascontiguousarray(peer_v[0]),
        g_attn=bc(attn_norm[0], D), g_gn=bc(ret_gn[0], 512), g_ffn=bc(ffn_norm[0], D), g_fin=bc(final_norm, D), **cs)
    in_maps = []
    for c in range(8):
        b, hf = c // 2, c % 2
        xo = np.ascontiguousarray(x[b, hf * LH:(hf + 1) * LH])
        xp = np.ascontiguousarray(x[b, 0:LH]) if hf == 1 else np.zeros((LH, D), f32)
        pos = np.concatenate([np.arange(LH), hf * LH + np.arange(LH)])
        m = dict(shared)
        m.update(xp=xp, xo=xo, ropetab=_ropetab(pos), c_pbias=np.full((128, 1), 0.0 if hf == 1 else -BIG, f32))
        in_maps.append(m)
    return in_maps


def kernel(x, attn_norm, w_in, ret_gn, w_ret_o, w_dsa_o, w_out, ffn_norm, peer_wq, peer_subkeys, peer_u, peer_v, final_norm):
    args = [np.asarray(a) for a in (x, attn_norm, w_in, ret_gn, w_ret_o, w_dsa_o, w_out, ffn_norm, peer_wq,
                                    peer_subkeys, peer_u, peer_v, final_norm)]
    in_maps = _make_in_maps(*args)
    if "nc" not in _NC_CACHE:
        _NC_CACHE["nc"] = build_program(DEBUG)
    res = run_bass_kernel_spmd(_NC_CACHE["nc"], in_maps, core_ids=list(range(8)))
    outp = np.empty((4, L, D), np.float32)
    for c in range(8):
        b, hf = c // 2, c % 2
        outp[b, hf * LH:(hf + 1) * LH] = np.asarray(res.results[c]["out"], np.float32)
    if DEBUG:
        _NC_CACHE["res"] = res
    return outp
```
